# Optimizing a Trainium2 kernel written in Bass

```python
import math
import jax, jax.numpy as jnp
from jax import lax
import numpy as np

D_MODEL = 1024
BATCH = 2
SEQ = 8192
DEPTH = 2

N_EVEN = (DEPTH + 1) // 2
N_ODD = DEPTH // 2
GROUP_WIDTH = D_MODEL // 2

RET_HEADS = 4
RET_HEAD_DIM = GROUP_WIDTH // RET_HEADS
RET_W = RET_HEADS * RET_HEAD_DIM
RET_THETA = 10000.0
DIFF_HEADS = 4
DIFF_HEAD_DIM = GROUP_WIDTH // (2 * DIFF_HEADS)
DIFF_V_DIM = 2 * DIFF_HEAD_DIM
DIFF_QK_W = 2 * DIFF_HEADS * DIFF_HEAD_DIM
DIFF_V_W = DIFF_HEADS * DIFF_V_DIM
ROPE_THETA = 500000.0
DIFF_ROT_DIM = DIFF_HEAD_DIM // 4
MLA_HEADS = 4
MLA_Q_RANK = 256
MLA_KV_RANK = 128
MLA_NOPE = 64
MLA_ROPE = 32
MLA_V = GROUP_WIDTH // MLA_HEADS
GLA_HEADS = 4
GLA_K_DIM = GROUP_WIDTH // 2 // GLA_HEADS
GLA_V_DIM = GROUP_WIDTH // GLA_HEADS
GLA_K_W = GLA_HEADS * GLA_K_DIM
GLA_V_W = GLA_HEADS * GLA_V_DIM
GLA_GATE_RANK = 16
GLA_TAU = 16.0
CHUNK = 64
Q_BLOCK = 128
N_GROUPS = 4
EXPERTS_PER_GROUP = 8
N_EXPERTS = N_GROUPS * EXPERTS_PER_GROUP
TOP_K_IN_GROUP = 2
D_EXPERT = 512
MOE_BLOCK = 128
ALPHA = (2.0 * DEPTH) ** 0.25
BETA = (8.0 * DEPTH) ** -0.25
LN_EPS = 1e-5

EVEN_SPLITS = (RET_W, RET_W, RET_W, RET_W, DIFF_QK_W, DIFF_QK_W, DIFF_V_W)
EVEN_VALUE_BLOCKS = (2, 6)
EVEN_IN = sum(EVEN_SPLITS)
ODD_SPLITS = (MLA_Q_RANK, MLA_KV_RANK, MLA_ROPE, GLA_K_W, GLA_K_W, GLA_V_W, GLA_V_W, GLA_GATE_RANK, GLA_GATE_RANK)
ODD_VALUE_BLOCKS = (5,)
ODD_IN = sum(ODD_SPLITS)

kernel_name = 'hybrid_retnet_diffattn_mla_gla_hmoe_encoder'

F32 = jnp.float32


def _split(z, sizes):
    return jnp.split(z, np.cumsum(sizes)[:-1].tolist(), axis=-1)


def _heads(z, h):
    b, t, _ = z.shape
    return z.reshape(b, t, h, -1).transpose(0, 2, 1, 3)


def _merge(z):
    b, h, t, d = z.shape
    return z.transpose(0, 2, 1, 3).reshape(b, t, h * d)


def _layer_norm(x, g, b):
    xf = x.astype(F32)
    mu = jnp.mean(xf, -1, keepdims=True)
    var = jnp.mean(jnp.square(xf - mu), -1, keepdims=True)
    return ((xf - mu) * lax.rsqrt(var + LN_EPS) * g + b).astype(x.dtype)


def _group_norm(z):
    zf = z.astype(F32)
    mu = jnp.mean(zf, -1, keepdims=True)
    var = jnp.mean(jnp.square(zf - mu), -1, keepdims=True)
    return ((zf - mu) * lax.rsqrt(var + LN_EPS)).astype(z.dtype)


def _rms_norm(z, g):
    zf = z.astype(F32)
    return (zf * lax.rsqrt(jnp.mean(jnp.square(zf), -1, keepdims=True) + 1e-6) * g).astype(z.dtype)


def _rotary(x, rot_dim, theta):
    t = x.shape[2]
    half = rot_dim // 2
    pos = jnp.arange(t, dtype=F32)
    inv = jnp.power(jnp.float32(theta), -jnp.arange(0, rot_dim, 2, dtype=F32) / rot_dim)
    ang = pos[:, None] * inv[None, :]
    cos, sin = jnp.cos(ang), jnp.sin(ang)
    xr = x[..., :rot_dim].astype(F32)
    x1, x2 = xr[..., :half], xr[..., half:]
    rot = jnp.concatenate([x1 * cos - x2 * sin, x2 * cos + x1 * sin], -1).astype(x.dtype)
    return jnp.concatenate([rot, x[..., rot_dim:]], -1)


def _chunk_scan(q, k, v, log_a, strict):
    b, h, t, dk = q.shape
    dv = v.shape[-1]
    n = t // CHUNK

    def chunks(z):
        return z.reshape(b, h, n, CHUNK, z.shape[-1]).transpose(2, 0, 1, 3, 4)

    cum = jnp.cumsum(chunks(log_a.astype(F32)), axis=3)
    mask = jnp.tril(jnp.ones((CHUNK, CHUNK), bool), -1 if strict else 0)[None, None, :, :, None]

    def step(state, inp):
        qi, ki, vi, bi = inp
        qi, ki, vi = qi.astype(F32), ki.astype(F32), vi.astype(F32)
        o_inter = jnp.einsum('bhck,bhkv->bhcv', qi * jnp.exp(bi), state)
        diff = bi[:, :, :, None, :] - bi[:, :, None, :, :]
        decay = jnp.where(mask, jnp.exp(jnp.where(mask, diff, 0.0)), 0.0)
        scores = jnp.sum(qi[:, :, :, None, :] * ki[:, :, None, :, :] * decay, axis=-1)
        o_intra = jnp.einsum('bhij,bhjv->bhiv', scores, vi)
        b_last = bi[:, :, -1:, :]
        state = jnp.exp(b_last[:, :, 0, :, None]) * state + jnp.einsum('bhck,bhcv->bhkv', ki * jnp.exp(b_last - bi), vi)
        return state, o_inter + o_intra

    state0 = jnp.zeros((b, h, dk, dv), F32)
    _, o = lax.scan(step, state0, (chunks(q), chunks(k), chunks(v), cum))
    return o.transpose(1, 2, 0, 3, 4).reshape(b, h, t, dv).astype(v.dtype)


def _bidirectional_scan(q, k, v, log_a_fwd, log_a_bwd):
    rev = lambda z: jnp.flip(z, axis=2)
    fwd = _chunk_scan(q, k, v, log_a_fwd, strict=False)
    bwd = rev(_chunk_scan(rev(q), rev(k), rev(v), rev(log_a_bwd), strict=True))
    return fwd + bwd


def _query_blocks(z):
    b, h, t, d = z.shape
    return z.reshape(b, h, t // Q_BLOCK, Q_BLOCK, d).transpose(2, 0, 1, 3, 4)


def _unblock(o):
    nb, b, h, qb, d = o.shape
    return o.transpose(1, 2, 0, 3, 4).reshape(b, h, nb * qb, d)


def _blocked_attention(q, k, v, scale):
    def one(qi):
        s = jnp.einsum('bhqd,bhkd->bhqk', qi, k).astype(F32) * scale
        p = jax.nn.softmax(s, axis=-1)
        return jnp.einsum('bhqk,bhkd->bhqd', p.astype(v.dtype), v)
    return _unblock(lax.map(one, _query_blocks(q)))


def _blocked_diff_attention(q1, q2, k1, k2, v, lam, scale):
    def one(qs):
        qa, qb = qs
        p1 = jax.nn.softmax(jnp.einsum('bhqd,bhkd->bhqk', qa, k1).astype(F32) * scale, axis=-1)
        p2 = jax.nn.softmax(jnp.einsum('bhqd,bhkd->bhqk', qb, k2).astype(F32) * scale, axis=-1)
        return jnp.einsum('bhqk,bhkd->bhqd', (p1 - lam * p2).astype(v.dtype), v)
    return _unblock(lax.map(one, (_query_blocks(q1), _query_blocks(q2))))


def _retention_diff_mixer(x, w_in, ret_decay_f, ret_decay_b, lq1, lk1, lq2, lk2, subln, w_out, layer_idx):
    rq, rk, rv, rg, dq, dk, dv = _split(x @ w_in, EVEN_SPLITS)
    b, t, _ = x.shape
    q = _rotary(_heads(rq, RET_HEADS), RET_HEAD_DIM, RET_THETA)
    k = _rotary(_heads(rk, RET_HEADS), RET_HEAD_DIM, RET_THETA) * (RET_HEAD_DIM ** -0.5)
    v = _heads(rv, RET_HEADS)
    la_f = jnp.broadcast_to((-jnp.exp(ret_decay_f.astype(F32)))[None, :, None, None], (b, RET_HEADS, t, 1))
    la_b = jnp.broadcast_to((-jnp.exp(ret_decay_b.astype(F32)))[None, :, None, None], (b, RET_HEADS, t, 1))
    ret = _merge(_group_norm(_bidirectional_scan(q, k, v, la_f, la_b))) * jax.nn.silu(rg)
    def pair(z):
        z = z.reshape(b, t, DIFF_HEADS, 2, DIFF_HEAD_DIM).transpose(0, 2, 3, 1, 4)
        return (_rotary(z[:, :, 0], DIFF_ROT_DIM, ROPE_THETA), _rotary(z[:, :, 1], DIFF_ROT_DIM, ROPE_THETA))
    q1, q2 = pair(dq)
    k1, k2 = pair(dk)
    vv = _heads(dv, DIFF_HEADS)
    lam_init = 0.8 - 0.6 * math.exp(-0.3 * layer_idx)
    lam = (jnp.exp(jnp.sum(lq1 * lk1)) - jnp.exp(jnp.sum(lq2 * lk2))).astype(F32) + lam_init
    o = _blocked_diff_attention(q1, q2, k1, k2, vv, lam, DIFF_HEAD_DIM ** -0.5)
    diff = _merge(_rms_norm(o, subln) * (1.0 - lam_init))
    return jnp.concatenate([ret, diff], axis=-1) @ w_out


def _mla_gla_mixer(x, w_in, q_norm, w_uq, kv_norm, w_ukv, gla_w2_f, gla_b_f, gla_w2_b, gla_b_b, gla_norm, w_out):
    cq, ckv, krope, gq, gk, gv, gg, lr_f, lr_b = _split(x @ w_in, ODD_SPLITS)
    qh = _heads(_rms_norm(cq, q_norm) @ w_uq, MLA_HEADS)
    q = jnp.concatenate([qh[..., :MLA_NOPE], _rotary(qh[..., MLA_NOPE:], MLA_ROPE, ROPE_THETA)], -1)
    kvh = _heads(_rms_norm(ckv, kv_norm) @ w_ukv, MLA_HEADS)
    k_nope, v = kvh[..., :MLA_NOPE], kvh[..., MLA_NOPE:]
    k_rope = _rotary(krope[:, None], MLA_ROPE, ROPE_THETA)
    k = jnp.concatenate([k_nope, jnp.broadcast_to(k_rope, k_nope.shape[:-1] + (MLA_ROPE,))], -1)
    mla = _merge(_blocked_attention(q, k, v, (MLA_NOPE + MLA_ROPE) ** -0.5))
    def log_gate(lr, w2, bias):
        return _heads(jax.nn.log_sigmoid((lr @ w2 + bias).astype(F32)) / GLA_TAU, GLA_HEADS)
    gq_h = _heads(gq, GLA_HEADS) * (GLA_K_DIM ** -0.5)
    o = _bidirectional_scan(gq_h, _heads(gk, GLA_HEADS), _heads(gv, GLA_HEADS), log_gate(lr_f, gla_w2_f, gla_b_f), log_gate(lr_b, gla_w2_b, gla_b_b))
    gla = _merge(_rms_norm(o, gla_norm)) * jax.nn.silu(gg)
    return jnp.concatenate([mla, gla], axis=-1) @ w_out


def _hier_moe(x, w_grp, b_grp, w_exp, b_exp, w_gate, w_up, w_down):
    b, t, d = x.shape
    n = b * t
    xf = x.reshape(n, d)
    grp_logits = (xf @ w_grp + b_grp).astype(F32)
    g_idx = jnp.argmax(grp_logits, axis=-1).astype(jnp.int32)
    p_grp = jnp.take_along_axis(jax.nn.softmax(grp_logits, -1), g_idx[:, None], axis=1)[:, 0]
    exp_logits = (xf @ w_exp + b_exp).astype(F32).reshape(n, N_GROUPS, EXPERTS_PER_GROUP)
    in_grp = jnp.take_along_axis(exp_logits, g_idx[:, None, None], axis=1)[:, 0]
    top_l, top_e = lax.top_k(in_grp, TOP_K_IN_GROUP)
    gate = p_grp[:, None] * jax.nn.softmax(top_l, axis=-1)
    eid = (g_idx[:, None] * EXPERTS_PER_GROUP + top_e).reshape(-1).astype(jnp.int32)
    tok = jnp.repeat(jnp.arange(n, dtype=jnp.int32), TOP_K_IN_GROUP)
    gw = gate.reshape(-1)
    a = eid.shape[0]
    order = jnp.argsort(eid)
    eid_s, tok_s, gw_s = eid[order], tok[order], gw[order]
    counts = jnp.bincount(eid, length=N_EXPERTS).astype(jnp.int32)
    padded = (counts + MOE_BLOCK - 1) // MOE_BLOCK * MOE_BLOCK
    start = jnp.cumsum(counts) - counts
    pend = jnp.cumsum(padded)
    pstart = pend - padded
    dest = pstart[eid_s] + (jnp.arange(a, dtype=jnp.int32) - start[eid_s])
    cap = a + N_EXPERTS * MOE_BLOCK
    nb = cap // MOE_BLOCK
    buf_tok = jnp.full((cap,), n, jnp.int32).at[dest].set(tok_s)
    buf_w = jnp.zeros((cap,), F32).at[dest].set(gw_s)
    blk_e = jnp.minimum(jnp.searchsorted(pend, jnp.arange(nb, dtype=jnp.int32) * MOE_BLOCK, side='right'), N_EXPERTS - 1).astype(jnp.int32)
    x_pad = jnp.concatenate([xf, jnp.zeros((1, d), xf.dtype)], axis=0)

    def expert_block(args):
        idx, e = args
        xb = x_pad[idx]
        hdn = jax.nn.silu(xb @ w_gate[e]) * (xb @ w_up[e])
        return hdn @ w_down[e]

    y = lax.map(expert_block, (buf_tok.reshape(nb, MOE_BLOCK), blk_e)).reshape(cap, d)
    out = jnp.zeros((n + 1, d), x.dtype).at[buf_tok].add(y * buf_w[:, None].astype(y.dtype))
    return out[:n].reshape(b, t, d)


def setup_inputs(seed: int = 0) -> dict:
    key = jax.random.key(seed)
    ks = iter(jax.random.split(key, 40))

    def nrm(shape, scale):
        return jax.random.normal(next(ks), shape, jnp.float32) * scale

    def gain(shape):
        return 1.0 + nrm(shape, 0.02)

    ev_col = np.concatenate([np.full((w,), BETA if i in EVEN_VALUE_BLOCKS else 1.0, np.float32) for i, w in enumerate(EVEN_SPLITS)])
    od_col = np.concatenate([np.full((w,), BETA if i in ODD_VALUE_BLOCKS else 1.0, np.float32) for i, w in enumerate(ODD_SPLITS)])
    ukv_col = np.tile(np.concatenate([np.ones((MLA_NOPE,), np.float32), np.full((MLA_V,), BETA, np.float32)]), MLA_HEADS)
    gam = 1.0 - 2.0 ** (-5.0 - np.arange(RET_HEADS))
    ret_base = jnp.asarray(np.log(-np.log(gam)), jnp.float32)
    return {
        'x': nrm((BATCH, SEQ, D_MODEL), 1.0),
        'ev_w_in': nrm((N_EVEN, D_MODEL, EVEN_IN), D_MODEL ** -0.5) * jnp.asarray(ev_col),
        'ev_ret_decay_f': ret_base + nrm((N_EVEN, RET_HEADS), 0.05),
        'ev_ret_decay_b': ret_base + nrm((N_EVEN, RET_HEADS), 0.05),
        'ev_lq1': nrm((N_EVEN, DIFF_HEAD_DIM), 0.1),
        'ev_lk1': nrm((N_EVEN, DIFF_HEAD_DIM), 0.1),
        'ev_lq2': nrm((N_EVEN, DIFF_HEAD_DIM), 0.1),
        'ev_lk2': nrm((N_EVEN, DIFF_HEAD_DIM), 0.1),
        'ev_subln': gain((N_EVEN, DIFF_V_DIM)),
        'ev_w_out': nrm((N_EVEN, D_MODEL, D_MODEL), D_MODEL ** -0.5 * BETA),
        'od_w_in': nrm((N_ODD, D_MODEL, ODD_IN), D_MODEL ** -0.5) * jnp.asarray(od_col),
        'od_q_norm': gain((N_ODD, MLA_Q_RANK)),
        'od_w_uq': nrm((N_ODD, MLA_Q_RANK, MLA_HEADS * (MLA_NOPE + MLA_ROPE)), MLA_Q_RANK ** -0.5),
        'od_kv_norm': gain((N_ODD, MLA_KV_RANK)),
        'od_w_ukv': nrm((N_ODD, MLA_KV_RANK, MLA_HEADS * (MLA_NOPE + MLA_V)), MLA_KV_RANK ** -0.5) * jnp.asarray(ukv_col),
        'od_gla_w2_f': nrm((N_ODD, GLA_GATE_RANK, GLA_K_W), GLA_GATE_RANK ** -0.5),
        'od_gla_b_f': nrm((N_ODD, GLA_K_W), 0.5),
        'od_gla_w2_b': nrm((N_ODD, GLA_GATE_RANK, GLA_K_W), GLA_GATE_RANK ** -0.5),
        'od_gla_b_b': nrm((N_ODD, GLA_K_W), 0.5),
        'od_gla_norm': gain((N_ODD, GLA_V_DIM)),
        'od_w_out': nrm((N_ODD, D_MODEL, D_MODEL), D_MODEL ** -0.5 * BETA),
        'ln1_g': gain((DEPTH, D_MODEL)),
        'ln1_b': nrm((DEPTH, D_MODEL), 0.02),
        'ln2_g': gain((DEPTH, D_MODEL)),
        'ln2_b': nrm((DEPTH, D_MODEL), 0.02),
        'moe_w_grp': nrm((DEPTH, D_MODEL, N_GROUPS), D_MODEL ** -0.5),
        'moe_b_grp': nrm((DEPTH, N_GROUPS), 0.01),
        'moe_w_exp': nrm((DEPTH, D_MODEL, N_EXPERTS), D_MODEL ** -0.5),
        'moe_b_exp': nrm((DEPTH, N_EXPERTS), 0.01),
        'moe_w_gate': nrm((DEPTH, N_EXPERTS, D_MODEL, D_EXPERT), D_MODEL ** -0.5),
        'moe_w_up': nrm((DEPTH, N_EXPERTS, D_MODEL, D_EXPERT), D_MODEL ** -0.5 * BETA),
        'moe_w_down': nrm((DEPTH, N_EXPERTS, D_EXPERT, D_MODEL), D_EXPERT ** -0.5 * BETA),
    }


def reference(x, ev_w_in, ev_ret_decay_f, ev_ret_decay_b, ev_lq1, ev_lk1, ev_lq2, ev_lk2, ev_subln, ev_w_out,
              od_w_in, od_q_norm, od_w_uq, od_kv_norm, od_w_ukv, od_gla_w2_f, od_gla_b_f, od_gla_w2_b, od_gla_b_b, od_gla_norm, od_w_out,
              ln1_g, ln1_b, ln2_g, ln2_b,
              moe_w_grp, moe_b_grp, moe_w_exp, moe_b_exp, moe_w_gate, moe_w_up, moe_w_down):
    for i in range(DEPTH):
        j = i // 2
        if i % 2 == 0:
            mix = _retention_diff_mixer(x, ev_w_in[j], ev_ret_decay_f[j], ev_ret_decay_b[j], ev_lq1[j], ev_lk1[j], ev_lq2[j], ev_lk2[j], ev_subln[j], ev_w_out[j], i)
        else:
            mix = _mla_gla_mixer(x, od_w_in[j], od_q_norm[j], od_w_uq[j], od_kv_norm[j], od_w_ukv[j], od_gla_w2_f[j], od_gla_b_f[j], od_gla_w2_b[j], od_gla_b_b[j], od_gla_norm[j], od_w_out[j])
        x = _layer_norm(ALPHA * x + mix, ln1_g[i], ln1_b[i])
        ffn = _hier_moe(x, moe_w_grp[i], moe_b_grp[i], moe_w_exp[i], moe_b_exp[i], moe_w_gate[i], moe_w_up[i], moe_w_down[i])
        x = _layer_norm(ALPHA * x + ffn, ln2_g[i], ln2_b[i])
    return x
```

```python
import contextlib
import math
import numpy as np
import concourse.bass as bass
import concourse.mybir as mybir
from concourse.bass_utils import run_bass_kernel_spmd

F32 = mybir.dt.float32
BF16 = mybir.dt.bfloat16
AF = mybir.ActivationFunctionType
ALU = mybir.AluOpType
AX = mybir.AxisListType

DEPTH = 2
ALPHA = (2.0 * DEPTH) ** 0.25
LN_EPS = 1e-5
NCORES = 8


class _Op:
    __slots__ = ("eng", "fn", "deps", "is_dma", "stream", "sidx", "signal", "cnt")


class Prog:
    ENGS = ("pe", "act", "dve", "pool", "sp")

    def __init__(self, nc, same_engine_sync=True):
        self.nc = nc
        self.ops = []
        self.lastw = {}
        self.readers = {}
        self.streams = {}
        self.same_engine_sync = same_engine_sync
        self.bank_last = {}

    def _add(self, eng, fn, reads, writes, is_dma=False, stream=None):
        o = _Op()
        o.eng = eng; o.fn = fn; o.is_dma = is_dma; o.stream = stream
        o.signal = False; o.cnt = 0; o.sidx = 0
        deps = set()
        for r in reads:
            if r in self.lastw:
                deps.add(self.lastw[r])
        for w in writes:
            if w in self.lastw:
                deps.add(self.lastw[w])
            for rr in self.readers.get(w, ()):
                deps.add(rr)
        oid = len(self.ops)
        banks = set()
        for k_ in tuple(reads) + tuple(writes):
            if k_.startswith("pb") and k_[2].isdigit():
                banks.add(k_[2])
        for b_ in banks:
            lb = self.bank_last.get(b_)
            if lb is not None and self.ops[lb].eng != eng:
                deps.add(lb)
            self.bank_last[b_] = oid
        o.deps = deps
        if is_dma:
            n = self.streams.get(stream, 0) + 1
            self.streams[stream] = n
            o.sidx = n
        self.ops.append(o)
        for w in writes:
            self.lastw[w] = oid
            self.readers[w] = []
        for r in reads:
            if r not in writes:
                self.readers.setdefault(r, []).append(oid)
        return oid

    def op(self, eng, fn, reads=(), writes=()):
        return self._add(eng, fn, tuple(reads), tuple(writes))

    def dma(self, q, stream, out, in_, reads=(), writes=()):
        return self._add(q, (out, in_), tuple(reads), tuple(writes), True, stream)

    def _skip(self, do, o):
        return (do.eng == o.eng and not o.is_dma and not do.is_dma
                and (do.eng == "pe" or not self.same_engine_sync))

    def emit(self):
        nc = self.nc
        ops = self.ops
        for o in ops:
            for d in o.deps:
                do = ops[d]
                if do.is_dma or self._skip(do, o):
                    continue
                do.signal = True
        cnt = {e: 0 for e in self.ENGS}
        for o in ops:
            if not o.is_dma and o.signal:
                cnt[o.eng] += 1
                o.cnt = cnt[o.eng]
        with contextlib.ExitStack() as st:
            esem = {e: st.enter_context(nc.semaphore("s_" + e)) for e in self.ENGS}
            ssem = {s: st.enter_context(nc.semaphore("d_" + s)) for s in self.streams}
            block = st.enter_context(nc.Block())
            per = {e: [i for i, o in enumerate(ops) if o.eng == e] for e in self.ENGS}

            def run(engname, eobj):
                known = {}
                for i in per[engname]:
                    o = ops[i]
                    need = {}
                    for d in o.deps:
                        do = ops[d]
                        if do.is_dma:
                            key = ("d", do.stream); val = 16 * do.sidx
                        else:
                            if self._skip(do, o):
                                continue
                            key = ("e", do.eng); val = do.cnt
                        if val > need.get(key, 0):
                            need[key] = val
                    for key, val in need.items():
                        if known.get(key, 0) >= val:
                            continue
                        known[key] = val
                        sem = ssem[key[1]] if key[0] == "d" else esem[key[1]]
                        eobj.wait_ge(sem, val)
                    if o.is_dma:
                        out, in_ = o.fn
                        eobj.dma_start(out=out, in_=in_).then_inc(ssem[o.stream], 16)
                    else:
                        ins = o.fn(eobj)
                        if o.signal:
                            ins.then_inc(esem[engname], 1)
                if engname == "sp":
                    for s, n in self.streams.items():
                        eobj.wait_ge(ssem[s], 16 * n)

            @block.tensor
            def _(e): run("pe", e)

            @block.scalar
            def _(e): run("act", e)

            @block.vector
            def _(e): run("dve", e)

            @block.gpsimd
            def _(e): run("pool", e)

            @block.sync
            def _(e): run("sp", e)


def _bcast_rows(handle, row, n, parts=128):
    return bass.AP(handle, row * n, [[0, parts], [1, n]])


def build_post(ntok=2048):
    nc = bass.Bass("TRN2", target_bir_lowering=False)
    NT = ntok // 128
    NB = ntok // 512
    D = 1024
    NE = 32
    xres_h = nc.dram_tensor("xres", [ntok, D], F32, kind="ExternalInput")
    catT_h = nc.dram_tensor("catT", [D, ntok], F32, kind="ExternalInput")
    wout_h = nc.dram_tensor("w_out", [D, D], F32, kind="ExternalInput")
    lnp_h = nc.dram_tensor("lnp", [4, D], F32, kind="ExternalInput")
    wr_h = nc.dram_tensor("w_r", [D, 36], F32, kind="ExternalInput")
    br_h = nc.dram_tensor("b_r", [1, 36], F32, kind="ExternalInput")
    wg_h = nc.dram_tensor("w_gate", [NE, D, 512], F32, kind="ExternalInput")
    wu_h = nc.dram_tensor("w_up", [NE, D, 512], F32, kind="ExternalInput")
    wd_h = nc.dram_tensor("w_down", [NE, 512, D], F32, kind="ExternalInput")
    xo_h = nc.dram_tensor("xo", [ntok, D], F32, kind="ExternalOutput")
    xres = xres_h.ap(); catT = catT_h.ap(); xo = xo_h.ap()
    BIG = 30000.0

    with contextlib.ExitStack() as st:
        def sb(name, shape, dt):
            return st.enter_context(nc.sbuf_tensor("s_" + name, shape, dt))
        wbuf = [sb("wbuf%d" % i, [128, 12288], BF16) for i in range(2)]
        x1T = sb("x1T", [128, 8, ntok], BF16)
        yacc = sb("yacc", [128, NT, D], F32)
        G = sb("G", [128, NT, NE], F32)
        gb = [sb("lng", [128, D], F32), sb("lnb", [128, D], F32)]
        wr = sb("wr", [128, 8, 36], F32)
        brb = sb("brb", [128, 36], F32)
        ident = sb("ident", [128, 128], F32)
        ones = sb("ones", [128, 128], F32)
        ct = [sb("ct%d" % i, [128, 8, 128], BF16) for i in range(2)]
        xt = [sb("xt%d" % i, [128, D], F32) for i in range(2)]
        x1f = [sb("x1f%d" % i, [128, 8, 128], F32) for i in range(2)]
        hT = [sb("hT%d" % i, [128, 4, 512], BF16) for i in range(2)]
        sg = [sb("sg%d" % i, [128, 512], F32) for i in range(2)]
        sm = [sb("sm%d" % i, [128, 256], F32) for i in range(2)]
        pb = [st.enter_context(nc.psum_tensor("pb%d" % i, [128, 512], F32)) for i in range(8)]

        P = Prog(nc)
        P.op("pool", lambda e: e.memset(ones[:], 1.0), writes=["ones"])
        P.op("pool", lambda e: e.affine_select(out=ident[:], in_=ones[:], pattern=[[-1, 128]],
                                               compare_op=ALU.is_equal, fill=0.0, base=0,
                                               channel_multiplier=1),
             reads=["ones"], writes=["ident"])
        wout_v = wbuf[0][:, 0:8192].rearrange("p (k n) -> p k n", k=8)
        P.dma("pool", "wg0", wout_v, wout_h.ap().rearrange("(k p) n -> p k n", p=128), writes=["w0g", "w0u"])
        P.dma("sp", "c_lng", gb[0][:], _bcast_rows(lnp_h, 0, D), writes=["lng"])
        P.dma("sp", "c_lnb", gb[1][:], _bcast_rows(lnp_h, 1, D), writes=["lnb"])
        P.dma("sp", "c_wr", wr[:], wr_h.ap().rearrange("(k p) n -> p k n", p=128), writes=["wr"])
        P.dma("sp", "c_brb", brb[:], _bcast_rows(br_h, 0, 36), writes=["brb"])

        def layer_norm(src_ap_fn, srckey, s, dst_ap, dstkey, alpha):
            smt = sm[s]; k = "sm%d" % s
            stats = smt[:, 0:12]; mv = smt[:, 12:14]; rs = smt[:, 14:15]
            P.op("dve", lambda e: e.bn_stats(out=smt[:, 0:6], in_=src_ap_fn(0, 512)), reads=[srckey], writes=[k + "a"])
            P.op("dve", lambda e: e.bn_stats(out=smt[:, 6:12], in_=src_ap_fn(512, 1024)), reads=[srckey], writes=[k + "b"])
            P.op("dve", lambda e: e.bn_aggr(out=mv, in_=stats), reads=[k + "a", k + "b"], writes=[k + "mv"])
            P.op("act", lambda e: e.activation(out=rs, in_=smt[:, 13:14], func=AF.Sqrt, bias=LN_EPS,
                                               scale=alpha * alpha), reads=[k + "mv"], writes=[k + "rs"])
            P.op("dve", lambda e: e.reciprocal(rs, rs), reads=[k + "rs"], writes=[k + "rs"])
            if alpha != 1.0:
                P.op("dve", lambda e: e.tensor_scalar(rs, rs, alpha, None, ALU.mult), reads=[k + "rs"], writes=[k + "rs"])
            P.op("dve", lambda e: e.tensor_scalar(dst_ap, src_ap_fn(0, 1024), smt[:, 12:13], rs, ALU.subtract, ALU.mult),
                 reads=[srckey, k + "mv", k + "rs"], writes=[dstkey])
            P.op("dve", lambda e: e.tensor_tensor(dst_ap, dst_ap, gb[0][:], ALU.mult), reads=[dstkey, "lng"], writes=[dstkey])
            P.op("dve", lambda e: e.tensor_tensor(dst_ap, dst_ap, gb[1][:], ALU.add), reads=[dstkey, "lnb"], writes=[dstkey])

        for i in range(NT):
            s = i % 2
            cts = ct[s]; xts = xt[s]; x1fs = x1f[s]; smt = sm[s]
            tsl = slice(i * 128, (i + 1) * 128)
            P.dma("pool", "ct%d" % s, cts[:], catT.rearrange("(k p) t -> p k t", p=128)[:, :, tsl], writes=["ct%d" % s])
            P.dma("sp", "xt%d" % s, xts[:], xres[tsl, :], writes=["xt%d" % s])
            for h in range(2):
                for kc in range(8):
                    P.op("pe", (lambda e, h=h, kc=kc, cts=cts: e.matmul(pb[h][:], cts[:, kc, :], wout_v[:, kc, h * 512:(h + 1) * 512],
                                                                        start=(kc == 0), stop=(kc == 7))),
                         reads=["ct%d" % s, "w0g", "w0u"], writes=["pb%d" % h])
            for h in range(2):
                P.op("dve", (lambda e, h=h, xts=xts: e.scalar_tensor_tensor(out=xts[:, h * 512:(h + 1) * 512], in0=xts[:, h * 512:(h + 1) * 512],
                                                                            scalar=ALPHA, in1=pb[h][:], op0=ALU.mult, op1=ALU.add)),
                     reads=["xt%d" % s, "pb%d" % h], writes=["xt%d" % s])
            layer_norm(lambda a, b, xts=xts: xts[:, a:b], "xt%d" % s, s, yacc[:, i, :], "yacc%d" % i, 1.0)
            for kc in range(8):
                P.op("pe", (lambda e, kc=kc, i=i: e.transpose(pb[2 + kc // 4][:, (kc % 4) * 128:(kc % 4 + 1) * 128],
                                                              yacc[:, i, kc * 128:(kc + 1) * 128], ident[:])),
                     reads=["yacc%d" % i, "ident"], writes=["pb%d" % (2 + kc // 4)])
            P.op("act", lambda e, x1fs=x1fs: e.copy(out=x1fs[:, 0:4, :], in_=pb[2][:].rearrange("p (k t) -> p k t", k=4)),
                 reads=["pb2"], writes=["x1f%da" % s])
            P.op("dve", lambda e, x1fs=x1fs: e.tensor_copy(out=x1fs[:, 4:8, :], in_=pb[3][:].rearrange("p (k t) -> p k t", k=4)),
                 reads=["pb3"], writes=["x1f%db" % s])
            P.op("act", lambda e, tsl=tsl: e.copy(out=x1T[:, 0:4, tsl], in_=pb[2][:].rearrange("p (k t) -> p k t", k=4)),
                 reads=["pb2"], writes=["x1T_%da" % i])
            P.op("dve", lambda e, tsl=tsl: e.tensor_copy(out=x1T[:, 4:8, tsl], in_=pb[3][:].rearrange("p (k t) -> p k t", k=4)),
                 reads=["pb3"], writes=["x1T_%db" % i])
            for kc in range(8):
                P.op("pe", (lambda e, kc=kc, x1fs=x1fs: e.matmul(pb[4][:, 0:36], x1fs[:, kc, :], wr[:, kc, :], start=(kc == 0), stop=(kc == 7))),
                     reads=["x1f%da" % s, "x1f%db" % s, "wr"], writes=["pb4"])
            k = "r%d" % s
            lg = smt[:, 16:52]; gl = smt[:, 16:20]; el = smt[:, 20:52]
            gmax = smt[:, 52:53]; ngmax = smt[:, 53:54]; gsum = smt[:, 54:55]
            oh = smt[:, 56:60]; pen = smt[:, 60:64]; eg = smt[:, 64:68]
            msk = smt[:, 68:100]; m1 = smt[:, 100:132]; m2 = smt[:, 132:164]; msk2 = smt[:, 164:196]
            top1 = smt[:, 196:197]; top2 = smt[:, 197:198]; dd = smt[:, 198:199]; ee = smt[:, 199:200]
            w1 = smt[:, 200:201]; w2 = smt[:, 201:202]; tmp = smt[:, 204:236]
            P.op("dve", lambda e, lg=lg: e.tensor_tensor(lg, pb[4][:, 0:36], brb[:], ALU.add), reads=["pb4", "brb"], writes=[k])
            P.op("dve", lambda e, gmax=gmax, gl=gl: e.reduce_max(out=gmax, in_=gl, axis=AX.X), reads=[k], writes=[k + "gm"])
            P.op("dve", lambda e, oh=oh, gl=gl, gmax=gmax: e.tensor_scalar(oh, gl, gmax, None, ALU.is_ge), reads=[k, k + "gm"], writes=[k + "oh"])
            P.op("dve", lambda e, ngmax=ngmax, gmax=gmax: e.tensor_scalar(ngmax, gmax, -1.0, None, ALU.mult), reads=[k + "gm"], writes=[k + "ngm"])
            P.op("act", lambda e, eg=eg, gl=gl, ngmax=ngmax, gsum=gsum: e.activation(out=eg, in_=gl, func=AF.Exp, bias=ngmax, scale=1.0, accum_out=gsum),
                 reads=[k, k + "ngm"], writes=[k + "eg", k + "gs"])
            P.op("dve", lambda e, gsum=gsum: e.reciprocal(gsum, gsum), reads=[k + "gs"], writes=[k + "gs"])
            P.op("dve", lambda e, pen=pen, oh=oh: e.tensor_scalar(pen, oh, 1.0, BIG, ALU.subtract, ALU.mult), reads=[k + "oh"], writes=[k + "pen"])
            P.op("dve", lambda e, msk=msk, el=el, pen=pen: e.tensor_tensor(msk.rearrange("p (g j) -> p g j", g=4), el.rearrange("p (g j) -> p g j", g=4),
                                                                          pen.unsqueeze(2).to_broadcast([128, 4, 8]), ALU.add),
                 reads=[k, k + "pen"], writes=[k + "msk"])
            P.op("dve", lambda e, top1=top1, msk=msk: e.reduce_max(out=top1, in_=msk, axis=AX.X), reads=[k + "msk"], writes=[k + "t1"])
            P.op("dve", lambda e, m1=m1, msk=msk, top1=top1: e.tensor_scalar(m1, msk, top1, None, ALU.is_ge), reads=[k + "msk", k + "t1"], writes=[k + "m1"])
            P.op("dve", lambda e, msk2=msk2, m1=m1, msk=msk: e.scalar_tensor_tensor(out=msk2, in0=m1, scalar=-BIG, in1=msk, op0=ALU.mult, op1=ALU.add),
                 reads=[k + "m1", k + "msk"], writes=[k + "msk2"])
            P.op("dve", lambda e, top2=top2, msk2=msk2: e.reduce_max(out=top2, in_=msk2, axis=AX.X), reads=[k + "msk2"], writes=[k + "t2"])
            P.op("dve", lambda e, m2=m2, msk2=msk2, top2=top2: e.tensor_scalar(m2, msk2, top2, None, ALU.is_ge), reads=[k + "msk2", k + "t2"], writes=[k + "m2"])
            P.op("dve", lambda e, dd=dd, top2=top2, top1=top1: e.tensor_tensor(dd, top2, top1, ALU.subtract), reads=[k + "t1", k + "t2"], writes=[k + "dd"])
            P.op("act", lambda e, ee=ee, dd=dd: e.activation(out=ee, in_=dd, func=AF.Exp), reads=[k + "dd"], writes=[k + "ee"])
            P.op("dve", lambda e, w1=w1, ee=ee: e.tensor_scalar(w1, ee, 1.0, None, ALU.add), reads=[k + "ee"], writes=[k + "w1"])
            P.op("dve", lambda e, w1=w1: e.reciprocal(w1, w1), reads=[k + "w1"], writes=[k + "w1"])
            P.op("dve", lambda e, w1=w1, gsum=gsum: e.tensor_scalar(w1, w1, gsum, 1.0 / ALPHA, ALU.mult, ALU.mult), reads=[k + "w1", k + "gs"], writes=[k + "w1"])
            P.op("dve", lambda e, w2=w2, ee=ee, w1=w1: e.tensor_tensor(w2, ee, w1, ALU.mult), reads=[k + "ee", k + "w1"], writes=[k + "w2"])
            P.op("dve", lambda e, tmp=tmp, m1=m1, w1=w1: e.tensor_scalar(tmp, m1, w1, None, ALU.mult), reads=[k + "m1", k + "w1"], writes=[k + "tmp"])
            P.op("dve", lambda e, i=i, m2=m2, w2=w2, tmp=tmp: e.scalar_tensor_tensor(out=G[:, i, :], in0=m2, scalar=w2, in1=tmp, op0=ALU.mult, op1=ALU.add),
                 reads=[k + "m2", k + "w2", k + "tmp"], writes=["G%d" % i])

        P.dma("sp", "c_lng", gb[0][:], _bcast_rows(lnp_h, 2, D), writes=["lng"])
        P.dma("sp", "c_lnb", gb[1][:], _bcast_rows(lnp_h, 3, D), writes=["lnb"])

        def load_expert(e_):
            s = e_ % 2
            wb = wbuf[s]
            P.dma("pool", "wg%d" % s, wb[:, 0:4096].rearrange("p (k n) -> p k n", k=8),
                  wg_h.ap()[e_].rearrange("(k p) n -> p k n", p=128), writes=["w%dg" % s])
            P.dma("pool", "wu%d" % s, wb[:, 4096:8192].rearrange("p (k n) -> p k n", k=8),
                  wu_h.ap()[e_].rearrange("(k p) n -> p k n", p=128), writes=["w%du" % s])
            P.dma("pool", "wd%d" % s, wb[:, 8192:12288].rearrange("p (k n) -> p k n", k=4),
                  wd_h.ap()[e_].rearrange("(k p) n -> p k n", p=128), writes=["w%dd" % s])

        load_expert(0)
        hcnt = 0
        for e_ in range(NE):
            s = e_ % 2
            wb = wbuf[s]
            wgv = wb[:, 0:4096].rearrange("p (k n) -> p k n", k=8)
            wuv = wb[:, 4096:8192].rearrange("p (k n) -> p k n", k=8)
            wdv = wb[:, 8192:12288].rearrange("p (k n) -> p k n", k=4)
            if e_ + 1 < NE:
                load_expert(e_ + 1)
            for tb in range(NB):
                hs = hcnt % 2; hcnt += 1
                hTs = hT[hs]
                tkeys = []
                for j in range(4):
                    tkeys += ["x1T_%da" % (tb * 4 + j), "x1T_%db" % (tb * 4 + j)]
                for c in range(4):
                    pg = pb[(c % 2) * 2]; pu = pb[(c % 2) * 2 + 1]
                    kg = "pb%d" % ((c % 2) * 2); ku = "pb%d" % ((c % 2) * 2 + 1)
                    for kc in range(8):
                        P.op("pe", (lambda e, pg=pg, kc=kc, c=c, wgv=wgv, tb=tb: e.matmul(pg[:], wgv[:, kc, c * 128:(c + 1) * 128],
                                                                                          x1T[:, kc, tb * 512:(tb + 1) * 512], start=(kc == 0), stop=(kc == 7))),
                             reads=["w%dg" % s] + tkeys, writes=[kg])
                    for kc in range(8):
                        P.op("pe", (lambda e, pu=pu, kc=kc, c=c, wuv=wuv, tb=tb: e.matmul(pu[:], wuv[:, kc, c * 128:(c + 1) * 128],
                                                                                          x1T[:, kc, tb * 512:(tb + 1) * 512], start=(kc == 0), stop=(kc == 7))),
                             reads=["w%du" % s] + tkeys, writes=[ku])
                    sgs = sg[c % 2]
                    P.op("act", lambda e, sgs=sgs, pg=pg: e.activation(out=sgs[:], in_=pg[:], func=AF.Silu), reads=[kg], writes=["sg%d" % (c % 2)])
                    P.op("dve", lambda e, hTs=hTs, c=c, sgs=sgs, pu=pu: e.tensor_tensor(hTs[:, c, :], sgs[:], pu[:], ALU.mult),
                         reads=["sg%d" % (c % 2), ku], writes=["hT%d_%d" % (hs, c)])
                for tt in range(4):
                    ti = tb * 4 + tt
                    for dh in range(2):
                        py = pb[4 + (tt * 2 + dh) % 4]; ky = "pb%d" % (4 + (tt * 2 + dh) % 4)
                        for c in range(4):
                            P.op("pe", (lambda e, py=py, c=c, tt=tt, dh=dh, hTs=hTs, wdv=wdv: e.matmul(py[:], hTs[:, c, tt * 128:(tt + 1) * 128],
                                                                                                 wdv[:, c, dh * 512:(dh + 1) * 512], start=(c == 0), stop=(c == 3))),
                                 reads=["hT%d_%d" % (hs, c) for c in range(4)] + ["w%dd" % s], writes=[ky])
                        P.op("dve", (lambda e, py=py, ti=ti, dh=dh, e_=e_: e.scalar_tensor_tensor(out=yacc[:, ti, dh * 512:(dh + 1) * 512], in0=py[:],
                                                                                                scalar=G[:, ti, e_:e_ + 1], in1=yacc[:, ti, dh * 512:(dh + 1) * 512],
                                                                                                op0=ALU.mult, op1=ALU.add)),
                             reads=[ky, "G%d" % ti, "yacc%d" % ti], writes=["yacc%d" % ti])

        for i in range(NT):
            s = i % 2
            xts = xt[s]
            layer_norm(lambda a, b, i=i: yacc[:, i, a:b], "yacc%d" % i, s, xts[:], "xt%d" % s, ALPHA)
            P.dma("sp", "out%d" % s, xo[i * 128:(i + 1) * 128, :], xts[:], reads=["xt%d" % s], writes=["xo%d" % i])
        P.emit()
    return nc


def _mk_consts(nc, P, sb):
    C = {}
    C["ones"] = sb("c_ones", [128, 128], F32)
    C["ident"] = sb("c_ident", [128, 128], F32)
    C["tri_le"] = sb("c_tri_le", [128, 128], F32)
    C["tri_ge"] = sb("c_tri_ge", [128, 128], F32)
    C["tri_gt"] = sb("c_tri_gt", [128, 128], F32)
    C["tri_lt"] = sb("c_tri_lt", [128, 128], F32)
    C["ones_bf"] = sb("c_ones_bf", [128, 128], BF16)
    ones = C["ones"]
    P.op("pool", lambda e: e.memset(ones[:], 1.0), writes=["c_ones"])
    P.op("pool", lambda e: e.memset(C["ones_bf"][:], 1.0), writes=["c_ones_bf"])

    def sel(name, step, cm, cmp):
        t = C[name]
        P.op("pool", lambda e: e.affine_select(out=t[:], in_=ones[:], pattern=[[step, 128]], compare_op=cmp,
                                               fill=0.0, base=0, channel_multiplier=cm),
             reads=["c_ones"], writes=["c_" + name])
    sel("ident", -1, 1, ALU.is_equal)
    sel("tri_le", 1, -1, ALU.is_ge)
    sel("tri_ge", -1, 1, ALU.is_ge)
    sel("tri_gt", -1, 1, ALU.is_gt)
    sel("tri_lt", 1, -1, ALU.is_gt)
    return C


def _scan_factors(P, C, ps, pskey, gf, gfkey, gb, gbkey, dk, F, fkey):
    P.op("pe", lambda e: e.matmul(ps[0:dk, 0:128], gf, C["tri_le"][:], start=True, stop=True),
         reads=[gfkey, "c_tri_le"], writes=[pskey + "a"])
    P.op("pe", lambda e: e.matmul(ps[0:dk, 128:256], gb, C["tri_ge"][:], start=True, stop=True),
         reads=[gbkey, "c_tri_ge"], writes=[pskey + "b"])
    P.op("pe", lambda e: e.matmul(ps[:, 256:256 + dk], C["tri_gt"][:], gf, start=True, stop=True),
         reads=[gfkey, "c_tri_gt"], writes=[pskey + "c"])
    P.op("pe", lambda e: e.matmul(ps[:, 384:384 + dk], C["tri_lt"][:], gb, start=True, stop=True),
         reads=[gbkey, "c_tri_lt"], writes=[pskey + "d"])
    P.op("act", lambda e: e.activation(out=F["E1f"], in_=ps[0:dk, 0:128], func=AF.Exp), reads=[pskey + "a"], writes=[fkey + "E1f"])
    P.op("act", lambda e: e.activation(out=F["E2f"], in_=ps[0:dk, 0:128], func=AF.Exp, scale=-1.0), reads=[pskey + "a"], writes=[fkey + "E2f"])
    P.op("act", lambda e: e.activation(out=F["E1b"], in_=ps[0:dk, 128:256], func=AF.Exp), reads=[pskey + "b"], writes=[fkey + "E1b"])
    P.op("act", lambda e: e.activation(out=F["E2b"], in_=ps[0:dk, 128:256], func=AF.Exp, scale=-1.0), reads=[pskey + "b"], writes=[fkey + "E2b"])
    P.op("act", lambda e: e.activation(out=F["E3f"], in_=ps[:, 256:256 + dk], func=AF.Exp), reads=[pskey + "c"], writes=[fkey + "E3f"])
    P.op("act", lambda e: e.activation(out=F["E3b"], in_=ps[:, 384:384 + dk], func=AF.Exp), reads=[pskey + "d"], writes=[fkey + "E3b"])


def build_mix0(T=8192, stage=99):
    nc = bass.Bass("TRN2", target_bir_lowering=False)
    NBK = T // 512
    NCH = T // 128
    D = 1024
    NW = 11 * 128
    xT_h = nc.dram_tensor("xT", [D, T], F32, kind="ExternalInput")
    wA_h = nc.dram_tensor("wA", [D, NW], F32, kind="ExternalInput")
    tabs_h = nc.dram_tensor("tabs", [4, 128, T], F32, kind="ExternalInput")
    dec_h = nc.dram_tensor("dec", [1, 2], F32, kind="ExternalInput")
    lv_h = nc.dram_tensor("lv", [4, 64], F32, kind="ExternalInput")
    subln_h = nc.dram_tensor("subln", [1, 128], F32, kind="ExternalInput")
    o_h = nc.dram_tensor("o", [T, 256], F32, kind="ExternalOutput")
    xT = xT_h.ap().rearrange("(k p) t -> p k t", p=128)
    tabs = tabs_h.ap().rearrange("f p t -> p f t")
    oo = o_h.ap()
    KSC = 128.0 ** -0.5
    LAM_INIT = 0.8 - 0.6 * math.exp(-0.3 * 0)
    SCL = 64.0 ** -0.5

    with contextlib.ExitStack() as st:
        def sb(name, shape, dt):
            return st.enter_context(nc.sbuf_tensor("s_" + name, shape, dt))
        P = Prog(nc)
        C = _mk_consts(nc, P, sb)
        wA = sb("wA", [128, 8, NW], BF16)
        xb = [sb("xb%d" % i, [128, 8, 512], BF16) for i in range(2)]
        tab = [sb("tab%d" % i, [128, 4, 512], F32) for i in range(2)]
        KT_r = sb("KT_r", [128, T], BF16)
        Ktok_r = sb("Ktok_r", [128, NCH, 128], BF16)
        V_r = sb("V_r", [128, NCH, 128], BF16)
        Rst = sb("Rst", [128, NCH, 128], BF16)
        KT_d = sb("KT_d", [128, T], BF16)
        V_d = sb("V_d", [128, NCH, 130], BF16)
        fac = sb("fac", [128, 6, 128], F32)
        Gc = sb("Gc", [128, 2, 128], F32)
        sc = sb("sc", [128, 64], F32)
        lvb = sb("lvb", [128, 4, 64], F32)
        sublnb = sb("sublnb", [128, 128], F32)
        Rf = sb("Rf", [128, 128], F32)
        Sf = sb("Sf", [128, 128], F32)
        S_bf = sb("S_bf", [128, 128], BF16)
        t1 = [sb("t1_%d" % i, [128, 512], F32) for i in range(2)]
        t2 = [sb("t2_%d" % i, [128, 512], F32) for i in range(2)]
        ktf = sb("ktf", [128, 512], F32)
        sq = sb("sq", [128, 512], BF16)
        QTd = [sb("QTd%d" % i, [128, 512], BF16) for i in range(2)]
        gate = [sb("gate%d" % i, [128, 4, 128], F32) for i in range(2)]
        outt = [sb("outt%d" % i, [128, 4, 256], F32) for i in range(2)]
        cw = [sb("cw%d" % i, [128, 6, 128], BF16) for i in range(2)]
        pm = [sb("pm%d" % i, [128, 2, 128], BF16) for i in range(2)]
        pT = [sb("pT%d" % i, [128, 512], BF16) for i in range(4)]
        od = [sb("od%d" % i, [128, 128], F32) for i in range(2)]
        smx = [sb("smx%d" % i, [128, 32], F32) for i in range(2)]
        pb = [st.enter_context(nc.psum_tensor("pb%d" % i, [128, 512], F32)) for i in range(8)]

        if stage == -3:
            P.emit(); return nc
        P.dma("pool", "wA", wA[:], wA_h.ap().rearrange("(k p) n -> p k n", p=128), writes=["wA"])
        P.dma("sp", "c_dec", sc[:, 0:2], _bcast_rows(dec_h, 0, 2), writes=["dec"])
        P.dma("sp", "c_lv", lvb[:], bass.AP(lv_h, 0, [[0, 128], [1, 256]]), writes=["lvb"])
        P.dma("sp", "c_subln", sublnb[:], _bcast_rows(subln_h, 0, 128), writes=["sublnb"])
        if stage == -2:
            P.emit(); return nc
        P.op("act", lambda e: e.activation(out=sc[:, 2:4], in_=sc[:, 0:2], func=AF.Exp), reads=["dec"], writes=["la"])
        P.op("dve", lambda e: e.tensor_scalar(sc[:, 2:4], sc[:, 2:4], -1.0, None, ALU.mult), reads=["la"], writes=["la"])
        for d_ in range(2):
            P.op("dve", lambda e, d_=d_: e.tensor_scalar(Gc[:, d_, :], C["ones"][:], sc[:, 2 + d_:3 + d_], None, ALU.mult),
                 reads=["la", "c_ones"], writes=["Gc%d" % d_])
        F = {"E1f": fac[:, 0, :], "E2f": fac[:, 1, :], "E1b": fac[:, 2, :], "E2b": fac[:, 3, :], "E3f": fac[:, 4, :], "E3b": fac[:, 5, :]}
        _scan_factors(P, C, pb[7], "pb7", Gc[:, 0, :], "Gc0", Gc[:, 1, :], "Gc1", 128, F, "fac")
        FK = ["fac" + k for k in ("E1f", "E2f", "E1b", "E2b", "E3f", "E3b")]
        dSf = fac[:, 0, 127:128]
        dSb = fac[:, 2, 0:1]
        if stage == -1:
            P.emit(); return nc
        P.op("dve", lambda e: e.tensor_tensor(lvb[:, 0, :], lvb[:, 0, :], lvb[:, 1, :], ALU.mult), reads=["lvb"], writes=["lvb"])
        P.op("dve", lambda e: e.tensor_tensor(lvb[:, 2, :], lvb[:, 2, :], lvb[:, 3, :], ALU.mult), reads=["lvb"], writes=["lvb"])
        P.op("dve", lambda e: e.reduce_sum(out=sc[:, 4:5], in_=lvb[:, 0, :], axis=AX.X), reads=["lvb"], writes=["lam_a"])
        P.op("dve", lambda e: e.reduce_sum(out=sc[:, 5:6], in_=lvb[:, 2, :], axis=AX.X), reads=["lvb"], writes=["lam_b"])
        P.op("act", lambda e: e.activation(out=sc[:, 6:8], in_=sc[:, 4:6], func=AF.Exp), reads=["lam_a", "lam_b"], writes=["lam_e"])
        P.op("dve", lambda e: e.tensor_tensor(sc[:, 8:9], sc[:, 6:7], sc[:, 7:8], ALU.subtract), reads=["lam_e"], writes=["lam"])
        P.op("dve", lambda e: e.tensor_scalar(sc[:, 9:10], sc[:, 8:9], LAM_INIT, -1.0, ALU.add, ALU.mult), reads=["lam"], writes=["nlam"])
        P.op("dve", lambda e: e.tensor_scalar(sublnb[:], sublnb[:], 1.0 - LAM_INIT, None, ALU.mult), reads=["sublnb"], writes=["sublnb"])
        P.op("pool", lambda e: e.memset(Rf[:], 0.0), writes=["Rf"])
        P.op("pool", lambda e: e.memset(Sf[:], 0.0), writes=["Sf"])
        P.op("pool", lambda e: e.memset(S_bf[:], 0.0), writes=["S_bf"])
        P.op("pool", lambda e: e.memset(sc[:, 10:11], 0.0), writes=["kmax2"])
        P.op("pool", lambda e: e.memset(V_d[:, :, 128:130], 1.0), writes=["V_d_ones"])

        def load_block(tb, s):
            P.dma("pool", "xb%d" % s, xb[s][:], xT[:, :, tb * 512:(tb + 1) * 512], writes=["xb%d" % s])
            P.dma("sp", "tab%d" % s, tab[s][:], tabs[:, :, tb * 512:(tb + 1) * 512], writes=["tab%d" % s])

        def proj_fm(blk, bank, s):
            for kc in range(8):
                P.op("pe", (lambda e, kc=kc: e.matmul(pb[bank][:], wA[:, kc, blk * 128:(blk + 1) * 128], xb[s][:, kc, :],
                                                      start=(kc == 0), stop=(kc == 7))),
                     reads=["wA", "xb%d" % s], writes=["pb%d" % bank])

        def rotary(bx, bp, s, fc, fs, dst, dstkey, slot):
            a = t1[slot]; b = t2[slot]
            P.op("dve", lambda e: e.tensor_tensor(a[:], pb[bp][:], tab[s][:, fs, :], ALU.mult), reads=["pb%d" % bp, "tab%d" % s], writes=["t1_%d" % slot])
            P.op("dve", lambda e: e.tensor_tensor(b[:], pb[bx][:], tab[s][:, fc, :], ALU.mult), reads=["pb%d" % bx, "tab%d" % s], writes=["t2_%d" % slot])
            P.op("pool", lambda e: e.tensor_tensor(dst, a[:], b[:], ALU.add), reads=["t1_%d" % slot, "t2_%d" % slot], writes=[dstkey])

        def sumsq_max(src, srckey, bank, dst, dstkey):
            P.op("pool", lambda e: e.tensor_tensor(sq[:], src, src, ALU.mult), reads=[srckey], writes=["sq"])
            P.op("pe", lambda e: e.matmul(pb[bank][:], C["ones_bf"][:], sq[:], start=True, stop=True), reads=["sq", "c_ones_bf"], writes=["pb%d" % bank])
            P.op("dve", lambda e: e.reduce_max(out=dst, in_=pb[bank][:], axis=AX.X), reads=["pb%d" % bank], writes=[dstkey])

        def mm(out, lhsT, rhs, start, stop, reads, writes, skip=False):
            P.op("pe", lambda e: e.matmul(out, lhsT, rhs, start=start, stop=stop, skip_group_check=skip), reads=reads, writes=writes)

        def p1_values(tb, s, tt):
            bank = 4 + tt // 2
            o0 = (tt % 2) * 256
            key = "pb%d_%d" % (bank, tt % 2)
            for kc in range(8):
                mm(pb[bank][:, o0:o0 + 256], xb[s][:, kc, tt * 128:(tt + 1) * 128], wA[:, kc, 8 * 128:10 * 128], kc == 0, kc == 7,
                   ["wA", "xb%d" % s], [key])
            ci = tb * 4 + tt
            P.op("act", lambda e: e.copy(out=V_r[:, ci, :], in_=pb[bank][:, o0:o0 + 128]), reads=[key], writes=["V_r%d" % ci])
            P.op("dve", lambda e: e.tensor_copy(out=V_d[:, ci, 0:128], in_=pb[bank][:, o0 + 128:o0 + 256]), reads=[key], writes=["V_d%d" % ci])

        def p1_chunk(ci):
            cs = ci % 2
            P.op("act", lambda e: e.copy(out=Rst[:, ci, :], in_=Rf[:]), reads=["Rf"], writes=["Rst%d" % ci])
            P.op("pool", lambda e: e.tensor_tensor(cw[cs][:, 4, :], Ktok_r[:, ci, :], F["E3b"], ALU.mult),
                 reads=["Ktok_r%d" % (ci // 4), "facE3b"], writes=["cw%d_4" % cs])
            mm(pb[7][:, 0:128], cw[cs][:, 4, :], V_r[:, ci, :], True, True, ["cw%d_4" % cs, "V_r%d" % ci], ["pb7u"])
            P.op("dve", lambda e: e.scalar_tensor_tensor(out=Rf[:], in0=Rf[:], scalar=dSb, in1=pb[7][:, 0:128], op0=ALU.mult, op1=ALU.add),
                 reads=["Rf", "pb7u", "facE1b"], writes=["Rf"])

        def p1_block(n_, tb):
            s = n_ % 2
            bsl = slice(tb * 512, (tb + 1) * 512)
            load_block(tb, s)
            proj_fm(2, 0, s); proj_fm(3, 1, s); proj_fm(6, 2, s); proj_fm(7, 3, s)
            rotary(0, 1, s, 0, 1, ktf[:], "ktf", 0)
            P.op("act", lambda e: e.copy(out=KT_r[:, bsl], in_=ktf[:]), reads=["ktf"], writes=["KT_r%d" % tb])
            for j in range(4):
                P.op("pe", lambda e, j=j: e.transpose(pb[6][:, j * 128:(j + 1) * 128], ktf[:, j * 128:(j + 1) * 128], C["ident"][:]),
                     reads=["ktf", "c_ident"], writes=["pb6"])
            P.op("act", lambda e: e.copy(out=Ktok_r[:, tb * 4:(tb + 1) * 4, :], in_=pb[6][:].rearrange("p (j d) -> p j d", j=4)),
                 reads=["pb6"], writes=["Ktok_r%d" % tb])
            rotary(2, 3, s, 2, 3, KT_d[:, bsl], "KT_d%d" % tb, 1)
            sumsq_max(KT_d[:, bsl], "KT_d%d" % tb, 2, sc[:, 11:12], "ktmp")
            P.op("dve", lambda e: e.tensor_tensor(sc[:, 10:11], sc[:, 10:11], sc[:, 11:12], ALU.max), reads=["kmax2", "ktmp"], writes=["kmax2"])
            for tt in range(4):
                p1_values(tb, s, tt)
            for ci in range(tb * 4 + 3, tb * 4 - 1, -1):
                p1_chunk(ci)

        for n_, tb in enumerate(range(NBK - 1, -1, -1) if stage >= 1 else []):
            p1_block(n_, tb)

        def p2_gate_tt(s, tt):
            for kc in range(8):
                mm(pb[3][:, tt * 128:(tt + 1) * 128], xb[s][:, kc, tt * 128:(tt + 1) * 128], wA[:, kc, 10 * 128:11 * 128], kc == 0, kc == 7,
                   ["wA", "xb%d" % s], ["pb3"])

        def p2_ret_chunk(tb, s, j):
            ci = tb * 4 + j
            cs = ci % 2
            csl = slice(j * 128, (j + 1) * 128)
            gsl = slice(ci * 128, (ci + 1) * 128)
            cwk = "cw%d_" % cs
            c = cw[cs]
            P.op("dve", lambda e: e.scalar_tensor_tensor(out=c[:, 0, :], in0=ktf[:, csl], scalar=KSC, in1=F["E1f"], op0=ALU.mult, op1=ALU.mult),
                 reads=["ktf", "facE1f"], writes=[cwk + "0"])
            P.op("dve", lambda e: e.scalar_tensor_tensor(out=c[:, 1, :], in0=ktf[:, csl], scalar=KSC, in1=F["E1b"], op0=ALU.mult, op1=ALU.mult),
                 reads=["ktf", "facE1b"], writes=[cwk + "1"])
            P.op("dve", lambda e: e.tensor_tensor(c[:, 2, :], KT_r[:, gsl], F["E2f"], ALU.mult), reads=["KT_r%d" % tb, "facE2f"], writes=[cwk + "2"])
            P.op("pool", lambda e: e.tensor_tensor(c[:, 3, :], KT_r[:, gsl], F["E2b"], ALU.mult), reads=["KT_r%d" % tb, "facE2b"], writes=[cwk + "3"])
            mm(pb[2][:, 0:128], c[:, 2, :], c[:, 0, :], True, True, [cwk + "2", cwk + "0"], ["pb2a"])
            mm(pb[2][:, 128:256], c[:, 3, :], c[:, 1, :], True, True, [cwk + "3", cwk + "1"], ["pb2b"])
            pmc = pm[cs]
            P.op("dve", lambda e: e.tensor_tensor(pmc[:, 0, :], pb[2][:, 0:128], C["tri_le"][:], ALU.mult), reads=["pb2a", "c_tri_le"], writes=["pm%d_0" % cs])
            P.op("dve", lambda e: e.tensor_tensor(pmc[:, 1, :], pb[2][:, 128:256], C["tri_gt"][:], ALU.mult), reads=["pb2b", "c_tri_gt"], writes=["pm%d_1" % cs])
            oreg = pb[2][:, 256:384]
            mm(oreg, pmc[:, 0, :], V_r[:, ci, :], True, False, ["pm%d_0" % cs, "V_r%d" % ci], ["pb2o"])
            mm(oreg, pmc[:, 1, :], V_r[:, ci, :], False, False, ["pm%d_1" % cs, "V_r%d" % ci], ["pb2o"])
            mm(oreg, c[:, 0, :], S_bf[:], False, False, [cwk + "0", "S_bf"], ["pb2o"])
            mm(oreg, c[:, 1, :], Rst[:, ci, :], False, True, [cwk + "1", "Rst%d" % ci], ["pb2o"])
            P.op("pool", lambda e: e.tensor_tensor(c[:, 4, :], Ktok_r[:, ci, :], F["E3f"], ALU.mult), reads=["Ktok_r%d" % tb, "facE3f"], writes=[cwk + "4"])
            mm(pb[2][:, 384:512], c[:, 4, :], V_r[:, ci, :], True, True, [cwk + "4", "V_r%d" % ci], ["pb2u"])
            P.op("dve", lambda e: e.scalar_tensor_tensor(out=Sf[:], in0=Sf[:], scalar=dSf, in1=pb[2][:, 384:512], op0=ALU.mult, op1=ALU.add),
                 reads=["Sf", "pb2u", "facE1f"], writes=["Sf"])
            P.op("act", lambda e: e.copy(out=S_bf[:], in_=Sf[:]), reads=["Sf"], writes=["S_bf"])
            sx = smx[s]; xk = "gn%d" % s
            odc = od[cs]
            P.op("dve", lambda e: e.bn_stats(out=sx[:, 8:14], in_=oreg), reads=["pb2o"], writes=[xk + "st"])
            P.op("dve", lambda e: e.bn_aggr(out=sx[:, 14:16], in_=sx[:, 8:14]), reads=[xk + "st"], writes=[xk + "mv"])
            P.op("act", lambda e: e.activation(out=sx[:, 16:17], in_=sx[:, 15:16], func=AF.Sqrt, bias=LN_EPS, scale=1.0), reads=[xk + "mv"], writes=[xk + "rs"])
            P.op("dve", lambda e: e.reciprocal(sx[:, 16:17], sx[:, 16:17]), reads=[xk + "rs"], writes=[xk + "rs"])
            P.op("dve", lambda e: e.tensor_scalar(odc[:], oreg, sx[:, 14:15], sx[:, 16:17], ALU.subtract, ALU.mult),
                 reads=["pb2o", xk + "mv", xk + "rs"], writes=["od%d" % cs])
            P.op("pool", lambda e: e.tensor_tensor(outt[s][:, j, 0:128], odc[:], gate[s][:, j, :], ALU.mult),
                 reads=["od%d" % cs, "gate%d" % s], writes=["outt%d_r%d" % (s, j)])

        ptc = [0]

        def p2_attn_step(s, comp, kt):
            rows = slice(comp * 64, comp * 64 + 64)
            sbank = kt % 2
            psl = ptc[0] % 4; ptc[0] += 1
            negc = smx[s][:, 3:4]
            mm(pb[sbank][:], KT_d[rows, kt * 128:(kt + 1) * 128], QTd[s][rows, :], True, True, ["KT_d%d" % (kt // 4), "QTd%d" % s], ["pb%d" % sbank])
            P.op("act", lambda e: e.activation(out=pT[psl][:], in_=pb[sbank][:], func=AF.Exp, bias=negc, scale=SCL),
                 reads=["pb%d" % sbank, "negc%d" % s], writes=["pT%d" % psl])
            for qt in range(4):
                bank = 4 + comp * 2 + qt // 2
                areg = pb[bank][:, (qt % 2) * 256:(qt % 2) * 256 + 129]
                mm(areg, pT[psl][:, qt * 128:(qt + 1) * 128], V_d[:, kt, 0:129], kt == 0 and qt % 2 == 0, kt == NCH - 1,
                   ["pT%d" % psl, "V_d%d" % kt, "V_d_ones"], ["pb%d_acc%d" % (bank, qt)], skip=True)

        def p2_attn_epi(s, qt):
            a0 = pb[4 + qt // 2][:, (qt % 2) * 256:(qt % 2) * 256 + 129]
            a1 = pb[6 + qt // 2][:, (qt % 2) * 256:(qt % 2) * 256 + 129]
            k0 = "pb%d_acc%d" % (4 + qt // 2, qt); k1 = "pb%d_acc%d" % (6 + qt // 2, qt)
            sx = smx[s]; xk = "da%d" % s; cs = qt % 2
            odc = od[cs]
            P.op("dve", lambda e: e.reciprocal(sx[:, 20:21], a0[:, 128:129]), reads=[k0], writes=[xk + "z0"])
            P.op("dve", lambda e: e.reciprocal(sx[:, 21:22], a1[:, 128:129]), reads=[k1], writes=[xk + "z1"])
            P.op("dve", lambda e: e.tensor_tensor(sx[:, 21:22], sx[:, 21:22], sc[:, 9:10], ALU.mult), reads=[xk + "z1", "nlam"], writes=[xk + "z1"])
            P.op("dve", lambda e: e.tensor_scalar(odc[:], a0[:, 0:128], sx[:, 20:21], None, ALU.mult), reads=[k0, xk + "z0"], writes=["od%d" % cs])
            P.op("dve", lambda e: e.scalar_tensor_tensor(out=odc[:], in0=a1[:, 0:128], scalar=sx[:, 21:22], in1=odc[:], op0=ALU.mult, op1=ALU.add),
                 reads=[k1, xk + "z1", "od%d" % cs], writes=["od%d" % cs])
            P.op("act", lambda e: e.activation(out=t1[0][:, 0:128], in_=odc[:], func=AF.Square, accum_out=sx[:, 22:23]),
                 reads=["od%d" % cs], writes=["t1_0", xk + "ss"])
            P.op("act", lambda e: e.activation(out=sx[:, 23:24], in_=sx[:, 22:23], func=AF.Sqrt, bias=1e-6, scale=1.0 / 128.0), reads=[xk + "ss"], writes=[xk + "rs"])
            P.op("dve", lambda e: e.reciprocal(sx[:, 23:24], sx[:, 23:24]), reads=[xk + "rs"], writes=[xk + "rs"])
            P.op("dve", lambda e: e.scalar_tensor_tensor(out=outt[s][:, qt, 128:256], in0=odc[:], scalar=sx[:, 23:24], in1=sublnb[:], op0=ALU.mult, op1=ALU.mult),
                 reads=["od%d" % cs, xk + "rs", "sublnb"], writes=["outt%d_d%d" % (s, qt)])

        def p2_block(tb):
            s = tb % 2
            sx = smx[s]
            load_block(tb, s)
            proj_fm(0, 0, s); proj_fm(1, 1, s); proj_fm(4, 2, s); proj_fm(5, 3, s)
            rotary(0, 1, s, 0, 1, ktf[:], "ktf", 0)
            rotary(2, 3, s, 2, 3, QTd[s][:], "QTd%d" % s, 1)
            sumsq_max(QTd[s][:], "QTd%d" % s, 1, sx[:, 0:1], "qmax%d" % s)
            P.op("dve", lambda e: e.tensor_tensor(sx[:, 1:2], sx[:, 0:1], sc[:, 10:11], ALU.mult), reads=["qmax%d" % s, "kmax2"], writes=["c2_%d" % s])
            P.op("act", lambda e: e.activation(out=sx[:, 2:3], in_=sx[:, 1:2], func=AF.Sqrt, scale=(1.01 * SCL) ** 2), reads=["c2_%d" % s], writes=["c_%d" % s])
            P.op("dve", lambda e: e.tensor_scalar(sx[:, 3:4], sx[:, 2:3], -1.0, None, ALU.mult), reads=["c_%d" % s], writes=["negc%d" % s])
            for tt in range(4):
                p2_gate_tt(s, tt)
            P.op("act", lambda e: e.activation(out=gate[s][:], in_=pb[3][:].rearrange("p (j d) -> p j d", j=4), func=AF.Silu), reads=["pb3"], writes=["gate%d" % s])
            for j in range(4):
                p2_ret_chunk(tb, s, j)
            if stage >= 3:
                for comp in range(2):
                    for kt in range(NCH):
                        p2_attn_step(s, comp, kt)
                for qt in range(4):
                    p2_attn_epi(s, qt)
            P.dma("sp", "out%d" % s, oo[tb * 512:(tb + 1) * 512, :].rearrange("(j p) c -> p j c", p=128), outt[s][:],
                  reads=["outt%d_r%d" % (s, j) for j in range(4)] + (["outt%d_d%d" % (s, j) for j in range(4)] if stage >= 3 else []), writes=["o%d" % tb])

        for tb in (range(NBK) if stage >= 2 else []):
            p2_block(tb)
        P.emit()
    return nc


def build_mix1(T=8192, stage=99):
    nc = bass.Bass("TRN2", target_bir_lowering=False)
    NBK = T // 512
    NCH = T // 128
    D = 1024
    NW = 8 * 128 + 320
    TOK0 = 8 * 128
    xT_h = nc.dram_tensor("xT", [D, T], F32, kind="ExternalInput")
    wC_h = nc.dram_tensor("wC", [D, NW], F32, kind="ExternalInput")
    wuq_h = nc.dram_tensor("wuq", [256, 192], F32, kind="ExternalInput")
    wukv_h = nc.dram_tensor("wukv", [128, 192], F32, kind="ExternalInput")
    nrm_h = nc.dram_tensor("nrm", [128, 3], F32, kind="ExternalInput")
    w2_h = nc.dram_tensor("w2", [48, 64], F32, kind="ExternalInput")
    gbias_h = nc.dram_tensor("gbias", [1, 128], F32, kind="ExternalInput")
    gnorm_h = nc.dram_tensor("gnorm", [1, 128], F32, kind="ExternalInput")
    tabs_h = nc.dram_tensor("tabs", [2, 128, T], F32, kind="ExternalInput")
    o_h = nc.dram_tensor("o", [T, 256], F32, kind="ExternalOutput")
    xT = xT_h.ap().rearrange("(k p) t -> p k t", p=128)
    tabs = tabs_h.ap().rearrange("f p t -> p f t")
    oo = o_h.ap()
    SCL = 96.0 ** -0.5
    QSC = 64.0 ** -0.5

    with contextlib.ExitStack() as st:
        def sb(name, shape, dt):
            return st.enter_context(nc.sbuf_tensor("s_" + name, shape, dt))
        P = Prog(nc)
        C = _mk_consts(nc, P, sb)
        wC = sb("wC", [128, 8, NW], BF16)
        wuq = sb("wuq", [128, 2, 192], BF16)
        wukv = sb("wukv", [128, 192], BF16)
        nrm = sb("nrm", [128, 3], F32)
        w2 = sb("w2", [48, 64], BF16)
        gbb = sb("gbb", [128, 2, 64], F32)
        gnb = sb("gnb", [128, 128], F32)
        xb = [sb("xb%d" % i, [128, 8, 512], BF16) for i in range(2)]
        tab = [sb("tab%d" % i, [128, 2, 512], F32) for i in range(2)]
        KT_m = sb("KT_m", [128, T], BF16)
        V_m = sb("V_m", [128, NCH, 130], BF16)
        KT_g = sb("KT_g", [128, T], BF16)
        Ktok_g = sb("Ktok_g", [128, NCH, 64], BF16)
        V_g = sb("V_g", [128, NCH, 128], BF16)
        Rst = sb("Rst", [128, NCH, 128], BF16)
        fac = sb("fac", [128, 6, 128], F32)
        sc = sb("sc", [128, 64], F32)
        Rf = sb("Rf", [128, 128], F32)
        Sf = sb("Sf", [128, 128], F32)
        S_bf = sb("S_bf", [128, 128], BF16)
        t1 = sb("t1", [128, 512], F32)
        t2 = sb("t2", [128, 512], F32)
        sqf = sb("sqf", [128, 2, 512], F32)
        rsb = sb("rsb", [128, 512], F32)
        cg = sb("cg", [128, 2, 512], BF16)
        sq = sb("sq", [128, 512], BF16)
        qtf = sb("qtf", [128, 512], F32)
        lrT = sb("lrT", [128, 512], BF16)
        Gblk = sb("Gblk", [128, 2, 4, 64], F32)
        zt = sb("zt", [128, 512], F32)
        QTm = [sb("QTm%d" % i, [128, 512], BF16) for i in range(2)]
        gate = [sb("gate%d" % i, [128, 4, 128], F32) for i in range(2)]
        outt = [sb("outt%d" % i, [128, 4, 256], F32) for i in range(2)]
        cw = [sb("cw%d" % i, [128, 6, 128], BF16) for i in range(2)]
        pm = [sb("pm%d" % i, [128, 2, 128], BF16) for i in range(2)]
        pT = [sb("pT%d" % i, [128, 512], BF16) for i in range(4)]
        od = [sb("od%d" % i, [128, 128], F32) for i in range(2)]
        smx = [sb("smx%d" % i, [128, 32], F32) for i in range(2)]
        pb = [st.enter_context(nc.psum_tensor("pb%d" % i, [128, 512], F32)) for i in range(8)]

        P.dma("pool", "wC", wC[:], wC_h.ap().rearrange("(k p) n -> p k n", p=128), writes=["wC"])
        P.dma("pool", "wuq", wuq[:], wuq_h.ap().rearrange("(k p) n -> p k n", p=128), writes=["wuq"])
        P.dma("pool", "wukv", wukv[:], wukv_h.ap(), writes=["wukv"])
        P.dma("pool", "w2", w2[:], w2_h.ap(), writes=["w2"])
        P.dma("sp", "c_nrm", nrm[:], nrm_h.ap(), writes=["nrm"])
        P.dma("sp", "c_gbb", gbb[:], bass.AP(gbias_h, 0, [[0, 128], [1, 128]]), writes=["gbb"])
        P.dma("sp", "c_gnb", gnb[:], _bcast_rows(gnorm_h, 0, 128), writes=["gnb"])
        P.op("pool", lambda e: e.memset(Rf[:], 0.0), writes=["Rf"])
        P.op("pool", lambda e: e.memset(Sf[:], 0.0), writes=["Sf"])
        P.op("pool", lambda e: e.memset(S_bf[:], 0.0), writes=["S_bf"])
        P.op("pool", lambda e: e.memset(sc[:, 10:11], 0.0), writes=["kmax2"])
        P.op("pool", lambda e: e.memset(V_m[:, :, 128:130], 1.0), writes=["V_m_ones"])
        F = {"E1f": fac[0:64, 0, :], "E2f": fac[0:64, 1, :], "E1b": fac[0:64, 2, :], "E2b": fac[0:64, 3, :], "E3f": fac[:, 4, 0:64], "E3b": fac[:, 5, 0:64]}
        dSf = fac[0:64, 0, 127:128]
        dSb = fac[0:64, 2, 0:1]

        def mm(out, lhsT, rhs, start, stop, reads, writes, skip=False):
            P.op("pe", lambda e: e.matmul(out, lhsT, rhs, start=start, stop=stop, skip_group_check=skip), reads=reads, writes=writes)

        def load_block(tb, s):
            P.dma("pool", "xb%d" % s, xb[s][:], xT[:, :, tb * 512:(tb + 1) * 512], writes=["xb%d" % s])
            P.dma("sp", "tab%d" % s, tab[s][:], tabs[:, :, tb * 512:(tb + 1) * 512], writes=["tab%d" % s])

        def proj_fm(blk, bank, s):
            for kc in range(8):
                mm(pb[bank][:], wC[:, kc, blk * 128:(blk + 1) * 128], xb[s][:, kc, :], kc == 0, kc == 7, ["wC", "xb%d" % s], ["pb%d" % bank])

        def rms_bcast(src_banks, nchunk, bank, ndim, dst, dstkey):
            for c in range(nchunk):
                P.op("act", lambda e, c=c: e.activation(out=sqf[:, c, :], in_=pb[src_banks[c]][:], func=AF.Square),
                     reads=["pb%d" % src_banks[c]], writes=["sqf%d" % c])
            for c in range(nchunk):
                mm(pb[bank][:], C["ones"][:], sqf[:, c, :], c == 0, c == nchunk - 1, ["c_ones", "sqf%d" % c], ["pb%d" % bank])
            P.op("act", lambda e: e.activation(out=dst, in_=pb[bank][:], func=AF.Sqrt, bias=1e-6, scale=1.0 / ndim), reads=["pb%d" % bank], writes=[dstkey])
            P.op("dve", lambda e: e.reciprocal(dst, dst), reads=[dstkey], writes=[dstkey])

        def sumsq_max(src, srckey, nrows, bank, dst, dstkey):
            P.op("pool", lambda e: e.tensor_tensor(sq[0:nrows, :], src, src, ALU.mult), reads=list(srckey), writes=["sq"])
            mm(pb[bank][:], C["ones_bf"][0:nrows, :], sq[0:nrows, :], True, True, ["sq", "c_ones_bf"], ["pb%d" % bank])
            P.op("dve", lambda e: e.reduce_max(out=dst, in_=pb[bank][:], axis=AX.X), reads=["pb%d" % bank], writes=[dstkey])

        def rope_rows(bx, bp, s, dst, dstkey, extra=None, extrakey=None):
            r = slice(64, 96)
            P.op("dve", lambda e: e.tensor_tensor(t1[r, :], pb[bp][r, :], tab[s][r, 1, :], ALU.mult), reads=["pb%d" % bp, "tab%d" % s], writes=["t1r"])
            P.op("dve", lambda e: e.tensor_tensor(t2[r, :], pb[bx][r, :], tab[s][r, 0, :], ALU.mult), reads=["pb%d" % bx, "tab%d" % s], writes=["t2r"])
            if extra is None:
                P.op("pool", lambda e: e.tensor_tensor(dst, t1[r, :], t2[r, :], ALU.add), reads=["t1r", "t2r"], writes=[dstkey])
            else:
                P.op("pool", lambda e: e.tensor_tensor(t1[r, :], t1[r, :], t2[r, :], ALU.add), reads=["t1r", "t2r"], writes=["t1r"])
                P.op("pool", lambda e: e.tensor_tensor(dst, t1[r, :], extra, ALU.mult), reads=["t1r", extrakey], writes=[dstkey])

        def gates(s, d_, tt):
            r = slice(0, 16) if d_ == 0 else slice(32, 48)
            reg = pb[6][:, (d_ * 4 + tt) * 64:(d_ * 4 + tt + 1) * 64]
            key = "pb6g%d_%d" % (d_, tt)
            mm(reg, lrT[r, tt * 128:(tt + 1) * 128], w2[r, :], True, True, ["lrT", "w2"], [key])
            z = zt[:, (d_ * 4 + tt) * 64:(d_ * 4 + tt + 1) * 64]
            zk = "zt%d_%d" % (d_, tt)
            P.op("dve", lambda e: e.tensor_tensor(z, reg, gbb[:, d_, :], ALU.add), reads=[key, "gbb"], writes=[zk])
            P.op("act", lambda e: e.activation(out=z, in_=z, func=AF.Exp, scale=-1.0), reads=[zk], writes=[zk])
            P.op("act", lambda e: e.activation(out=z, in_=z, func=AF.Ln, bias=1.0, scale=1.0), reads=[zk], writes=[zk])
            P.op("dve", lambda e: e.tensor_scalar(Gblk[:, d_, tt, :], z, -1.0 / 16.0, None, ALU.mult), reads=[zk], writes=["G%d_%d" % (d_, tt)])

        def p1_tt(tb, s, tt):
            ci = tb * 4 + tt
            tsl = slice(tt * 128, (tt + 1) * 128)
            mm(pb[5][:, tsl], cg[:, 0, tsl], wukv[:, 64:192], True, True, ["cg0", "wukv"], ["pb5v%d" % tt])
            mm(pb[2][:, 256 + tt:257 + tt], sqf[:, 0, tsl], C["ones"][:, 0:1], True, True, ["sqf0", "c_ones"], ["pb2c%d" % tt])
            sx = smx[s]
            P.op("act", lambda e: e.activation(out=sx[:, 24 + tt:25 + tt], in_=pb[2][:, 256 + tt:257 + tt], func=AF.Sqrt, bias=1e-6, scale=1.0 / 128.0),
                 reads=["pb2c%d" % tt], writes=["rc%d_%d" % (s, tt)])
            P.op("dve", lambda e: e.reciprocal(sx[:, 24 + tt:25 + tt], sx[:, 24 + tt:25 + tt]), reads=["rc%d_%d" % (s, tt)], writes=["rc%d_%d" % (s, tt)])
            P.op("dve", lambda e: e.tensor_scalar(V_m[:, ci, 0:128], pb[5][:, tsl], sx[:, 24 + tt:25 + tt], None, ALU.mult),
                 reads=["pb5v%d" % tt, "rc%d_%d" % (s, tt)], writes=["V_m%d" % ci])
            bank = tt % 2
            key = "pb%dtok" % bank
            for kc in range(8):
                mm(pb[bank][:, 0:320], xb[s][:, kc, tsl], wC[:, kc, TOK0:TOK0 + 320], kc == 0, kc == 7, ["wC", "xb%d" % s], [key])
            P.op("act", lambda e: e.copy(out=V_g[:, ci, :], in_=pb[bank][:, 0:128]), reads=[key], writes=["V_g%d" % ci])
            P.op("dve", lambda e: e.tensor_copy(out=Ktok_g[:, ci, :], in_=pb[bank][:, 256:320]), reads=[key], writes=["Ktok_g%d" % ci])
            gates(s, 1, tt)

        def p1_chunk(tb, tt):
            ci = tb * 4 + tt
            cs = ci % 2
            g = Gblk[:, 1, tt, :]
            _scan_factors(P, C, pb[7], "pb7", g, "G1_%d" % tt, g, "G1_%d" % tt, 64, F, "fac")
            P.op("act", lambda e: e.copy(out=Rst[0:64, ci, :], in_=Rf[0:64, :]), reads=["Rf"], writes=["Rst%d" % ci])
            P.op("pool", lambda e: e.tensor_tensor(cw[cs][:, 4, 0:64], Ktok_g[:, ci, :], F["E3b"], ALU.mult),
                 reads=["Ktok_g%d" % ci, "facE3b"], writes=["cw%d_4" % cs])
            mm(pb[4][0:64, 128:256], cw[cs][:, 4, 0:64], V_g[:, ci, :], True, True, ["cw%d_4" % cs, "V_g%d" % ci], ["pb4u"])
            P.op("dve", lambda e: e.scalar_tensor_tensor(out=Rf[0:64, :], in0=Rf[0:64, :], scalar=dSb, in1=pb[4][0:64, 128:256], op0=ALU.mult, op1=ALU.add),
                 reads=["Rf", "pb4u", "facE1b"], writes=["Rf"])

        def p1_block(n_, tb):
            s = n_ % 2
            bsl = slice(tb * 512, (tb + 1) * 512)
            load_block(tb, s)
            proj_fm(2, 0, s)
            rms_bcast([0], 1, 1, 128.0, rsb[:], "rsb")
            P.op("dve", lambda e: e.tensor_scalar(cg[:, 0, :], pb[0][:], nrm[:, 2:3], None, ALU.mult), reads=["pb0", "nrm"], writes=["cg0"])
            mm(pb[2][0:64, :], wukv[:, 0:64], cg[:, 0, :], True, True, ["wukv", "cg0"], ["pb2"])
            P.op("dve", lambda e: e.tensor_tensor(KT_m[0:64, bsl], pb[2][0:64, :], rsb[0:64, :], ALU.mult), reads=["pb2", "rsb"], writes=["KT_m%da" % tb])
            proj_fm(3, 3, s); proj_fm(4, 4, s)
            rope_rows(3, 4, s, KT_m[64:96, bsl], "KT_m%db" % tb)
            sumsq_max(KT_m[0:96, bsl], ["KT_m%da" % tb, "KT_m%db" % tb], 96, 3, sc[:, 11:12], "ktmp")
            P.op("dve", lambda e: e.tensor_tensor(sc[:, 10:11], sc[:, 10:11], sc[:, 11:12], ALU.max), reads=["kmax2", "ktmp", "KT_m%db" % tb], writes=["kmax2"])
            proj_fm(6, 3, s)
            P.op("act", lambda e: e.copy(out=KT_g[0:64, bsl], in_=pb[3][0:64, :]), reads=["pb3"], writes=["KT_g%d" % tb])
            proj_fm(7, 4, s)
            P.op("act", lambda e: e.copy(out=lrT[0:48, :], in_=pb[4][0:48, :]), reads=["pb4"], writes=["lrT"])
            for tt in range(4):
                p1_tt(tb, s, tt)
            for tt in range(3, -1, -1):
                p1_chunk(tb, tt)

        for n_, tb in enumerate(range(NBK - 1, -1, -1) if stage >= 1 else []):
            p1_block(n_, tb)

        def p2_gate_tt(s, tt):
            tsl = slice(tt * 128, (tt + 1) * 128)
            for kc in range(8):
                mm(pb[2][:, tsl], xb[s][:, kc, tsl], wC[:, kc, TOK0 + 128:TOK0 + 256], kc == 0, kc == 7, ["wC", "xb%d" % s], ["pb2"])

        def p2_gla_chunk(tb, s, j):
            ci = tb * 4 + j
            cs = ci % 2
            csl = slice(j * 128, (j + 1) * 128)
            gsl = slice(ci * 128, (ci + 1) * 128)
            cwk = "cw%d_" % cs
            c = cw[cs]
            _scan_factors(P, C, pb[7], "pb7", Gblk[:, 0, j, :], "G0_%d" % j, Gblk[:, 1, j, :], "G1_%d" % j, 64, F, "fac")
            P.op("dve", lambda e: e.scalar_tensor_tensor(out=c[0:64, 0, :], in0=qtf[0:64, csl], scalar=QSC, in1=F["E1f"], op0=ALU.mult, op1=ALU.mult),
                 reads=["qtf", "facE1f"], writes=[cwk + "0"])
            P.op("dve", lambda e: e.scalar_tensor_tensor(out=c[0:64, 1, :], in0=qtf[0:64, csl], scalar=QSC, in1=F["E1b"], op0=ALU.mult, op1=ALU.mult),
                 reads=["qtf", "facE1b"], writes=[cwk + "1"])
            P.op("dve", lambda e: e.tensor_tensor(c[0:64, 2, :], KT_g[0:64, gsl], F["E2f"], ALU.mult), reads=["KT_g%d" % tb, "facE2f"], writes=[cwk + "2"])
            P.op("pool", lambda e: e.tensor_tensor(c[0:64, 3, :], KT_g[0:64, gsl], F["E2b"], ALU.mult), reads=["KT_g%d" % tb, "facE2b"], writes=[cwk + "3"])
            mm(pb[3][:, 0:128], c[0:64, 2, :], c[0:64, 0, :], True, True, [cwk + "2", cwk + "0"], ["pb3a"])
            mm(pb[3][:, 128:256], c[0:64, 3, :], c[0:64, 1, :], True, True, [cwk + "3", cwk + "1"], ["pb3b"])
            pmc = pm[cs]
            P.op("dve", lambda e: e.tensor_tensor(pmc[:, 0, :], pb[3][:, 0:128], C["tri_le"][:], ALU.mult), reads=["pb3a", "c_tri_le"], writes=["pm%d_0" % cs])
            P.op("dve", lambda e: e.tensor_tensor(pmc[:, 1, :], pb[3][:, 128:256], C["tri_gt"][:], ALU.mult), reads=["pb3b", "c_tri_gt"], writes=["pm%d_1" % cs])
            oreg = pb[3][:, 256:384]
            mm(oreg, pmc[:, 0, :], V_g[:, ci, :], True, False, ["pm%d_0" % cs, "V_g%d" % ci], ["pb3o"])
            mm(oreg, pmc[:, 1, :], V_g[:, ci, :], False, False, ["pm%d_1" % cs, "V_g%d" % ci], ["pb3o"])
            mm(oreg, c[0:64, 0, :], S_bf[0:64, :], False, False, [cwk + "0", "S_bf"], ["pb3o"])
            mm(oreg, c[0:64, 1, :], Rst[0:64, ci, :], False, True, [cwk + "1", "Rst%d" % ci], ["pb3o"])
            P.op("pool", lambda e: e.tensor_tensor(c[:, 4, 0:64], Ktok_g[:, ci, :], F["E3f"], ALU.mult), reads=["Ktok_g%d" % ci, "facE3f"], writes=[cwk + "4"])
            mm(pb[3][0:64, 384:512], c[:, 4, 0:64], V_g[:, ci, :], True, True, [cwk + "4", "V_g%d" % ci], ["pb3u"])
            P.op("dve", lambda e: e.scalar_tensor_tensor(out=Sf[0:64, :], in0=Sf[0:64, :], scalar=dSf, in1=pb[3][0:64, 384:512], op0=ALU.mult, op1=ALU.add),
                 reads=["Sf", "pb3u", "facE1f"], writes=["Sf"])
            P.op("act", lambda e: e.copy(out=S_bf[0:64, :], in_=Sf[0:64, :]), reads=["Sf"], writes=["S_bf"])
            sx = smx[s]; xk = "gn%d" % s
            odc = od[cs]
            P.op("act", lambda e: e.activation(out=odc[:], in_=oreg, func=AF.Square, accum_out=sx[:, 8:9]), reads=["pb3o"], writes=["od%d" % cs, xk + "ss"])
            P.op("act", lambda e: e.activation(out=sx[:, 9:10], in_=sx[:, 8:9], func=AF.Sqrt, bias=1e-6, scale=1.0 / 128.0), reads=[xk + "ss"], writes=[xk + "rs"])
            P.op("dve", lambda e: e.reciprocal(sx[:, 9:10], sx[:, 9:10]), reads=[xk + "rs"], writes=[xk + "rs"])
            P.op("dve", lambda e: e.scalar_tensor_tensor(out=odc[:], in0=oreg, scalar=sx[:, 9:10], in1=gnb[:], op0=ALU.mult, op1=ALU.mult),
                 reads=["pb3o", xk + "rs", "gnb", "od%d" % cs], writes=["od%d" % cs])
            P.op("pool", lambda e: e.tensor_tensor(outt[s][:, j, 128:256], odc[:], gate[s][:, j, :], ALU.mult),
                 reads=["od%d" % cs, "gate%d" % s], writes=["outt%d_g%d" % (s, j)])

        ptc = [0]

        def p2_attn_step(s, kt):
            sbank = kt % 2
            psl = ptc[0] % 4; ptc[0] += 1
            negc = smx[s][:, 3:4]
            mm(pb[sbank][:], KT_m[0:96, kt * 128:(kt + 1) * 128], QTm[s][0:96, :], True, True,
               ["KT_m%da" % (kt // 4), "KT_m%db" % (kt // 4), "QTm%da" % s, "QTm%db" % s], ["pb%d" % sbank])
            P.op("act", lambda e: e.activation(out=pT[psl][:], in_=pb[sbank][:], func=AF.Exp, bias=negc, scale=SCL),
                 reads=["pb%d" % sbank, "negc%d" % s], writes=["pT%d" % psl])
            for qt in range(4):
                bank = 4 + qt // 2
                areg = pb[bank][:, (qt % 2) * 256:(qt % 2) * 256 + 129]
                mm(areg, pT[psl][:, qt * 128:(qt + 1) * 128], V_m[:, kt, 0:129], kt == 0 and qt % 2 == 0, kt == NCH - 1,
                   ["pT%d" % psl, "V_m%d" % kt, "V_m_ones"], ["pb%d_acc%d" % (bank, qt)], skip=True)

        def p2_attn_epi(s, qt):
            a0 = pb[4 + qt // 2][:, (qt % 2) * 256:(qt % 2) * 256 + 129]
            k0 = "pb%d_acc%d" % (4 + qt // 2, qt)
            sx = smx[s]; xk = "da%d" % s
            P.op("dve", lambda e: e.reciprocal(sx[:, 20:21], a0[:, 128:129]), reads=[k0], writes=[xk + "z0"])
            P.op("dve", lambda e: e.tensor_scalar(outt[s][:, qt, 0:128], a0[:, 0:128], sx[:, 20:21], None, ALU.mult), reads=[k0, xk + "z0"], writes=["outt%d_m%d" % (s, qt)])

        def p2_block(tb):
            s = tb % 2
            sx = smx[s]
            load_block(tb, s)
            proj_fm(0, 0, s); proj_fm(1, 1, s)
            rms_bcast([0, 1], 2, 2, 256.0, rsb[:], "rsb")
            for c_ in range(2):
                P.op("dve", lambda e, c_=c_: e.tensor_scalar(cg[:, c_, :], pb[c_][:], nrm[:, c_:c_ + 1], None, ALU.mult), reads=["pb%d" % c_, "nrm"], writes=["cg%d" % c_])
            for c_ in range(2):
                mm(pb[3][0:96, :], wuq[:, c_, 0:96], cg[:, c_, :], c_ == 0, c_ == 1, ["wuq", "cg%d" % c_], ["pb3"])
            for c_ in range(2):
                mm(pb[4][0:96, :], wuq[:, c_, 96:192], cg[:, c_, :], c_ == 0, c_ == 1, ["wuq", "cg%d" % c_], ["pb4"])
            P.op("dve", lambda e: e.tensor_tensor(QTm[s][0:64, :], pb[3][0:64, :], rsb[0:64, :], ALU.mult), reads=["pb3", "rsb"], writes=["QTm%da" % s])
            rope_rows(3, 4, s, QTm[s][64:96, :], "QTm%db" % s, extra=rsb[64:96, :], extrakey="rsb")
            sumsq_max(QTm[s][0:96, :], ["QTm%da" % s, "QTm%db" % s], 96, 2, sx[:, 0:1], "qmax%d" % s)
            P.op("dve", lambda e: e.tensor_tensor(sx[:, 1:2], sx[:, 0:1], sc[:, 10:11], ALU.mult), reads=["qmax%d" % s, "kmax2", "QTm%db" % s], writes=["c2_%d" % s])
            P.op("act", lambda e: e.activation(out=sx[:, 2:3], in_=sx[:, 1:2], func=AF.Sqrt, scale=(1.01 * SCL) ** 2), reads=["c2_%d" % s], writes=["c_%d" % s])
            P.op("dve", lambda e: e.tensor_scalar(sx[:, 3:4], sx[:, 2:3], -1.0, None, ALU.mult), reads=["c_%d" % s], writes=["negc%d" % s])
            proj_fm(5, 6, s)
            P.op("act", lambda e: e.copy(out=qtf[0:64, :], in_=pb[6][0:64, :]), reads=["pb6"], writes=["qtf"])
            proj_fm(7, 6, s)
            P.op("act", lambda e: e.copy(out=lrT[0:48, :], in_=pb[6][0:48, :]), reads=["pb6"], writes=["lrT"])
            for tt in range(4):
                gates(s, 0, tt)
                gates(s, 1, tt)
            for tt in range(4):
                p2_gate_tt(s, tt)
            P.op("act", lambda e: e.activation(out=gate[s][:], in_=pb[2][:].rearrange("p (j d) -> p j d", j=4), func=AF.Silu), reads=["pb2"], writes=["gate%d" % s])
            for j in range(4):
                p2_gla_chunk(tb, s, j)
            if stage >= 3:
                for kt in range(NCH):
                    p2_attn_step(s, kt)
                for qt in range(4):
                    p2_attn_epi(s, qt)
            P.dma("sp", "out%d" % s, oo[tb * 512:(tb + 1) * 512, :].rearrange("(j p) c -> p j c", p=128), outt[s][:],
                  reads=["outt%d_g%d" % (s, j) for j in range(4)] + (["outt%d_m%d" % (s, j) for j in range(4)] if stage >= 3 else []), writes=["o%d" % tb])

        for tb in (range(NBK) if stage >= 2 else []):
            p2_block(tb)
        P.emit()
    return nc


def _rot_tables(T, rot_dim, theta, ndim, period):
    half = rot_dim // 2
    pos = np.arange(T, dtype=np.float32)
    inv = np.power(np.float32(theta), -np.arange(0, rot_dim, 2, dtype=np.float32) / np.float32(rot_dim)).astype(np.float32)
    ang = (pos[None, :] * inv[:, None]).astype(np.float32)
    cos = np.cos(ang.astype(np.float64)).astype(np.float32)
    sin = np.sin(ang.astype(np.float64)).astype(np.float32)
    ct = np.ones((ndim, T), np.float32)
    stb = np.zeros((ndim, T), np.float32)
    for d in range(ndim):
        l = d % period
        if l < half:
            ct[d] = cos[l]; stb[d] = -sin[l]
        elif l < rot_dim:
            ct[d] = cos[l - half]; stb[d] = sin[l - half]
    return ct, stb


def _rot_perm(ndim, rot_dim, period, offset=0):
    half = rot_dim // 2
    p = np.arange(ndim)
    for d in range(ndim):
        l = (d - offset) % period
        if d < offset:
            continue
        if l < half:
            p[d] = d + half
        elif l < rot_dim:
            p[d] = d - half
    return p


def _pack_mix0_weights(w_in, h):
    rq = w_in[:, 0 * 512 + h * 128: 0 * 512 + (h + 1) * 128]
    rk = w_in[:, 1 * 512 + h * 128: 1 * 512 + (h + 1) * 128]
    rv = w_in[:, 2 * 512 + h * 128: 2 * 512 + (h + 1) * 128]
    rg = w_in[:, 3 * 512 + h * 128: 3 * 512 + (h + 1) * 128]
    dq = w_in[:, 4 * 512 + h * 128: 4 * 512 + (h + 1) * 128]
    dk = w_in[:, 5 * 512 + h * 128: 5 * 512 + (h + 1) * 128]
    dv = w_in[:, 6 * 512 + h * 128: 6 * 512 + (h + 1) * 128]
    pr = _rot_perm(128, 128, 128)
    pd = _rot_perm(128, 16, 64)
    return np.ascontiguousarray(np.concatenate([rq, rq[:, pr], rk, rk[:, pr], dq, dq[:, pd], dk, dk[:, pd], rv, dv, rg], axis=1))


def _mix0_tables(T):
    cR, sR = _rot_tables(T, 128, 10000.0, 128, 128)
    cD, sD = _rot_tables(T, 16, 500000.0, 128, 64)
    return np.ascontiguousarray(np.stack([cR, sR, cD, sD]))


def _pack_mix1(inp_w_in, w_uq, w_ukv, q_norm, kv_norm, w2f, bf, w2b, bb, gla_norm, h):
    o = np.cumsum([0, 256, 128, 32, 256, 256, 512, 512, 16, 16])
    w = inp_w_in
    cq = w[:, o[0]:o[1]]; ckv = w[:, o[1]:o[2]]; kr = w[:, o[2]:o[3]]
    gq = w[:, o[3] + h * 64:o[3] + (h + 1) * 64]; gk = w[:, o[4] + h * 64:o[4] + (h + 1) * 64]
    gv = w[:, o[5] + h * 128:o[5] + (h + 1) * 128]; gg = w[:, o[6] + h * 128:o[6] + (h + 1) * 128]
    lrf = w[:, o[7]:o[8]]; lrb = w[:, o[8]:o[9]]
    z = lambda n: np.zeros((1024, n), np.float32)
    p32 = np.concatenate([np.arange(16, 32), np.arange(0, 16)])
    blk3 = np.concatenate([z(64), kr, z(32)], 1)
    blk4 = np.concatenate([z(64), kr[:, p32], z(32)], 1)
    blk5 = np.concatenate([gq, z(64)], 1)
    blk6 = np.concatenate([gk, z(64)], 1)
    blk7 = np.concatenate([lrf, z(16), lrb, z(80)], 1)
    wC = np.ascontiguousarray(np.concatenate([cq, ckv, blk3, blk4, blk5, blk6, blk7, gv, gg, gk], 1))
    uq = w_uq[:, h * 96:(h + 1) * 96]
    pq = np.concatenate([np.arange(64), 64 + p32])
    wuq = np.ascontiguousarray(np.concatenate([uq, uq[:, pq]], 1))
    wukv = np.ascontiguousarray(w_ukv[:, h * 192:(h + 1) * 192])
    nrm = np.ascontiguousarray(np.stack([q_norm[0:128], q_norm[128:256], kv_norm], 1))
    w2 = np.zeros((48, 64), np.float32)
    w2[0:16] = w2f[:, h * 64:(h + 1) * 64]; w2[32:48] = w2b[:, h * 64:(h + 1) * 64]
    gbias = np.concatenate([bf[h * 64:(h + 1) * 64], bb[h * 64:(h + 1) * 64]])[None, :]
    return {"wC": wC, "wuq": wuq, "wukv": wukv, "nrm": nrm, "w2": w2, "gbias": np.ascontiguousarray(gbias), "gnorm": np.ascontiguousarray(gla_norm[None, :])}


def _mix1_tables(T):
    half = 16
    pos = np.arange(T, dtype=np.float32)
    inv = np.power(np.float32(500000.0), -np.arange(0, 32, 2, dtype=np.float32) / np.float32(32)).astype(np.float32)
    ang = (pos[None, :] * inv[:, None]).astype(np.float32)
    cos = np.cos(ang.astype(np.float64)).astype(np.float32); sin = np.sin(ang.astype(np.float64)).astype(np.float32)
    ct = np.ones((128, T), np.float32); stb = np.zeros((128, T), np.float32)
    for l in range(32):
        if l < half:
            ct[64 + l] = cos[l]; stb[64 + l] = -sin[l]
        else:
            ct[64 + l] = cos[l - half]; stb[64 + l] = sin[l - half]
    return np.ascontiguousarray(np.stack([ct, stb]))


_PROGS = {}


def _prog(name, builder):
    if name not in _PROGS:
        _PROGS[name] = builder()
    return _PROGS[name]


def _run(nc, maps):
    res = run_bass_kernel_spmd(nc, maps, core_ids=list(range(NCORES)))
    return res.results


def _post_launch(x_flat, cat_flat, w_out, lnp, w_r, b_r, wg, wu, wd):
    nc = _prog("post", lambda: build_post(2048))
    maps = []
    for c in range(NCORES):
        sl = slice(c * 2048, (c + 1) * 2048)
        maps.append({"xres": np.ascontiguousarray(x_flat[sl]), "catT": np.ascontiguousarray(cat_flat[sl].T), "w_out": w_out,
                     "lnp": lnp, "w_r": w_r, "b_r": b_r, "w_gate": wg, "w_up": wu, "w_down": wd})
    outs = _run(nc, maps)
    return np.concatenate([outs[c]["xo"] for c in range(NCORES)], axis=0)


def kernel(x, ev_w_in, ev_ret_decay_f, ev_ret_decay_b, ev_lq1, ev_lk1, ev_lq2, ev_lk2, ev_subln, ev_w_out,
           od_w_in, od_q_norm, od_w_uq, od_kv_norm, od_w_ukv, od_gla_w2_f, od_gla_b_f, od_gla_w2_b, od_gla_b_b, od_gla_norm, od_w_out,
           ln1_g, ln1_b, ln2_g, ln2_b, moe_w_grp, moe_b_grp, moe_w_exp, moe_b_exp, moe_w_gate, moe_w_up, moe_w_down):
    f32 = lambda a: np.ascontiguousarray(np.asarray(a, dtype=np.float32))
    x = f32(x)
    B, T, D = x.shape
    H = 4

    def post(layer, x_flat, cat_flat, w_out):
        lnp = f32(np.stack([np.asarray(ln1_g)[layer], np.asarray(ln1_b)[layer], np.asarray(ln2_g)[layer], np.asarray(ln2_b)[layer]]))
        w_r = f32(np.concatenate([np.asarray(moe_w_grp)[layer], np.asarray(moe_w_exp)[layer]], axis=1))
        b_r = f32(np.concatenate([np.asarray(moe_b_grp)[layer], np.asarray(moe_b_exp)[layer]])[None, :])
        return _post_launch(x_flat, cat_flat, f32(w_out), lnp, w_r, b_r, f32(np.asarray(moe_w_gate)[layer]),
                            f32(np.asarray(moe_w_up)[layer]), f32(np.asarray(moe_w_down)[layer]))

    nc0 = _prog("mix0", lambda: build_mix0(T))
    tabs0 = _mix0_tables(T)
    w_in0 = f32(np.asarray(ev_w_in)[0])
    lv = f32(np.stack([np.asarray(ev_lq1)[0], np.asarray(ev_lk1)[0], np.asarray(ev_lq2)[0], np.asarray(ev_lk2)[0]]))
    subln = f32(np.asarray(ev_subln)[0][None, :])
    xT = [np.ascontiguousarray(x[b].T) for b in range(B)]
    maps = []
    for c in range(NCORES):
        b, h = divmod(c, H)
        dec = f32(np.array([[np.asarray(ev_ret_decay_f)[0][h], np.asarray(ev_ret_decay_b)[0][h]]]))
        maps.append({"xT": xT[b], "wA": _pack_mix0_weights(w_in0, h), "tabs": tabs0, "dec": dec, "lv": lv, "subln": subln})
    outs = _run(nc0, maps)
    cat = np.empty((B, T, D), np.float32)
    for c in range(NCORES):
        b, h = divmod(c, H)
        cat[b, :, h * 128:(h + 1) * 128] = outs[c]["o"][:, 0:128]
        cat[b, :, 512 + h * 128:512 + (h + 1) * 128] = outs[c]["o"][:, 128:256]
    x1 = post(0, x.reshape(B * T, D), cat.reshape(B * T, D), np.asarray(ev_w_out)[0]).reshape(B, T, D)

    nc1 = _prog("mix1", lambda: build_mix1(T))
    tabs1 = _mix1_tables(T)
    xT = [np.ascontiguousarray(x1[b].T) for b in range(B)]
    maps = []
    for c in range(NCORES):
        b, h = divmod(c, H)
        m = _pack_mix1(f32(np.asarray(od_w_in)[0]), f32(np.asarray(od_w_uq)[0]), f32(np.asarray(od_w_ukv)[0]), f32(np.asarray(od_q_norm)[0]),
                       f32(np.asarray(od_kv_norm)[0]), f32(np.asarray(od_gla_w2_f)[0]), f32(np.asarray(od_gla_b_f)[0]),
                       f32(np.asarray(od_gla_w2_b)[0]), f32(np.asarray(od_gla_b_b)[0]), f32(np.asarray(od_gla_norm)[0]), h)
        m["xT"] = xT[b]; m["tabs"] = tabs1
        maps.append(m)
    outs = _run(nc1, maps)
    for c in range(NCORES):
        b, h = divmod(c, H)
        cat[b, :, h * 128:(h + 1) * 128] = outs[c]["o"][:, 0:128]
        cat[b, :, 512 + h * 128:512 + (h + 1) * 128] = outs[c]["o"][:, 128:256]
    x2 = post(1, x1.reshape(B * T, D), cat.reshape(B * T, D), np.asarray(od_w_out)[0]).reshape(B, T, D)
    return x2.astype(np.float32)
```

```python
import contextlib
import math
import numpy as np
import concourse.bass as bass
import concourse.mybir as mybir
from concourse.bass_utils import run_bass_kernel_spmd

F32 = mybir.dt.float32
BF16 = mybir.dt.bfloat16
AF = mybir.ActivationFunctionType
ALU = mybir.AluOpType
AX = mybir.AxisListType

DEPTH = 2
ALPHA = (2.0 * DEPTH) ** 0.25
LN_EPS = 1e-5
NCORES = 8


class _Op:
    __slots__ = ("eng", "fn", "deps", "is_dma", "stream", "sidx", "signal", "cnt")


class Prog:
    ENGS = ("pe", "act", "dve", "pool", "sp")

    def __init__(self, nc, same_engine_sync=True):
        self.nc = nc
        self.ops = []
        self.lastw = {}
        self.readers = {}
        self.streams = {}
        self.same_engine_sync = same_engine_sync
        self.bank_last = {}

    def _add(self, eng, fn, reads, writes, is_dma=False, stream=None):
        o = _Op()
        o.eng = eng; o.fn = fn; o.is_dma = is_dma; o.stream = stream
        o.signal = False; o.cnt = 0; o.sidx = 0
        deps = set()
        for r in reads:
            if r in self.lastw:
                deps.add(self.lastw[r])
        for w in writes:
            if w in self.lastw:
                deps.add(self.lastw[w])
            for rr in self.readers.get(w, ()):
                deps.add(rr)
        oid = len(self.ops)
        banks = set()
        for k_ in tuple(reads) + tuple(writes):
            if k_.startswith("pb") and k_[2].isdigit():
                banks.add(k_[2])
        for b_ in banks:
            lb = self.bank_last.get(b_)
            if lb is not None and self.ops[lb].eng != eng:
                deps.add(lb)
            self.bank_last[b_] = oid
        o.deps = deps
        if is_dma:
            n = self.streams.get(stream, 0) + 1
            self.streams[stream] = n
            o.sidx = n
        self.ops.append(o)
        for w in writes:
            self.lastw[w] = oid
            self.readers[w] = []
        for r in reads:
            if r not in writes:
                self.readers.setdefault(r, []).append(oid)
        return oid

    def op(self, eng, fn, reads=(), writes=()):
        return self._add(eng, fn, tuple(reads), tuple(writes))

    def dma(self, q, stream, out, in_, reads=(), writes=()):
        return self._add(q, (out, in_), tuple(reads), tuple(writes), True, stream)

    def _skip(self, do, o):
        return (do.eng == o.eng and not o.is_dma and not do.is_dma
                and (do.eng == "pe" or not self.same_engine_sync))

    def emit(self):
        nc = self.nc
        ops = self.ops
        for o in ops:
            for d in o.deps:
                do = ops[d]
                if do.is_dma or self._skip(do, o):
                    continue
                do.signal = True
        cnt = {e: 0 for e in self.ENGS}
        for o in ops:
            if not o.is_dma and o.signal:
                cnt[o.eng] += 1
                o.cnt = cnt[o.eng]
        with contextlib.ExitStack() as st:
            esem = {e: st.enter_context(nc.semaphore("s_" + e)) for e in self.ENGS}
            ssem = {s: st.enter_context(nc.semaphore("d_" + s)) for s in self.streams}
            block = st.enter_context(nc.Block())
            per = {e: [i for i, o in enumerate(ops) if o.eng == e] for e in self.ENGS}

            def run(engname, eobj):
                known = {}
                for i in per[engname]:
                    o = ops[i]
                    need = {}
                    for d in o.deps:
                        do = ops[d]
                        if do.is_dma:
                            key = ("d", do.stream); val = 16 * do.sidx
                        else:
                            if self._skip(do, o):
                                continue
                            key = ("e", do.eng); val = do.cnt
                        if val > need.get(key, 0):
                            need[key] = val
                    for key, val in need.items():
                        if known.get(key, 0) >= val:
                            continue
                        known[key] = val
                        sem = ssem[key[1]] if key[0] == "d" else esem[key[1]]
                        eobj.wait_ge(sem, val)
                    if o.is_dma:
                        out, in_ = o.fn
                        eobj.dma_start(out=out, in_=in_).then_inc(ssem[o.stream], 16)
                    else:
                        ins = o.fn(eobj)
                        if o.signal:
                            ins.then_inc(esem[engname], 1)
                if engname == "sp":
                    for s, n in self.streams.items():
                        eobj.wait_ge(ssem[s], 16 * n)

            @block.tensor
            def _(e): run("pe", e)

            @block.scalar
            def _(e): run("act", e)

            @block.vector
            def _(e): run("dve", e)

            @block.gpsimd
            def _(e): run("pool", e)

            @block.sync
            def _(e): run("sp", e)


def _bcast_rows(handle, row, n, parts=128):
    return bass.AP(handle, row * n, [[0, parts], [1, n]])


def build_post(ntok=2048):
    nc = bass.Bass("TRN2", target_bir_lowering=False)
    NT = ntok // 128
    NB = ntok // 512
    D = 1024
    NE = 32
    xres_h = nc.dram_tensor("xres", [ntok, D], F32, kind="ExternalInput")
    catT_h = nc.dram_tensor("catT", [D, ntok], F32, kind="ExternalInput")
    wout_h = nc.dram_tensor("w_out", [D, D], F32, kind="ExternalInput")
    lnp_h = nc.dram_tensor("lnp", [4, D], F32, kind="ExternalInput")
    wr_h = nc.dram_tensor("w_r", [D, 36], F32, kind="ExternalInput")
    br_h = nc.dram_tensor("b_r", [1, 36], F32, kind="ExternalInput")
    wg_h = nc.dram_tensor("w_gate", [NE, D, 512], F32, kind="ExternalInput")
    wu_h = nc.dram_tensor("w_up", [NE, D, 512], F32, kind="ExternalInput")
    wd_h = nc.dram_tensor("w_down", [NE, 512, D], F32, kind="ExternalInput")
    xo_h = nc.dram_tensor("xo", [ntok, D], F32, kind="ExternalOutput")
    xres = xres_h.ap(); catT = catT_h.ap(); xo = xo_h.ap()
    BIG = 30000.0

    with contextlib.ExitStack() as st:
        def sb(name, shape, dt):
            return st.enter_context(nc.sbuf_tensor("s_" + name, shape, dt))
        wbuf = [sb("wbuf%d" % i, [128, 12288], BF16) for i in range(2)]
        x1T = sb("x1T", [128, 8, ntok], BF16)
        yacc = sb("yacc", [128, NT, D], F32)
        G = sb("G", [128, NT, NE], F32)
        gb = [sb("lng", [128, D], F32), sb("lnb", [128, D], F32)]
        wr = sb("wr", [128, 8, 36], F32)
        brb = sb("brb", [128, 36], F32)
        ident = sb("ident", [128, 128], F32)
        ones = sb("ones", [128, 128], F32)
        ct = [sb("ct%d" % i, [128, 8, 128], BF16) for i in range(2)]
        xt = [sb("xt%d" % i, [128, D], F32) for i in range(2)]
        x1f = [sb("x1f%d" % i, [128, 8, 128], F32) for i in range(2)]
        hT = [sb("hT%d" % i, [128, 4, 512], BF16) for i in range(2)]
        sg = [sb("sg%d" % i, [128, 512], F32) for i in range(2)]
        sm = [sb("sm%d" % i, [128, 256], F32) for i in range(2)]
        pb = [st.enter_context(nc.psum_tensor("pb%d" % i, [128, 512], F32)) for i in range(8)]

        P = Prog(nc)
        P.op("pool", lambda e: e.memset(ones[:], 1.0), writes=["ones"])
        P.op("pool", lambda e: e.affine_select(out=ident[:], in_=ones[:], pattern=[[-1, 128]],
                                               compare_op=ALU.is_equal, fill=0.0, base=0,
                                               channel_multiplier=1),
             reads=["ones"], writes=["ident"])
        wout_v = wbuf[0][:, 0:8192].rearrange("p (k n) -> p k n", k=8)
        P.dma("pool", "wg0", wout_v, wout_h.ap().rearrange("(k p) n -> p k n", p=128), writes=["w0g", "w0u"])
        P.dma("sp", "c_lng", gb[0][:], _bcast_rows(lnp_h, 0, D), writes=["lng"])
        P.dma("sp", "c_lnb", gb[1][:], _bcast_rows(lnp_h, 1, D), writes=["lnb"])
        P.dma("sp", "c_wr", wr[:], wr_h.ap().rearrange("(k p) n -> p k n", p=128), writes=["wr"])
        P.dma("sp", "c_brb", brb[:], _bcast_rows(br_h, 0, 36), writes=["brb"])

        def layer_norm(src_ap_fn, srckey, s, dst_ap, dstkey, alpha):
            smt = sm[s]; k = "sm%d" % s
            stats = smt[:, 0:12]; mv = smt[:, 12:14]; rs = smt[:, 14:15]
            P.op("dve", lambda e: e.bn_stats(out=smt[:, 0:6], in_=src_ap_fn(0, 512)), reads=[srckey], writes=[k + "a"])
            P.op("dve", lambda e: e.bn_stats(out=smt[:, 6:12], in_=src_ap_fn(512, 1024)), reads=[srckey], writes=[k + "b"])
            P.op("dve", lambda e: e.bn_aggr(out=mv, in_=stats), reads=[k + "a", k + "b"], writes=[k + "mv"])
            P.op("act", lambda e: e.activation(out=rs, in_=smt[:, 13:14], func=AF.Sqrt, bias=LN_EPS,
                                               scale=alpha * alpha), reads=[k + "mv"], writes=[k + "rs"])
            P.op("dve", lambda e: e.reciprocal(rs, rs), reads=[k + "rs"], writes=[k + "rs"])
            if alpha != 1.0:
                P.op("dve", lambda e: e.tensor_scalar(rs, rs, alpha, None, ALU.mult), reads=[k + "rs"], writes=[k + "rs"])
            P.op("dve", lambda e: e.tensor_scalar(dst_ap, src_ap_fn(0, 1024), smt[:, 12:13], rs, ALU.subtract, ALU.mult),
                 reads=[srckey, k + "mv", k + "rs"], writes=[dstkey])
            P.op("dve", lambda e: e.tensor_tensor(dst_ap, dst_ap, gb[0][:], ALU.mult), reads=[dstkey, "lng"], writes=[dstkey])
            P.op("dve", lambda e: e.tensor_tensor(dst_ap, dst_ap, gb[1][:], ALU.add), reads=[dstkey, "lnb"], writes=[dstkey])

        for i in range(NT):
            s = i % 2
            cts = ct[s]; xts = xt[s]; x1fs = x1f[s]; smt = sm[s]
            tsl = slice(i * 128, (i + 1) * 128)
            P.dma("pool", "ct%d" % s, cts[:], catT.rearrange("(k p) t -> p k t", p=128)[:, :, tsl], writes=["ct%d" % s])
            P.dma("sp", "xt%d" % s, xts[:], xres[tsl, :], writes=["xt%d" % s])
            for h in range(2):
                for kc in range(8):
                    P.op("pe", (lambda e, h=h, kc=kc, cts=cts: e.matmul(pb[h][:], cts[:, kc, :], wout_v[:, kc, h * 512:(h + 1) * 512],
                                                                        start=(kc == 0), stop=(kc == 7))),
                         reads=["ct%d" % s, "w0g", "w0u"], writes=["pb%d" % h])
            for h in range(2):
                P.op("dve", (lambda e, h=h, xts=xts: e.scalar_tensor_tensor(out=xts[:, h * 512:(h + 1) * 512], in0=xts[:, h * 512:(h + 1) * 512],
                                                                            scalar=ALPHA, in1=pb[h][:], op0=ALU.mult, op1=ALU.add)),
                     reads=["xt%d" % s, "pb%d" % h], writes=["xt%d" % s])
            layer_norm(lambda a, b, xts=xts: xts[:, a:b], "xt%d" % s, s, yacc[:, i, :], "yacc%d" % i, 1.0)
            for kc in range(8):
                P.op("pe", (lambda e, kc=kc, i=i: e.transpose(pb[2 + kc // 4][:, (kc % 4) * 128:(kc % 4 + 1) * 128],
                                                              yacc[:, i, kc * 128:(kc + 1) * 128], ident[:])),
                     reads=["yacc%d" % i, "ident"], writes=["pb%d" % (2 + kc // 4)])
            P.op("act", lambda e, x1fs=x1fs: e.copy(out=x1fs[:, 0:4, :], in_=pb[2][:].rearrange("p (k t) -> p k t", k=4)),
                 reads=["pb2"], writes=["x1f%da" % s])
            P.op("dve", lambda e, x1fs=x1fs: e.tensor_copy(out=x1fs[:, 4:8, :], in_=pb[3][:].rearrange("p (k t) -> p k t", k=4)),
                 reads=["pb3"], writes=["x1f%db" % s])
            P.op("act", lambda e, tsl=tsl: e.copy(out=x1T[:, 0:4, tsl], in_=pb[2][:].rearrange("p (k t) -> p k t", k=4)),
                 reads=["pb2"], writes=["x1T_%da" % i])
            P.op("dve", lambda e, tsl=tsl: e.tensor_copy(out=x1T[:, 4:8, tsl], in_=pb[3][:].rearrange("p (k t) -> p k t", k=4)),
                 reads=["pb3"], writes=["x1T_%db" % i])
            for kc in range(8):
                P.op("pe", (lambda e, kc=kc, x1fs=x1fs: e.matmul(pb[4][:, 0:36], x1fs[:, kc, :], wr[:, kc, :], start=(kc == 0), stop=(kc == 7))),
                     reads=["x1f%da" % s, "x1f%db" % s, "wr"], writes=["pb4"])
            k = "r%d" % s
            lg = smt[:, 16:52]; gl = smt[:, 16:20]; el = smt[:, 20:52]
            gmax = smt[:, 52:53]; ngmax = smt[:, 53:54]; gsum = smt[:, 54:55]
            oh = smt[:, 56:60]; pen = smt[:, 60:64]; eg = smt[:, 64:68]
            msk = smt[:, 68:100]; m1 = smt[:, 100:132]; m2 = smt[:, 132:164]; msk2 = smt[:, 164:196]
            top1 = smt[:, 196:197]; top2 = smt[:, 197:198]; dd = smt[:, 198:199]; ee = smt[:, 199:200]
            w1 = smt[:, 200:201]; w2 = smt[:, 201:202]; tmp = smt[:, 204:236]
            P.op("dve", lambda e, lg=lg: e.tensor_tensor(lg, pb[4][:, 0:36], brb[:], ALU.add), reads=["pb4", "brb"], writes=[k])
            P.op("dve", lambda e, gmax=gmax, gl=gl: e.reduce_max(out=gmax, in_=gl, axis=AX.X), reads=[k], writes=[k + "gm"])
            P.op("dve", lambda e, oh=oh, gl=gl, gmax=gmax: e.tensor_scalar(oh, gl, gmax, None, ALU.is_ge), reads=[k, k + "gm"], writes=[k + "oh"])
            P.op("dve", lambda e, ngmax=ngmax, gmax=gmax: e.tensor_scalar(ngmax, gmax, -1.0, None, ALU.mult), reads=[k + "gm"], writes=[k + "ngm"])
            P.op("act", lambda e, eg=eg, gl=gl, ngmax=ngmax, gsum=gsum: e.activation(out=eg, in_=gl, func=AF.Exp, bias=ngmax, scale=1.0, accum_out=gsum),
                 reads=[k, k + "ngm"], writes=[k + "eg", k + "gs"])
            P.op("dve", lambda e, gsum=gsum: e.reciprocal(gsum, gsum), reads=[k + "gs"], writes=[k + "gs"])
            P.op("dve", lambda e, pen=pen, oh=oh: e.tensor_scalar(pen, oh, 1.0, BIG, ALU.subtract, ALU.mult), reads=[k + "oh"], writes=[k + "pen"])
            P.op("dve", lambda e, msk=msk, el=el, pen=pen: e.tensor_tensor(msk.rearrange("p (g j) -> p g j", g=4), el.rearrange("p (g j) -> p g j", g=4),
                                                                          pen.unsqueeze(2).to_broadcast([128, 4, 8]), ALU.add),
                 reads=[k, k + "pen"], writes=[k + "msk"])
            P.op("dve", lambda e, top1=top1, msk=msk: e.reduce_max(out=top1, in_=msk, axis=AX.X), reads=[k + "msk"], writes=[k + "t1"])
            P.op("dve", lambda e, m1=m1, msk=msk, top1=top1: e.tensor_scalar(m1, msk, top1, None, ALU.is_ge), reads=[k + "msk", k + "t1"], writes=[k + "m1"])
            P.op("dve", lambda e, msk2=msk2, m1=m1, msk=msk: e.scalar_tensor_tensor(out=msk2, in0=m1, scalar=-BIG, in1=msk, op0=ALU.mult, op1=ALU.add),
                 reads=[k + "m1", k + "msk"], writes=[k + "msk2"])
            P.op("dve", lambda e, top2=top2, msk2=msk2: e.reduce_max(out=top2, in_=msk2, axis=AX.X), reads=[k + "msk2"], writes=[k + "t2"])
            P.op("dve", lambda e, m2=m2, msk2=msk2, top2=top2: e.tensor_scalar(m2, msk2, top2, None, ALU.is_ge), reads=[k + "msk2", k + "t2"], writes=[k + "m2"])
            P.op("dve", lambda e, dd=dd, top2=top2, top1=top1: e.tensor_tensor(dd, top2, top1, ALU.subtract), reads=[k + "t1", k + "t2"], writes=[k + "dd"])
            P.op("act", lambda e, ee=ee, dd=dd: e.activation(out=ee, in_=dd, func=AF.Exp), reads=[k + "dd"], writes=[k + "ee"])
            P.op("dve", lambda e, w1=w1, ee=ee: e.tensor_scalar(w1, ee, 1.0, None, ALU.add), reads=[k + "ee"], writes=[k + "w1"])
            P.op("dve", lambda e, w1=w1: e.reciprocal(w1, w1), reads=[k + "w1"], writes=[k + "w1"])
            P.op("dve", lambda e, w1=w1, gsum=gsum: e.tensor_scalar(w1, w1, gsum, 1.0 / ALPHA, ALU.mult, ALU.mult), reads=[k + "w1", k + "gs"], writes=[k + "w1"])
            P.op("dve", lambda e, w2=w2, ee=ee, w1=w1: e.tensor_tensor(w2, ee, w1, ALU.mult), reads=[k + "ee", k + "w1"], writes=[k + "w2"])
            P.op("dve", lambda e, tmp=tmp, m1=m1, w1=w1: e.tensor_scalar(tmp, m1, w1, None, ALU.mult), reads=[k + "m1", k + "w1"], writes=[k + "tmp"])
            P.op("dve", lambda e, i=i, m2=m2, w2=w2, tmp=tmp: e.scalar_tensor_tensor(out=G[:, i, :], in0=m2, scalar=w2, in1=tmp, op0=ALU.mult, op1=ALU.add),
                 reads=[k + "m2", k + "w2", k + "tmp"], writes=["G%d" % i])

        P.dma("sp", "c_lng", gb[0][:], _bcast_rows(lnp_h, 2, D), writes=["lng"])
        P.dma("sp", "c_lnb", gb[1][:], _bcast_rows(lnp_h, 3, D), writes=["lnb"])

        def load_expert(e_):
            s = e_ % 2
            wb = wbuf[s]
            P.dma("pool", "wg%d" % s, wb[:, 0:4096].rearrange("p (k n) -> p k n", k=8),
                  wg_h.ap()[e_].rearrange("(k p) n -> p k n", p=128), writes=["w%dg" % s])
            P.dma("pool", "wu%d" % s, wb[:, 4096:8192].rearrange("p (k n) -> p k n", k=8),
                  wu_h.ap()[e_].rearrange("(k p) n -> p k n", p=128), writes=["w%du" % s])
            P.dma("pool", "wd%d" % s, wb[:, 8192:12288].rearrange("p (k n) -> p k n", k=4),
                  wd_h.ap()[e_].rearrange("(k p) n -> p k n", p=128), writes=["w%dd" % s])

        load_expert(0)
        hcnt = 0
        for e_ in range(NE):
            s = e_ % 2
            wb = wbuf[s]
            wgv = wb[:, 0:4096].rearrange("p (k n) -> p k n", k=8)
            wuv = wb[:, 4096:8192].rearrange("p (k n) -> p k n", k=8)
            wdv = wb[:, 8192:12288].rearrange("p (k n) -> p k n", k=4)
            if e_ + 1 < NE:
                load_expert(e_ + 1)
            for tb in range(NB):
                hs = hcnt % 2; hcnt += 1
                hTs = hT[hs]
                tkeys = []
                for j in range(4):
                    tkeys += ["x1T_%da" % (tb * 4 + j), "x1T_%db" % (tb * 4 + j)]
                for c in range(4):
                    pg = pb[(c % 2) * 2]; pu = pb[(c % 2) * 2 + 1]
                    kg = "pb%d" % ((c % 2) * 2); ku = "pb%d" % ((c % 2) * 2 + 1)
                    for kc in range(8):
                        P.op("pe", (lambda e, pg=pg, kc=kc, c=c, wgv=wgv, tb=tb: e.matmul(pg[:], wgv[:, kc, c * 128:(c + 1) * 128],
                                                                                          x1T[:, kc, tb * 512:(tb + 1) * 512], start=(kc == 0), stop=(kc == 7))),
                             reads=["w%dg" % s] + tkeys, writes=[kg])
                    for kc in range(8):
                        P.op("pe", (lambda e, pu=pu, kc=kc, c=c, wuv=wuv, tb=tb: e.matmul(pu[:], wuv[:, kc, c * 128:(c + 1) * 128],
                                                                                          x1T[:, kc, tb * 512:(tb + 1) * 512], start=(kc == 0), stop=(kc == 7))),
                             reads=["w%du" % s] + tkeys, writes=[ku])
                    sgs = sg[c % 2]
                    P.op("act", lambda e, sgs=sgs, pg=pg: e.activation(out=sgs[:], in_=pg[:], func=AF.Silu), reads=[kg], writes=["sg%d" % (c % 2)])
                    P.op("dve", lambda e, hTs=hTs, c=c, sgs=sgs, pu=pu: e.tensor_tensor(hTs[:, c, :], sgs[:], pu[:], ALU.mult),
                         reads=["sg%d" % (c % 2), ku], writes=["hT%d_%d" % (hs, c)])
                for tt in range(4):
                    ti = tb * 4 + tt
                    for dh in range(2):
                        py = pb[4 + (tt * 2 + dh) % 4]; ky = "pb%d" % (4 + (tt * 2 + dh) % 4)
                        for c in range(4):
                            P.op("pe", (lambda e, py=py, c=c, tt=tt, dh=dh, hTs=hTs, wdv=wdv: e.matmul(py[:], hTs[:, c, tt * 128:(tt + 1) * 128],
                                                                                                 wdv[:, c, dh * 512:(dh + 1) * 512], start=(c == 0), stop=(c == 3))),
                                 reads=["hT%d_%d" % (hs, c) for c in range(4)] + ["w%dd" % s], writes=[ky])
                        P.op("dve", (lambda e, py=py, ti=ti, dh=dh, e_=e_: e.scalar_tensor_tensor(out=yacc[:, ti, dh * 512:(dh + 1) * 512], in0=py[:],
                                                                                                scalar=G[:, ti, e_:e_ + 1], in1=yacc[:, ti, dh * 512:(dh + 1) * 512],
                                                                                                op0=ALU.mult, op1=ALU.add)),
                             reads=[ky, "G%d" % ti, "yacc%d" % ti], writes=["yacc%d" % ti])

        for i in range(NT):
            s = i % 2
            xts = xt[s]
            layer_norm(lambda a, b, i=i: yacc[:, i, a:b], "yacc%d" % i, s, xts[:], "xt%d" % s, ALPHA)
            P.dma("sp", "out%d" % s, xo[i * 128:(i + 1) * 128, :], xts[:], reads=["xt%d" % s], writes=["xo%d" % i])
        P.emit()
    return nc


def _mk_consts(nc, P, sb):
    C = {}
    C["ones"] = sb("c_ones", [128, 128], F32)
    C["ident"] = sb("c_ident", [128, 128], F32)
    C["tri_le"] = sb("c_tri_le", [128, 128], F32)
    C["tri_ge"] = sb("c_tri_ge", [128, 128], F32)
    C["tri_gt"] = sb("c_tri_gt", [128, 128], F32)
    C["tri_lt"] = sb("c_tri_lt", [128, 128], F32)
    C["ones_bf"] = sb("c_ones_bf", [128, 128], BF16)
    ones = C["ones"]
    P.op("pool", lambda e: e.memset(ones[:], 1.0), writes=["c_ones"])
    P.op("pool", lambda e: e.memset(C["ones_bf"][:], 1.0), writes=["c_ones_bf"])

    def sel(name, step, cm, cmp):
        t = C[name]
        P.op("pool", lambda e: e.affine_select(out=t[:], in_=ones[:], pattern=[[step, 128]], compare_op=cmp,
                                               fill=0.0, base=0, channel_multiplier=cm),
             reads=["c_ones"], writes=["c_" + name])
    sel("ident", -1, 1, ALU.is_equal)
    sel("tri_le", 1, -1, ALU.is_ge)
    sel("tri_ge", -1, 1, ALU.is_ge)
    sel("tri_gt", -1, 1, ALU.is_gt)
    sel("tri_lt", 1, -1, ALU.is_gt)
    return C


def _scan_factors(P, C, ps, pskey, gf, gfkey, gb, gbkey, dk, F, fkey):
    P.op("pe", lambda e: e.matmul(ps[0:dk, 0:128], gf, C["tri_le"][:], start=True, stop=True),
         reads=[gfkey, "c_tri_le"], writes=[pskey + "a"])
    P.op("pe", lambda e: e.matmul(ps[0:dk, 128:256], gb, C["tri_ge"][:], start=True, stop=True),
         reads=[gbkey, "c_tri_ge"], writes=[pskey + "b"])
    P.op("pe", lambda e: e.matmul(ps[:, 256:256 + dk], C["tri_gt"][:], gf, start=True, stop=True),
         reads=[gfkey, "c_tri_gt"], writes=[pskey + "c"])
    P.op("pe", lambda e: e.matmul(ps[:, 384:384 + dk], C["tri_lt"][:], gb, start=True, stop=True),
         reads=[gbkey, "c_tri_lt"], writes=[pskey + "d"])
    P.op("act", lambda e: e.activation(out=F["E1f"], in_=ps[0:dk, 0:128], func=AF.Exp), reads=[pskey + "a"], writes=[fkey + "E1f"])
    P.op("act", lambda e: e.activation(out=F["E2f"], in_=ps[0:dk, 0:128], func=AF.Exp, scale=-1.0), reads=[pskey + "a"], writes=[fkey + "E2f"])
    P.op("act", lambda e: e.activation(out=F["E1b"], in_=ps[0:dk, 128:256], func=AF.Exp), reads=[pskey + "b"], writes=[fkey + "E1b"])
    P.op("act", lambda e: e.activation(out=F["E2b"], in_=ps[0:dk, 128:256], func=AF.Exp, scale=-1.0), reads=[pskey + "b"], writes=[fkey + "E2b"])
    P.op("act", lambda e: e.activation(out=F["E3f"], in_=ps[:, 256:256 + dk], func=AF.Exp), reads=[pskey + "c"], writes=[fkey + "E3f"])
    P.op("act", lambda e: e.activation(out=F["E3b"], in_=ps[:, 384:384 + dk], func=AF.Exp), reads=[pskey + "d"], writes=[fkey + "E3b"])


def build_mix0(T=8192, stage=99):
    nc = bass.Bass("TRN2", target_bir_lowering=False)
    NBK = T // 512
    NCH = T // 128
    D = 1024
    NW = 11 * 128
    xT_h = nc.dram_tensor("xT", [D, T], F32, kind="ExternalInput")
    wA_h = nc.dram_tensor("wA", [D, NW], F32, kind="ExternalInput")
    tabs_h = nc.dram_tensor("tabs", [4, 128, T], F32, kind="ExternalInput")
    dec_h = nc.dram_tensor("dec", [1, 2], F32, kind="ExternalInput")
    lv_h = nc.dram_tensor("lv", [4, 64], F32, kind="ExternalInput")
    subln_h = nc.dram_tensor("subln", [1, 128], F32, kind="ExternalInput")
    o_h = nc.dram_tensor("o", [T, 256], F32, kind="ExternalOutput")
    xT = xT_h.ap().rearrange("(k p) t -> p k t", p=128)
    tabs = tabs_h.ap().rearrange("f p t -> p f t")
    oo = o_h.ap()
    KSC = 128.0 ** -0.5
    LAM_INIT = 0.8 - 0.6 * math.exp(-0.3 * 0)
    SCL = 64.0 ** -0.5

    with contextlib.ExitStack() as st:
        def sb(name, shape, dt):
            return st.enter_context(nc.sbuf_tensor("s_" + name, shape, dt))
        P = Prog(nc)
        C = _mk_consts(nc, P, sb)
        wA = sb("wA", [128, 8, NW], BF16)
        xb = [sb("xb%d" % i, [128, 8, 512], BF16) for i in range(2)]
        tab = [sb("tab%d" % i, [128, 4, 512], F32) for i in range(2)]
        KT_r = sb("KT_r", [128, T], BF16)
        Ktok_r = sb("Ktok_r", [128, NCH, 128], BF16)
        V_r = sb("V_r", [128, NCH, 128], BF16)
        Rst = sb("Rst", [128, NCH, 128], BF16)
        KT_d = sb("KT_d", [128, T], BF16)
        V_d = sb("V_d", [128, NCH, 130], BF16)
        fac = sb("fac", [128, 6, 128], F32)
        Gc = sb("Gc", [128, 2, 128], F32)
        sc = sb("sc", [128, 64], F32)
        lvb = sb("lvb", [128, 4, 64], F32)
        sublnb = sb("sublnb", [128, 128], F32)
        Rf = sb("Rf", [128, 128], F32)
        Sf = sb("Sf", [128, 128], F32)
        S_bf = sb("S_bf", [128, 128], BF16)
        t1 = [sb("t1_%d" % i, [128, 512], F32) for i in range(2)]
        t2 = [sb("t2_%d" % i, [128, 512], F32) for i in range(2)]
        ktf = sb("ktf", [128, 512], F32)
        sq = sb("sq", [128, 512], BF16)
        QTd = [sb("QTd%d" % i, [128, 512], BF16) for i in range(2)]
        gate = [sb("gate%d" % i, [128, 4, 128], F32) for i in range(2)]
        outt = [sb("outt%d" % i, [128, 4, 256], F32) for i in range(2)]
        cw = [sb("cw%d" % i, [128, 6, 128], BF16) for i in range(2)]
        pm = [sb("pm%d" % i, [128, 2, 128], BF16) for i in range(2)]
        pT = [sb("pT%d" % i, [128, 512], BF16) for i in range(4)]
        od = [sb("od%d" % i, [128, 128], F32) for i in range(2)]
        smx = [sb("smx%d" % i, [128, 32], F32) for i in range(2)]
        pb = [st.enter_context(nc.psum_tensor("pb%d" % i, [128, 512], F32)) for i in range(8)]

        if stage == -3:
            P.emit(); return nc
        P.dma("pool", "wA", wA[:], wA_h.ap().rearrange("(k p) n -> p k n", p=128), writes=["wA"])
        P.dma("sp", "c_dec", sc[:, 0:2], _bcast_rows(dec_h, 0, 2), writes=["dec"])
        P.dma("sp", "c_lv", lvb[:], bass.AP(lv_h, 0, [[0, 128], [1, 256]]), writes=["lvb"])
        P.dma("sp", "c_subln", sublnb[:], _bcast_rows(subln_h, 0, 128), writes=["sublnb"])
        if stage == -2:
            P.emit(); return nc
        P.op("act", lambda e: e.activation(out=sc[:, 2:4], in_=sc[:, 0:2], func=AF.Exp), reads=["dec"], writes=["la"])
        P.op("dve", lambda e: e.tensor_scalar(sc[:, 2:4], sc[:, 2:4], -1.0, None, ALU.mult), reads=["la"], writes=["la"])
        for d_ in range(2):
            P.op("dve", lambda e, d_=d_: e.tensor_scalar(Gc[:, d_, :], C["ones"][:], sc[:, 2 + d_:3 + d_], None, ALU.mult),
                 reads=["la", "c_ones"], writes=["Gc%d" % d_])
        F = {"E1f": fac[:, 0, :], "E2f": fac[:, 1, :], "E1b": fac[:, 2, :], "E2b": fac[:, 3, :], "E3f": fac[:, 4, :], "E3b": fac[:, 5, :]}
        _scan_factors(P, C, pb[7], "pb7", Gc[:, 0, :], "Gc0", Gc[:, 1, :], "Gc1", 128, F, "fac")
        FK = ["fac" + k for k in ("E1f", "E2f", "E1b", "E2b", "E3f", "E3b")]
        dSf = fac[:, 0, 127:128]
        dSb = fac[:, 2, 0:1]
        if stage == -1:
            P.emit(); return nc
        P.op("dve", lambda e: e.tensor_tensor(lvb[:, 0, :], lvb[:, 0, :], lvb[:, 1, :], ALU.mult), reads=["lvb"], writes=["lvb"])
        P.op("dve", lambda e: e.tensor_tensor(lvb[:, 2, :], lvb[:, 2, :], lvb[:, 3, :], ALU.mult), reads=["lvb"], writes=["lvb"])
        P.op("dve", lambda e: e.reduce_sum(out=sc[:, 4:5], in_=lvb[:, 0, :], axis=AX.X), reads=["lvb"], writes=["lam_a"])
        P.op("dve", lambda e: e.reduce_sum(out=sc[:, 5:6], in_=lvb[:, 2, :], axis=AX.X), reads=["lvb"], writes=["lam_b"])
        P.op("act", lambda e: e.activation(out=sc[:, 6:8], in_=sc[:, 4:6], func=AF.Exp), reads=["lam_a", "lam_b"], writes=["lam_e"])
        P.op("dve", lambda e: e.tensor_tensor(sc[:, 8:9], sc[:, 6:7], sc[:, 7:8], ALU.subtract), reads=["lam_e"], writes=["lam"])
        P.op("dve", lambda e: e.tensor_scalar(sc[:, 9:10], sc[:, 8:9], LAM_INIT, -1.0, ALU.add, ALU.mult), reads=["lam"], writes=["nlam"])
        P.op("dve", lambda e: e.tensor_scalar(sublnb[:], sublnb[:], 1.0 - LAM_INIT, None, ALU.mult), reads=["sublnb"], writes=["sublnb"])
        P.op("pool", lambda e: e.memset(Rf[:], 0.0), writes=["Rf"])
        P.op("pool", lambda e: e.memset(Sf[:], 0.0), writes=["Sf"])
        P.op("pool", lambda e: e.memset(S_bf[:], 0.0), writes=["S_bf"])
        P.op("pool", lambda e: e.memset(sc[:, 10:11], 0.0), writes=["kmax2"])
        P.op("pool", lambda e: e.memset(V_d[:, :, 128:130], 1.0), writes=["V_d_ones"])

        def load_block(tb, s):
            P.dma("pool", "xb%d" % s, xb[s][:], xT[:, :, tb * 512:(tb + 1) * 512], writes=["xb%d" % s])
            P.dma("sp", "tab%d" % s, tab[s][:], tabs[:, :, tb * 512:(tb + 1) * 512], writes=["tab%d" % s])

        def proj_fm(blk, bank, s):
            for kc in range(8):
                P.op("pe", (lambda e, kc=kc: e.matmul(pb[bank][:], wA[:, kc, blk * 128:(blk + 1) * 128], xb[s][:, kc, :],
                                                      start=(kc == 0), stop=(kc == 7))),
                     reads=["wA", "xb%d" % s], writes=["pb%d" % bank])

        def rotary(bx, bp, s, fc, fs, dst, dstkey, slot):
            a = t1[slot]; b = t2[slot]
            P.op("dve", lambda e: e.tensor_tensor(a[:], pb[bp][:], tab[s][:, fs, :], ALU.mult), reads=["pb%d" % bp, "tab%d" % s], writes=["t1_%d" % slot])
            P.op("dve", lambda e: e.tensor_tensor(b[:], pb[bx][:], tab[s][:, fc, :], ALU.mult), reads=["pb%d" % bx, "tab%d" % s], writes=["t2_%d" % slot])
            P.op("pool", lambda e: e.tensor_tensor(dst, a[:], b[:], ALU.add), reads=["t1_%d" % slot, "t2_%d" % slot], writes=[dstkey])

        def sumsq_max(src, srckey, bank, dst, dstkey):
            P.op("pool", lambda e: e.tensor_tensor(sq[:], src, src, ALU.mult), reads=[srckey], writes=["sq"])
            P.op("pe", lambda e: e.matmul(pb[bank][:], C["ones_bf"][:], sq[:], start=True, stop=True), reads=["sq", "c_ones_bf"], writes=["pb%d" % bank])
            P.op("dve", lambda e: e.reduce_max(out=dst, in_=pb[bank][:], axis=AX.X), reads=["pb%d" % bank], writes=[dstkey])

        def mm(out, lhsT, rhs, start, stop, reads, writes, skip=False):
            P.op("pe", lambda e: e.matmul(out, lhsT, rhs, start=start, stop=stop, skip_group_check=skip), reads=reads, writes=writes)

        def p1_values(tb, s, tt):
            bank = 4 + tt // 2
            o0 = (tt % 2) * 256
            key = "pb%d_%d" % (bank, tt % 2)
            for kc in range(8):
                mm(pb[bank][:, o0:o0 + 256], xb[s][:, kc, tt * 128:(tt + 1) * 128], wA[:, kc, 8 * 128:10 * 128], kc == 0, kc == 7,
                   ["wA", "xb%d" % s], [key])
            ci = tb * 4 + tt
            P.op("act", lambda e: e.copy(out=V_r[:, ci, :], in_=pb[bank][:, o0:o0 + 128]), reads=[key], writes=["V_r%d" % ci])
            P.op("dve", lambda e: e.tensor_copy(out=V_d[:, ci, 0:128], in_=pb[bank][:, o0 + 128:o0 + 256]), reads=[key], writes=["V_d%d" % ci])

        def p1_chunk(ci):
            cs = ci % 2
            P.op("act", lambda e: e.copy(out=Rst[:, ci, :], in_=Rf[:]), reads=["Rf"], writes=["Rst%d" % ci])
            P.op("pool", lambda e: e.tensor_tensor(cw[cs][:, 4, :], Ktok_r[:, ci, :], F["E3b"], ALU.mult),
                 reads=["Ktok_r%d" % (ci // 4), "facE3b"], writes=["cw%d_4" % cs])
            mm(pb[7][:, 0:128], cw[cs][:, 4, :], V_r[:, ci, :], True, True, ["cw%d_4" % cs, "V_r%d" % ci], ["pb7u"])
            P.op("dve", lambda e: e.scalar_tensor_tensor(out=Rf[:], in0=Rf[:], scalar=dSb, in1=pb[7][:, 0:128], op0=ALU.mult, op1=ALU.add),
                 reads=["Rf", "pb7u", "facE1b"], writes=["Rf"])

        def p1_block(n_, tb):
            s = n_ % 2
            bsl = slice(tb * 512, (tb + 1) * 512)
            load_block(tb, s)
            proj_fm(2, 0, s); proj_fm(3, 1, s); proj_fm(6, 2, s); proj_fm(7, 3, s)
            rotary(0, 1, s, 0, 1, ktf[:], "ktf", 0)
            P.op("act", lambda e: e.copy(out=KT_r[:, bsl], in_=ktf[:]), reads=["ktf"], writes=["KT_r%d" % tb])
            for j in range(4):
                P.op("pe", lambda e, j=j: e.transpose(pb[6][:, j * 128:(j + 1) * 128], ktf[:, j * 128:(j + 1) * 128], C["ident"][:]),
                     reads=["ktf", "c_ident"], writes=["pb6"])
            P.op("act", lambda e: e.copy(out=Ktok_r[:, tb * 4:(tb + 1) * 4, :], in_=pb[6][:].rearrange("p (j d) -> p j d", j=4)),
                 reads=["pb6"], writes=["Ktok_r%d" % tb])
            rotary(2, 3, s, 2, 3, KT_d[:, bsl], "KT_d%d" % tb, 1)
            sumsq_max(KT_d[:, bsl], "KT_d%d" % tb, 2, sc[:, 11:12], "ktmp")
            P.op("dve", lambda e: e.tensor_tensor(sc[:, 10:11], sc[:, 10:11], sc[:, 11:12], ALU.max), reads=["kmax2", "ktmp"], writes=["kmax2"])
            for tt in range(4):
                p1_values(tb, s, tt)
            for ci in range(tb * 4 + 3, tb * 4 - 1, -1):
                p1_chunk(ci)

        for n_, tb in enumerate(range(NBK - 1, -1, -1) if stage >= 1 else []):
            p1_block(n_, tb)

        def p2_gate_tt(s, tt):
            for kc in range(8):
                mm(pb[3][:, tt * 128:(tt + 1) * 128], xb[s][:, kc, tt * 128:(tt + 1) * 128], wA[:, kc, 10 * 128:11 * 128], kc == 0, kc == 7,
                   ["wA", "xb%d" % s], ["pb3"])

        def p2_ret_chunk(tb, s, j):
            ci = tb * 4 + j
            cs = ci % 2
            csl = slice(j * 128, (j + 1) * 128)
            gsl = slice(ci * 128, (ci + 1) * 128)
            cwk = "cw%d_" % cs
            c = cw[cs]
            P.op("dve", lambda e: e.scalar_tensor_tensor(out=c[:, 0, :], in0=ktf[:, csl], scalar=KSC, in1=F["E1f"], op0=ALU.mult, op1=ALU.mult),
                 reads=["ktf", "facE1f"], writes=[cwk + "0"])
            P.op("dve", lambda e: e.scalar_tensor_tensor(out=c[:, 1, :], in0=ktf[:, csl], scalar=KSC, in1=F["E1b"], op0=ALU.mult, op1=ALU.mult),
                 reads=["ktf", "facE1b"], writes=[cwk + "1"])
            P.op("dve", lambda e: e.tensor_tensor(c[:, 2, :], KT_r[:, gsl], F["E2f"], ALU.mult), reads=["KT_r%d" % tb, "facE2f"], writes=[cwk + "2"])
            P.op("pool", lambda e: e.tensor_tensor(c[:, 3, :], KT_r[:, gsl], F["E2b"], ALU.mult), reads=["KT_r%d" % tb, "facE2b"], writes=[cwk + "3"])
            mm(pb[2][:, 0:128], c[:, 2, :], c[:, 0, :], True, True, [cwk + "2", cwk + "0"], ["pb2a"])
            mm(pb[2][:, 128:256], c[:, 3, :], c[:, 1, :], True, True, [cwk + "3", cwk + "1"], ["pb2b"])
            pmc = pm[cs]
            P.op("dve", lambda e: e.tensor_tensor(pmc[:, 0, :], pb[2][:, 0:128], C["tri_le"][:], ALU.mult), reads=["pb2a", "c_tri_le"], writes=["pm%d_0" % cs])
            P.op("dve", lambda e: e.tensor_tensor(pmc[:, 1, :], pb[2][:, 128:256], C["tri_gt"][:], ALU.mult), reads=["pb2b", "c_tri_gt"], writes=["pm%d_1" % cs])
            oreg = pb[2][:, 256:384]
            mm(oreg, pmc[:, 0, :], V_r[:, ci, :], True, False, ["pm%d_0" % cs, "V_r%d" % ci], ["pb2o"])
            mm(oreg, pmc[:, 1, :], V_r[:, ci, :], False, False, ["pm%d_1" % cs, "V_r%d" % ci], ["pb2o"])
            mm(oreg, c[:, 0, :], S_bf[:], False, False, [cwk + "0", "S_bf"], ["pb2o"])
            mm(oreg, c[:, 1, :], Rst[:, ci, :], False, True, [cwk + "1", "Rst%d" % ci], ["pb2o"])
            P.op("pool", lambda e: e.tensor_tensor(c[:, 4, :], Ktok_r[:, ci, :], F["E3f"], ALU.mult), reads=["Ktok_r%d" % tb, "facE3f"], writes=[cwk + "4"])
            mm(pb[2][:, 384:512], c[:, 4, :], V_r[:, ci, :], True, True, [cwk + "4", "V_r%d" % ci], ["pb2u"])
            P.op("dve", lambda e: e.scalar_tensor_tensor(out=Sf[:], in0=Sf[:], scalar=dSf, in1=pb[2][:, 384:512], op0=ALU.mult, op1=ALU.add),
                 reads=["Sf", "pb2u", "facE1f"], writes=["Sf"])
            P.op("act", lambda e: e.copy(out=S_bf[:], in_=Sf[:]), reads=["Sf"], writes=["S_bf"])
            sx = smx[s]; xk = "gn%d" % s
            odc = od[cs]
            P.op("dve", lambda e: e.bn_stats(out=sx[:, 8:14], in_=oreg), reads=["pb2o"], writes=[xk + "st"])
            P.op("dve", lambda e: e.bn_aggr(out=sx[:, 14:16], in_=sx[:, 8:14]), reads=[xk + "st"], writes=[xk + "mv"])
            P.op("act", lambda e: e.activation(out=sx[:, 16:17], in_=sx[:, 15:16], func=AF.Sqrt, bias=LN_EPS, scale=1.0), reads=[xk + "mv"], writes=[xk + "rs"])
            P.op("dve", lambda e: e.reciprocal(sx[:, 16:17], sx[:, 16:17]), reads=[xk + "rs"], writes=[xk + "rs"])
            P.op("dve", lambda e: e.tensor_scalar(odc[:], oreg, sx[:, 14:15], sx[:, 16:17], ALU.subtract, ALU.mult),
                 reads=["pb2o", xk + "mv", xk + "rs"], writes=["od%d" % cs])
            P.op("pool", lambda e: e.tensor_tensor(outt[s][:, j, 0:128], odc[:], gate[s][:, j, :], ALU.mult),
                 reads=["od%d" % cs, "gate%d" % s], writes=["outt%d_r%d" % (s, j)])

        def p2_attn_qk(s, comp, kt, i):
            rows = slice(comp * 64, comp * 64 + 64)
            sbank = i % 2
            psl = i % 4
            negc = smx[s][:, 3:4]
            mm(pb[sbank][:], KT_d[rows, kt * 128:(kt + 1) * 128], QTd[s][rows, :], True, True, ["KT_d%d" % (kt // 4), "QTd%d" % s], ["pb%d" % sbank])
            P.op("act", lambda e: e.activation(out=pT[psl][:], in_=pb[sbank][:], func=AF.Exp, bias=negc, scale=SCL),
                 reads=["pb%d" % sbank, "negc%d" % s], writes=["pT%d" % psl])

        def p2_attn_pv(s, comp, kt, i):
            psl = i % 4
            for qt in range(4):
                bank = 4 + comp * 2 + qt // 2
                areg = pb[bank][:, (qt % 2) * 256:(qt % 2) * 256 + 129]
                mm(areg, pT[psl][:, qt * 128:(qt + 1) * 128], V_d[:, kt, 0:129], kt == 0 and qt % 2 == 0, kt == NCH - 1,
                   ["pT%d" % psl, "V_d%d" % kt, "V_d_ones"], ["pb%d_acc%d" % (bank, qt)], skip=True)

        def p2_attention(s):
            steps = [(comp, kt) for comp in range(2) for kt in range(NCH)]
            for i in range(min(2, len(steps))):
                p2_attn_qk(s, steps[i][0], steps[i][1], i)
            for i, (comp, kt) in enumerate(steps):
                p2_attn_pv(s, comp, kt, i)
                if i + 2 < len(steps):
                    p2_attn_qk(s, steps[i + 2][0], steps[i + 2][1], i + 2)

        def p2_attn_epi(s, qt):
            a0 = pb[4 + qt // 2][:, (qt % 2) * 256:(qt % 2) * 256 + 129]
            a1 = pb[6 + qt // 2][:, (qt % 2) * 256:(qt % 2) * 256 + 129]
            k0 = "pb%d_acc%d" % (4 + qt // 2, qt); k1 = "pb%d_acc%d" % (6 + qt // 2, qt)
            sx = smx[s]; xk = "da%d" % s; cs = qt % 2
            odc = od[cs]
            P.op("dve", lambda e: e.reciprocal(sx[:, 20:21], a0[:, 128:129]), reads=[k0], writes=[xk + "z0"])
            P.op("dve", lambda e: e.reciprocal(sx[:, 21:22], a1[:, 128:129]), reads=[k1], writes=[xk + "z1"])
            P.op("dve", lambda e: e.tensor_tensor(sx[:, 21:22], sx[:, 21:22], sc[:, 9:10], ALU.mult), reads=[xk + "z1", "nlam"], writes=[xk + "z1"])
            P.op("dve", lambda e: e.tensor_scalar(odc[:], a0[:, 0:128], sx[:, 20:21], None, ALU.mult), reads=[k0, xk + "z0"], writes=["od%d" % cs])
            P.op("dve", lambda e: e.scalar_tensor_tensor(out=odc[:], in0=a1[:, 0:128], scalar=sx[:, 21:22], in1=odc[:], op0=ALU.mult, op1=ALU.add),
                 reads=[k1, xk + "z1", "od%d" % cs], writes=["od%d" % cs])
            P.op("act", lambda e: e.activation(out=t1[0][:, 0:128], in_=odc[:], func=AF.Square, accum_out=sx[:, 22:23]),
                 reads=["od%d" % cs], writes=["t1_0", xk + "ss"])
            P.op("act", lambda e: e.activation(out=sx[:, 23:24], in_=sx[:, 22:23], func=AF.Sqrt, bias=1e-6, scale=1.0 / 128.0), reads=[xk + "ss"], writes=[xk + "rs"])
            P.op("dve", lambda e: e.reciprocal(sx[:, 23:24], sx[:, 23:24]), reads=[xk + "rs"], writes=[xk + "rs"])
            P.op("dve", lambda e: e.scalar_tensor_tensor(out=outt[s][:, qt, 128:256], in0=odc[:], scalar=sx[:, 23:24], in1=sublnb[:], op0=ALU.mult, op1=ALU.mult),
                 reads=["od%d" % cs, xk + "rs", "sublnb"], writes=["outt%d_d%d" % (s, qt)])

        def p2_block(tb):
            s = tb % 2
            sx = smx[s]
            load_block(tb, s)
            proj_fm(0, 0, s); proj_fm(1, 1, s); proj_fm(4, 2, s); proj_fm(5, 3, s)
            rotary(0, 1, s, 0, 1, ktf[:], "ktf", 0)
            rotary(2, 3, s, 2, 3, QTd[s][:], "QTd%d" % s, 1)
            sumsq_max(QTd[s][:], "QTd%d" % s, 1, sx[:, 0:1], "qmax%d" % s)
            P.op("dve", lambda e: e.tensor_tensor(sx[:, 1:2], sx[:, 0:1], sc[:, 10:11], ALU.mult), reads=["qmax%d" % s, "kmax2"], writes=["c2_%d" % s])
            P.op("act", lambda e: e.activation(out=sx[:, 2:3], in_=sx[:, 1:2], func=AF.Sqrt, scale=(1.01 * SCL) ** 2), reads=["c2_%d" % s], writes=["c_%d" % s])
            P.op("dve", lambda e: e.tensor_scalar(sx[:, 3:4], sx[:, 2:3], -1.0, None, ALU.mult), reads=["c_%d" % s], writes=["negc%d" % s])
            for tt in range(4):
                p2_gate_tt(s, tt)
            P.op("act", lambda e: e.activation(out=gate[s][:], in_=pb[3][:].rearrange("p (j d) -> p j d", j=4), func=AF.Silu), reads=["pb3"], writes=["gate%d" % s])
            for j in range(4):
                p2_ret_chunk(tb, s, j)
            if stage >= 3:
                p2_attention(s)
                for qt in range(4):
                    p2_attn_epi(s, qt)
            P.dma("sp", "out%d" % s, oo[tb * 512:(tb + 1) * 512, :].rearrange("(j p) c -> p j c", p=128), outt[s][:],
                  reads=["outt%d_r%d" % (s, j) for j in range(4)] + (["outt%d_d%d" % (s, j) for j in range(4)] if stage >= 3 else []), writes=["o%d" % tb])

        for tb in (range(NBK) if stage >= 2 else []):
            p2_block(tb)
        P.emit()
    return nc


def build_mix1(T=8192, stage=99):
    nc = bass.Bass("TRN2", target_bir_lowering=False)
    NBK = T // 512
    NCH = T // 128
    D = 1024
    NW = 8 * 128 + 320
    TOK0 = 8 * 128
    xT_h = nc.dram_tensor("xT", [D, T], F32, kind="ExternalInput")
    wC_h = nc.dram_tensor("wC", [D, NW], F32, kind="ExternalInput")
    wuq_h = nc.dram_tensor("wuq", [256, 192], F32, kind="ExternalInput")
    wukv_h = nc.dram_tensor("wukv", [128, 192], F32, kind="ExternalInput")
    nrm_h = nc.dram_tensor("nrm", [128, 3], F32, kind="ExternalInput")
    w2_h = nc.dram_tensor("w2", [48, 64], F32, kind="ExternalInput")
    gbias_h = nc.dram_tensor("gbias", [1, 128], F32, kind="ExternalInput")
    gnorm_h = nc.dram_tensor("gnorm", [1, 128], F32, kind="ExternalInput")
    tabs_h = nc.dram_tensor("tabs", [2, 128, T], F32, kind="ExternalInput")
    o_h = nc.dram_tensor("o", [T, 256], F32, kind="ExternalOutput")
    xT = xT_h.ap().rearrange("(k p) t -> p k t", p=128)
    tabs = tabs_h.ap().rearrange("f p t -> p f t")
    oo = o_h.ap()
    SCL = 96.0 ** -0.5
    QSC = 64.0 ** -0.5

    with contextlib.ExitStack() as st:
        def sb(name, shape, dt):
            return st.enter_context(nc.sbuf_tensor("s_" + name, shape, dt))
        P = Prog(nc)
        C = _mk_consts(nc, P, sb)
        wC = sb("wC", [128, 8, NW], BF16)
        wuq = sb("wuq", [128, 2, 192], BF16)
        wukv = sb("wukv", [128, 192], BF16)
        nrm = sb("nrm", [128, 3], F32)
        w2 = sb("w2", [48, 64], BF16)
        gbb = sb("gbb", [128, 2, 64], F32)
        gnb = sb("gnb", [128, 128], F32)
        xb = [sb("xb%d" % i, [128, 8, 512], BF16) for i in range(2)]
        tab = [sb("tab%d" % i, [128, 2, 512], F32) for i in range(2)]
        KT_m = sb("KT_m", [128, T], BF16)
        V_m = sb("V_m", [128, NCH, 130], BF16)
        KT_g = sb("KT_g", [128, T], BF16)
        Ktok_g = sb("Ktok_g", [128, NCH, 64], BF16)
        V_g = sb("V_g", [128, NCH, 128], BF16)
        Rst = sb("Rst", [128, NCH, 128], BF16)
        fac = sb("fac", [128, 6, 128], F32)
        sc = sb("sc", [128, 64], F32)
        Rf = sb("Rf", [128, 128], F32)
        Sf = sb("Sf", [128, 128], F32)
        S_bf = sb("S_bf", [128, 128], BF16)
        t1 = sb("t1", [128, 512], F32)
        t2 = sb("t2", [128, 512], F32)
        sqf = sb("sqf", [128, 2, 512], F32)
        rsb = sb("rsb", [128, 512], F32)
        cg = sb("cg", [128, 2, 512], BF16)
        sq = sb("sq", [128, 512], BF16)
        qtf = sb("qtf", [128, 512], F32)
        lrT = sb("lrT", [128, 512], BF16)
        Gblk = sb("Gblk", [128, 2, 4, 64], F32)
        zt = sb("zt", [128, 512], F32)
        QTm = [sb("QTm%d" % i, [128, 512], BF16) for i in range(2)]
        gate = [sb("gate%d" % i, [128, 4, 128], F32) for i in range(2)]
        outt = [sb("outt%d" % i, [128, 4, 256], F32) for i in range(2)]
        cw = [sb("cw%d" % i, [128, 6, 128], BF16) for i in range(2)]
        pm = [sb("pm%d" % i, [128, 2, 128], BF16) for i in range(2)]
        pT = [sb("pT%d" % i, [128, 512], BF16) for i in range(4)]
        od = [sb("od%d" % i, [128, 128], F32) for i in range(2)]
        smx = [sb("smx%d" % i, [128, 32], F32) for i in range(2)]
        pb = [st.enter_context(nc.psum_tensor("pb%d" % i, [128, 512], F32)) for i in range(8)]

        P.dma("pool", "wC", wC[:], wC_h.ap().rearrange("(k p) n -> p k n", p=128), writes=["wC"])
        P.dma("pool", "wuq", wuq[:], wuq_h.ap().rearrange("(k p) n -> p k n", p=128), writes=["wuq"])
        P.dma("pool", "wukv", wukv[:], wukv_h.ap(), writes=["wukv"])
        P.dma("pool", "w2", w2[:], w2_h.ap(), writes=["w2"])
        P.dma("sp", "c_nrm", nrm[:], nrm_h.ap(), writes=["nrm"])
        P.dma("sp", "c_gbb", gbb[:], bass.AP(gbias_h, 0, [[0, 128], [1, 128]]), writes=["gbb"])
        P.dma("sp", "c_gnb", gnb[:], _bcast_rows(gnorm_h, 0, 128), writes=["gnb"])
        P.op("pool", lambda e: e.memset(Rf[:], 0.0), writes=["Rf"])
        P.op("pool", lambda e: e.memset(Sf[:], 0.0), writes=["Sf"])
        P.op("pool", lambda e: e.memset(S_bf[:], 0.0), writes=["S_bf"])
        P.op("pool", lambda e: e.memset(sc[:, 10:11], 0.0), writes=["kmax2"])
        P.op("pool", lambda e: e.memset(V_m[:, :, 128:130], 1.0), writes=["V_m_ones"])
        F = {"E1f": fac[0:64, 0, :], "E2f": fac[0:64, 1, :], "E1b": fac[0:64, 2, :], "E2b": fac[0:64, 3, :], "E3f": fac[:, 4, 0:64], "E3b": fac[:, 5, 0:64]}
        dSf = fac[0:64, 0, 127:128]
        dSb = fac[0:64, 2, 0:1]

        def mm(out, lhsT, rhs, start, stop, reads, writes, skip=False):
            P.op("pe", lambda e: e.matmul(out, lhsT, rhs, start=start, stop=stop, skip_group_check=skip), reads=reads, writes=writes)

        def load_block(tb, s):
            P.dma("pool", "xb%d" % s, xb[s][:], xT[:, :, tb * 512:(tb + 1) * 512], writes=["xb%d" % s])
            P.dma("sp", "tab%d" % s, tab[s][:], tabs[:, :, tb * 512:(tb + 1) * 512], writes=["tab%d" % s])

        def proj_fm(blk, bank, s):
            for kc in range(8):
                mm(pb[bank][:], wC[:, kc, blk * 128:(blk + 1) * 128], xb[s][:, kc, :], kc == 0, kc == 7, ["wC", "xb%d" % s], ["pb%d" % bank])

        def rms_bcast(src_banks, nchunk, bank, ndim, dst, dstkey):
            for c in range(nchunk):
                P.op("act", lambda e, c=c: e.activation(out=sqf[:, c, :], in_=pb[src_banks[c]][:], func=AF.Square),
                     reads=["pb%d" % src_banks[c]], writes=["sqf%d" % c])
            for c in range(nchunk):
                mm(pb[bank][:], C["ones"][:], sqf[:, c, :], c == 0, c == nchunk - 1, ["c_ones", "sqf%d" % c], ["pb%d" % bank])
            P.op("act", lambda e: e.activation(out=dst, in_=pb[bank][:], func=AF.Sqrt, bias=1e-6, scale=1.0 / ndim), reads=["pb%d" % bank], writes=[dstkey])
            P.op("dve", lambda e: e.reciprocal(dst, dst), reads=[dstkey], writes=[dstkey])

        def sumsq_max(src, srckey, nrows, bank, dst, dstkey):
            P.op("pool", lambda e: e.tensor_tensor(sq[0:nrows, :], src, src, ALU.mult), reads=list(srckey), writes=["sq"])
            mm(pb[bank][:], C["ones_bf"][0:nrows, :], sq[0:nrows, :], True, True, ["sq", "c_ones_bf"], ["pb%d" % bank])
            P.op("dve", lambda e: e.reduce_max(out=dst, in_=pb[bank][:], axis=AX.X), reads=["pb%d" % bank], writes=[dstkey])

        def rope_rows(bx, bp, s, dst, dstkey, extra=None, extrakey=None):
            r = slice(64, 96)
            P.op("dve", lambda e: e.tensor_tensor(t1[r, :], pb[bp][r, :], tab[s][r, 1, :], ALU.mult), reads=["pb%d" % bp, "tab%d" % s], writes=["t1r"])
            P.op("dve", lambda e: e.tensor_tensor(t2[r, :], pb[bx][r, :], tab[s][r, 0, :], ALU.mult), reads=["pb%d" % bx, "tab%d" % s], writes=["t2r"])
            if extra is None:
                P.op("pool", lambda e: e.tensor_tensor(dst, t1[r, :], t2[r, :], ALU.add), reads=["t1r", "t2r"], writes=[dstkey])
            else:
                P.op("pool", lambda e: e.tensor_tensor(t1[r, :], t1[r, :], t2[r, :], ALU.add), reads=["t1r", "t2r"], writes=["t1r"])
                P.op("pool", lambda e: e.tensor_tensor(dst, t1[r, :], extra, ALU.mult), reads=["t1r", extrakey], writes=[dstkey])

        def gates(s, d_, tt):
            r = slice(0, 16) if d_ == 0 else slice(32, 48)
            reg = pb[6][:, (d_ * 4 + tt) * 64:(d_ * 4 + tt + 1) * 64]
            key = "pb6g%d_%d" % (d_, tt)
            mm(reg, lrT[r, tt * 128:(tt + 1) * 128], w2[r, :], True, True, ["lrT", "w2"], [key])
            z = zt[:, (d_ * 4 + tt) * 64:(d_ * 4 + tt + 1) * 64]
            zk = "zt%d_%d" % (d_, tt)
            P.op("dve", lambda e: e.tensor_tensor(z, reg, gbb[:, d_, :], ALU.add), reads=[key, "gbb"], writes=[zk])
            P.op("act", lambda e: e.activation(out=z, in_=z, func=AF.Exp, scale=-1.0), reads=[zk], writes=[zk])
            P.op("act", lambda e: e.activation(out=z, in_=z, func=AF.Ln, bias=1.0, scale=1.0), reads=[zk], writes=[zk])
            P.op("dve", lambda e: e.tensor_scalar(Gblk[:, d_, tt, :], z, -1.0 / 16.0, None, ALU.mult), reads=[zk], writes=["G%d_%d" % (d_, tt)])

        def p1_tt(tb, s, tt):
            ci = tb * 4 + tt
            tsl = slice(tt * 128, (tt + 1) * 128)
            mm(pb[5][:, tsl], cg[:, 0, tsl], wukv[:, 64:192], True, True, ["cg0", "wukv"], ["pb5v%d" % tt])
            mm(pb[2][:, 256 + tt:257 + tt], sqf[:, 0, tsl], C["ones"][:, 0:1], True, True, ["sqf0", "c_ones"], ["pb2c%d" % tt])
            sx = smx[s]
            P.op("act", lambda e: e.activation(out=sx[:, 24 + tt:25 + tt], in_=pb[2][:, 256 + tt:257 + tt], func=AF.Sqrt, bias=1e-6, scale=1.0 / 128.0),
                 reads=["pb2c%d" % tt], writes=["rc%d_%d" % (s, tt)])
            P.op("dve", lambda e: e.reciprocal(sx[:, 24 + tt:25 + tt], sx[:, 24 + tt:25 + tt]), reads=["rc%d_%d" % (s, tt)], writes=["rc%d_%d" % (s, tt)])
            P.op("dve", lambda e: e.tensor_scalar(V_m[:, ci, 0:128], pb[5][:, tsl], sx[:, 24 + tt:25 + tt], None, ALU.mult),
                 reads=["pb5v%d" % tt, "rc%d_%d" % (s, tt)], writes=["V_m%d" % ci])
            bank = tt % 2
            key = "pb%dtok" % bank
            for kc in range(8):
                mm(pb[bank][:, 0:320], xb[s][:, kc, tsl], wC[:, kc, TOK0:TOK0 + 320], kc == 0, kc == 7, ["wC", "xb%d" % s], [key])
            P.op("act", lambda e: e.copy(out=V_g[:, ci, :], in_=pb[bank][:, 0:128]), reads=[key], writes=["V_g%d" % ci])
            P.op("dve", lambda e: e.tensor_copy(out=Ktok_g[:, ci, :], in_=pb[bank][:, 256:320]), reads=[key], writes=["Ktok_g%d" % ci])
            gates(s, 1, tt)

        def p1_chunk(tb, tt):
            ci = tb * 4 + tt
            cs = ci % 2
            g = Gblk[:, 1, tt, :]
            _scan_factors(P, C, pb[7], "pb7", g, "G1_%d" % tt, g, "G1_%d" % tt, 64, F, "fac")
            P.op("act", lambda e: e.copy(out=Rst[0:64, ci, :], in_=Rf[0:64, :]), reads=["Rf"], writes=["Rst%d" % ci])
            P.op("pool", lambda e: e.tensor_tensor(cw[cs][:, 4, 0:64], Ktok_g[:, ci, :], F["E3b"], ALU.mult),
                 reads=["Ktok_g%d" % ci, "facE3b"], writes=["cw%d_4" % cs])
            mm(pb[4][0:64, 128:256], cw[cs][:, 4, 0:64], V_g[:, ci, :], True, True, ["cw%d_4" % cs, "V_g%d" % ci], ["pb4u"])
            P.op("dve", lambda e: e.scalar_tensor_tensor(out=Rf[0:64, :], in0=Rf[0:64, :], scalar=dSb, in1=pb[4][0:64, 128:256], op0=ALU.mult, op1=ALU.add),
                 reads=["Rf", "pb4u", "facE1b"], writes=["Rf"])

        def p1_block(n_, tb):
            s = n_ % 2
            bsl = slice(tb * 512, (tb + 1) * 512)
            load_block(tb, s)
            proj_fm(2, 0, s)
            rms_bcast([0], 1, 1, 128.0, rsb[:], "rsb")
            P.op("dve", lambda e: e.tensor_scalar(cg[:, 0, :], pb[0][:], nrm[:, 2:3], None, ALU.mult), reads=["pb0", "nrm"], writes=["cg0"])
            mm(pb[2][0:64, :], wukv[:, 0:64], cg[:, 0, :], True, True, ["wukv", "cg0"], ["pb2"])
            P.op("dve", lambda e: e.tensor_tensor(KT_m[0:64, bsl], pb[2][0:64, :], rsb[0:64, :], ALU.mult), reads=["pb2", "rsb"], writes=["KT_m%da" % tb])
            proj_fm(3, 3, s); proj_fm(4, 4, s)
            rope_rows(3, 4, s, KT_m[64:96, bsl], "KT_m%db" % tb)
            sumsq_max(KT_m[0:96, bsl], ["KT_m%da" % tb, "KT_m%db" % tb], 96, 3, sc[:, 11:12], "ktmp")
            P.op("dve", lambda e: e.tensor_tensor(sc[:, 10:11], sc[:, 10:11], sc[:, 11:12], ALU.max), reads=["kmax2", "ktmp", "KT_m%db" % tb], writes=["kmax2"])
            proj_fm(6, 3, s)
            P.op("act", lambda e: e.copy(out=KT_g[0:64, bsl], in_=pb[3][0:64, :]), reads=["pb3"], writes=["KT_g%d" % tb])
            proj_fm(7, 4, s)
            P.op("act", lambda e: e.copy(out=lrT[0:48, :], in_=pb[4][0:48, :]), reads=["pb4"], writes=["lrT"])
            for tt in range(4):
                p1_tt(tb, s, tt)
            for tt in range(3, -1, -1):
                p1_chunk(tb, tt)

        for n_, tb in enumerate(range(NBK - 1, -1, -1) if stage >= 1 else []):
            p1_block(n_, tb)

        def p2_gate_tt(s, tt):
            tsl = slice(tt * 128, (tt + 1) * 128)
            for kc in range(8):
                mm(pb[2][:, tsl], xb[s][:, kc, tsl], wC[:, kc, TOK0 + 128:TOK0 + 256], kc == 0, kc == 7, ["wC", "xb%d" % s], ["pb2"])

        def p2_gla_chunk(tb, s, j):
            ci = tb * 4 + j
            cs = ci % 2
            csl = slice(j * 128, (j + 1) * 128)
            gsl = slice(ci * 128, (ci + 1) * 128)
            cwk = "cw%d_" % cs
            c = cw[cs]
            _scan_factors(P, C, pb[7], "pb7", Gblk[:, 0, j, :], "G0_%d" % j, Gblk[:, 1, j, :], "G1_%d" % j, 64, F, "fac")
            P.op("dve", lambda e: e.scalar_tensor_tensor(out=c[0:64, 0, :], in0=qtf[0:64, csl], scalar=QSC, in1=F["E1f"], op0=ALU.mult, op1=ALU.mult),
                 reads=["qtf", "facE1f"], writes=[cwk + "0"])
            P.op("dve", lambda e: e.scalar_tensor_tensor(out=c[0:64, 1, :], in0=qtf[0:64, csl], scalar=QSC, in1=F["E1b"], op0=ALU.mult, op1=ALU.mult),
                 reads=["qtf", "facE1b"], writes=[cwk + "1"])
            P.op("dve", lambda e: e.tensor_tensor(c[0:64, 2, :], KT_g[0:64, gsl], F["E2f"], ALU.mult), reads=["KT_g%d" % tb, "facE2f"], writes=[cwk + "2"])
            P.op("pool", lambda e: e.tensor_tensor(c[0:64, 3, :], KT_g[0:64, gsl], F["E2b"], ALU.mult), reads=["KT_g%d" % tb, "facE2b"], writes=[cwk + "3"])
            mm(pb[3][:, 0:128], c[0:64, 2, :], c[0:64, 0, :], True, True, [cwk + "2", cwk + "0"], ["pb3a"])
            mm(pb[3][:, 128:256], c[0:64, 3, :], c[0:64, 1, :], True, True, [cwk + "3", cwk + "1"], ["pb3b"])
            pmc = pm[cs]
            P.op("dve", lambda e: e.tensor_tensor(pmc[:, 0, :], pb[3][:, 0:128], C["tri_le"][:], ALU.mult), reads=["pb3a", "c_tri_le"], writes=["pm%d_0" % cs])
            P.op("dve", lambda e: e.tensor_tensor(pmc[:, 1, :], pb[3][:, 128:256], C["tri_gt"][:], ALU.mult), reads=["pb3b", "c_tri_gt"], writes=["pm%d_1" % cs])
            oreg = pb[3][:, 256:384]
            mm(oreg, pmc[:, 0, :], V_g[:, ci, :], True, False, ["pm%d_0" % cs, "V_g%d" % ci], ["pb3o"])
            mm(oreg, pmc[:, 1, :], V_g[:, ci, :], False, False, ["pm%d_1" % cs, "V_g%d" % ci], ["pb3o"])
            mm(oreg, c[0:64, 0, :], S_bf[0:64, :], False, False, [cwk + "0", "S_bf"], ["pb3o"])
            mm(oreg, c[0:64, 1, :], Rst[0:64, ci, :], False, True, [cwk + "1", "Rst%d" % ci], ["pb3o"])
            P.op("pool", lambda e: e.tensor_tensor(c[:, 4, 0:64], Ktok_g[:, ci, :], F["E3f"], ALU.mult), reads=["Ktok_g%d" % ci, "facE3f"], writes=[cwk + "4"])
            mm(pb[3][0:64, 384:512], c[:, 4, 0:64], V_g[:, ci, :], True, True, [cwk + "4", "V_g%d" % ci], ["pb3u"])
            P.op("dve", lambda e: e.scalar_tensor_tensor(out=Sf[0:64, :], in0=Sf[0:64, :], scalar=dSf, in1=pb[3][0:64, 384:512], op0=ALU.mult, op1=ALU.add),
                 reads=["Sf", "pb3u", "facE1f"], writes=["Sf"])
            P.op("act", lambda e: e.copy(out=S_bf[0:64, :], in_=Sf[0:64, :]), reads=["Sf"], writes=["S_bf"])
            sx = smx[s]; xk = "gn%d" % s
            odc = od[cs]
            P.op("act", lambda e: e.activation(out=odc[:], in_=oreg, func=AF.Square, accum_out=sx[:, 8:9]), reads=["pb3o"], writes=["od%d" % cs, xk + "ss"])
            P.op("act", lambda e: e.activation(out=sx[:, 9:10], in_=sx[:, 8:9], func=AF.Sqrt, bias=1e-6, scale=1.0 / 128.0), reads=[xk + "ss"], writes=[xk + "rs"])
            P.op("dve", lambda e: e.reciprocal(sx[:, 9:10], sx[:, 9:10]), reads=[xk + "rs"], writes=[xk + "rs"])
            P.op("dve", lambda e: e.scalar_tensor_tensor(out=odc[:], in0=oreg, scalar=sx[:, 9:10], in1=gnb[:], op0=ALU.mult, op1=ALU.mult),
                 reads=["pb3o", xk + "rs", "gnb", "od%d" % cs], writes=["od%d" % cs])
            P.op("pool", lambda e: e.tensor_tensor(outt[s][:, j, 128:256], odc[:], gate[s][:, j, :], ALU.mult),
                 reads=["od%d" % cs, "gate%d" % s], writes=["outt%d_g%d" % (s, j)])

        def p2_attn_qk(s, kt, i):
            sbank = i % 2
            psl = i % 4
            negc = smx[s][:, 3:4]
            mm(pb[sbank][:], KT_m[0:96, kt * 128:(kt + 1) * 128], QTm[s][0:96, :], True, True,
               ["KT_m%da" % (kt // 4), "KT_m%db" % (kt // 4), "QTm%da" % s, "QTm%db" % s], ["pb%d" % sbank])
            P.op("act", lambda e: e.activation(out=pT[psl][:], in_=pb[sbank][:], func=AF.Exp, bias=negc, scale=SCL),
                 reads=["pb%d" % sbank, "negc%d" % s], writes=["pT%d" % psl])

        def p2_attn_pv(s, kt, i):
            psl = i % 4
            for qt in range(4):
                bank = 4 + qt // 2
                areg = pb[bank][:, (qt % 2) * 256:(qt % 2) * 256 + 129]
                mm(areg, pT[psl][:, qt * 128:(qt + 1) * 128], V_m[:, kt, 0:129], kt == 0 and qt % 2 == 0, kt == NCH - 1,
                   ["pT%d" % psl, "V_m%d" % kt, "V_m_ones"], ["pb%d_acc%d" % (bank, qt)], skip=True)

        def p2_attention(s):
            for i in range(min(2, NCH)):
                p2_attn_qk(s, i, i)
            for kt in range(NCH):
                p2_attn_pv(s, kt, kt)
                if kt + 2 < NCH:
                    p2_attn_qk(s, kt + 2, kt + 2)

        def p2_attn_epi(s, qt):
            a0 = pb[4 + qt // 2][:, (qt % 2) * 256:(qt % 2) * 256 + 129]
            k0 = "pb%d_acc%d" % (4 + qt // 2, qt)
            sx = smx[s]; xk = "da%d" % s
            P.op("dve", lambda e: e.reciprocal(sx[:, 20:21], a0[:, 128:129]), reads=[k0], writes=[xk + "z0"])
            P.op("dve", lambda e: e.tensor_scalar(outt[s][:, qt, 0:128], a0[:, 0:128], sx[:, 20:21], None, ALU.mult), reads=[k0, xk + "z0"], writes=["outt%d_m%d" % (s, qt)])

        def p2_block(tb):
            s = tb % 2
            sx = smx[s]
            load_block(tb, s)
            proj_fm(0, 0, s); proj_fm(1, 1, s)
            rms_bcast([0, 1], 2, 2, 256.0, rsb[:], "rsb")
            for c_ in range(2):
                P.op("dve", lambda e, c_=c_: e.tensor_scalar(cg[:, c_, :], pb[c_][:], nrm[:, c_:c_ + 1], None, ALU.mult), reads=["pb%d" % c_, "nrm"], writes=["cg%d" % c_])
            for c_ in range(2):
                mm(pb[3][0:96, :], wuq[:, c_, 0:96], cg[:, c_, :], c_ == 0, c_ == 1, ["wuq", "cg%d" % c_], ["pb3"])
            for c_ in range(2):
                mm(pb[4][0:96, :], wuq[:, c_, 96:192], cg[:, c_, :], c_ == 0, c_ == 1, ["wuq", "cg%d" % c_], ["pb4"])
            P.op("dve", lambda e: e.tensor_tensor(QTm[s][0:64, :], pb[3][0:64, :], rsb[0:64, :], ALU.mult), reads=["pb3", "rsb"], writes=["QTm%da" % s])
            rope_rows(3, 4, s, QTm[s][64:96, :], "QTm%db" % s, extra=rsb[64:96, :], extrakey="rsb")
            sumsq_max(QTm[s][0:96, :], ["QTm%da" % s, "QTm%db" % s], 96, 2, sx[:, 0:1], "qmax%d" % s)
            P.op("dve", lambda e: e.tensor_tensor(sx[:, 1:2], sx[:, 0:1], sc[:, 10:11], ALU.mult), reads=["qmax%d" % s, "kmax2", "QTm%db" % s], writes=["c2_%d" % s])
            P.op("act", lambda e: e.activation(out=sx[:, 2:3], in_=sx[:, 1:2], func=AF.Sqrt, scale=(1.01 * SCL) ** 2), reads=["c2_%d" % s], writes=["c_%d" % s])
            P.op("dve", lambda e: e.tensor_scalar(sx[:, 3:4], sx[:, 2:3], -1.0, None, ALU.mult), reads=["c_%d" % s], writes=["negc%d" % s])
            proj_fm(5, 6, s)
            P.op("act", lambda e: e.copy(out=qtf[0:64, :], in_=pb[6][0:64, :]), reads=["pb6"], writes=["qtf"])
            proj_fm(7, 6, s)
            P.op("act", lambda e: e.copy(out=lrT[0:48, :], in_=pb[6][0:48, :]), reads=["pb6"], writes=["lrT"])
            for tt in range(4):
                gates(s, 0, tt)
                gates(s, 1, tt)
            for tt in range(4):
                p2_gate_tt(s, tt)
            P.op("act", lambda e: e.activation(out=gate[s][:], in_=pb[2][:].rearrange("p (j d) -> p j d", j=4), func=AF.Silu), reads=["pb2"], writes=["gate%d" % s])
            for j in range(4):
                p2_gla_chunk(tb, s, j)
            if stage >= 3:
                p2_attention(s)
                for qt in range(4):
                    p2_attn_epi(s, qt)
            P.dma("sp", "out%d" % s, oo[tb * 512:(tb + 1) * 512, :].rearrange("(j p) c -> p j c", p=128), outt[s][:],
                  reads=["outt%d_g%d" % (s, j) for j in range(4)] + (["outt%d_m%d" % (s, j) for j in range(4)] if stage >= 3 else []), writes=["o%d" % tb])

        for tb in (range(NBK) if stage >= 2 else []):
            p2_block(tb)
        P.emit()
    return nc


def _rot_tables(T, rot_dim, theta, ndim, period):
    half = rot_dim // 2
    pos = np.arange(T, dtype=np.float32)
    inv = np.power(np.float32(theta), -np.arange(0, rot_dim, 2, dtype=np.float32) / np.float32(rot_dim)).astype(np.float32)
    ang = (pos[None, :] * inv[:, None]).astype(np.float32)
    cos = np.cos(ang.astype(np.float64)).astype(np.float32)
    sin = np.sin(ang.astype(np.float64)).astype(np.float32)
    ct = np.ones((ndim, T), np.float32)
    stb = np.zeros((ndim, T), np.float32)
    for d in range(ndim):
        l = d % period
        if l < half:
            ct[d] = cos[l]; stb[d] = -sin[l]
        elif l < rot_dim:
            ct[d] = cos[l - half]; stb[d] = sin[l - half]
    return ct, stb


def _rot_perm(ndim, rot_dim, period, offset=0):
    half = rot_dim // 2
    p = np.arange(ndim)
    for d in range(ndim):
        l = (d - offset) % period
        if d < offset:
            continue
        if l < half:
            p[d] = d + half
        elif l < rot_dim:
            p[d] = d - half
    return p


def _pack_mix0_weights(w_in, h):
    rq = w_in[:, 0 * 512 + h * 128: 0 * 512 + (h + 1) * 128]
    rk = w_in[:, 1 * 512 + h * 128: 1 * 512 + (h + 1) * 128]
    rv = w_in[:, 2 * 512 + h * 128: 2 * 512 + (h + 1) * 128]
    rg = w_in[:, 3 * 512 + h * 128: 3 * 512 + (h + 1) * 128]
    dq = w_in[:, 4 * 512 + h * 128: 4 * 512 + (h + 1) * 128]
    dk = w_in[:, 5 * 512 + h * 128: 5 * 512 + (h + 1) * 128]
    dv = w_in[:, 6 * 512 + h * 128: 6 * 512 + (h + 1) * 128]
    pr = _rot_perm(128, 128, 128)
    pd = _rot_perm(128, 16, 64)
    return np.ascontiguousarray(np.concatenate([rq, rq[:, pr], rk, rk[:, pr], dq, dq[:, pd], dk, dk[:, pd], rv, dv, rg], axis=1))


def _mix0_tables(T):
    cR, sR = _rot_tables(T, 128, 10000.0, 128, 128)
    cD, sD = _rot_tables(T, 16, 500000.0, 128, 64)
    return np.ascontiguousarray(np.stack([cR, sR, cD, sD]))


def _pack_mix1(inp_w_in, w_uq, w_ukv, q_norm, kv_norm, w2f, bf, w2b, bb, gla_norm, h):
    o = np.cumsum([0, 256, 128, 32, 256, 256, 512, 512, 16, 16])
    w = inp_w_in
    cq = w[:, o[0]:o[1]]; ckv = w[:, o[1]:o[2]]; kr = w[:, o[2]:o[3]]
    gq = w[:, o[3] + h * 64:o[3] + (h + 1) * 64]; gk = w[:, o[4] + h * 64:o[4] + (h + 1) * 64]
    gv = w[:, o[5] + h * 128:o[5] + (h + 1) * 128]; gg = w[:, o[6] + h * 128:o[6] + (h + 1) * 128]
    lrf = w[:, o[7]:o[8]]; lrb = w[:, o[8]:o[9]]
    z = lambda n: np.zeros((1024, n), np.float32)
    p32 = np.concatenate([np.arange(16, 32), np.arange(0, 16)])
    blk3 = np.concatenate([z(64), kr, z(32)], 1)
    blk4 = np.concatenate([z(64), kr[:, p32], z(32)], 1)
    blk5 = np.concatenate([gq, z(64)], 1)
    blk6 = np.concatenate([gk, z(64)], 1)
    blk7 = np.concatenate([lrf, z(16), lrb, z(80)], 1)
    wC = np.ascontiguousarray(np.concatenate([cq, ckv, blk3, blk4, blk5, blk6, blk7, gv, gg, gk], 1))
    uq = w_uq[:, h * 96:(h + 1) * 96]
    pq = np.concatenate([np.arange(64), 64 + p32])
    wuq = np.ascontiguousarray(np.concatenate([uq, uq[:, pq]], 1))
    wukv = np.ascontiguousarray(w_ukv[:, h * 192:(h + 1) * 192])
    nrm = np.ascontiguousarray(np.stack([q_norm[0:128], q_norm[128:256], kv_norm], 1))
    w2 = np.zeros((48, 64), np.float32)
    w2[0:16] = w2f[:, h * 64:(h + 1) * 64]; w2[32:48] = w2b[:, h * 64:(h + 1) * 64]
    gbias = np.concatenate([bf[h * 64:(h + 1) * 64], bb[h * 64:(h + 1) * 64]])[None, :]
    return {"wC": wC, "wuq": wuq, "wukv": wukv, "nrm": nrm, "w2": w2, "gbias": np.ascontiguousarray(gbias), "gnorm": np.ascontiguousarray(gla_norm[None, :])}


def _mix1_tables(T):
    half = 16
    pos = np.arange(T, dtype=np.float32)
    inv = np.power(np.float32(500000.0), -np.arange(0, 32, 2, dtype=np.float32) / np.float32(32)).astype(np.float32)
    ang = (pos[None, :] * inv[:, None]).astype(np.float32)
    cos = np.cos(ang.astype(np.float64)).astype(np.float32); sin = np.sin(ang.astype(np.float64)).astype(np.float32)
    ct = np.ones((128, T), np.float32); stb = np.zeros((128, T), np.float32)
    for l in range(32):
        if l < half:
            ct[64 + l] = cos[l]; stb[64 + l] = -sin[l]
        else:
            ct[64 + l] = cos[l - half]; stb[64 + l] = sin[l - half]
    return np.ascontiguousarray(np.stack([ct, stb]))


_PROGS = {}


def _prog(name, builder):
    if name not in _PROGS:
        _PROGS[name] = builder()
    return _PROGS[name]


def _run(nc, maps):
    res = run_bass_kernel_spmd(nc, maps, core_ids=list(range(NCORES)))
    return res.results


def _post_launch(x_flat, cat_flat, w_out, lnp, w_r, b_r, wg, wu, wd):
    nc = _prog("post", lambda: build_post(2048))
    maps = []
    for c in range(NCORES):
        sl = slice(c * 2048, (c + 1) * 2048)
        maps.append({"xres": np.ascontiguousarray(x_flat[sl]), "catT": np.ascontiguousarray(cat_flat[sl].T), "w_out": w_out,
                     "lnp": lnp, "w_r": w_r, "b_r": b_r, "w_gate": wg, "w_up": wu, "w_down": wd})
    outs = _run(nc, maps)
    return np.concatenate([outs[c]["xo"] for c in range(NCORES)], axis=0)


def kernel(x, ev_w_in, ev_ret_decay_f, ev_ret_decay_b, ev_lq1, ev_lk1, ev_lq2, ev_lk2, ev_subln, ev_w_out,
           od_w_in, od_q_norm, od_w_uq, od_kv_norm, od_w_ukv, od_gla_w2_f, od_gla_b_f, od_gla_w2_b, od_gla_b_b, od_gla_norm, od_w_out,
           ln1_g, ln1_b, ln2_g, ln2_b, moe_w_grp, moe_b_grp, moe_w_exp, moe_b_exp, moe_w_gate, moe_w_up, moe_w_down):
    f32 = lambda a: np.ascontiguousarray(np.asarray(a, dtype=np.float32))
    x = f32(x)
    B, T, D = x.shape
    H = 4

    def post(layer, x_flat, cat_flat, w_out):
        lnp = f32(np.stack([np.asarray(ln1_g)[layer], np.asarray(ln1_b)[layer], np.asarray(ln2_g)[layer], np.asarray(ln2_b)[layer]]))
        w_r = f32(np.concatenate([np.asarray(moe_w_grp)[layer], np.asarray(moe_w_exp)[layer]], axis=1))
        b_r = f32(np.concatenate([np.asarray(moe_b_grp)[layer], np.asarray(moe_b_exp)[layer]])[None, :])
        return _post_launch(x_flat, cat_flat, f32(w_out), lnp, w_r, b_r, f32(np.asarray(moe_w_gate)[layer]),
                            f32(np.asarray(moe_w_up)[layer]), f32(np.asarray(moe_w_down)[layer]))

    nc0 = _prog("mix0", lambda: build_mix0(T))
    tabs0 = _mix0_tables(T)
    w_in0 = f32(np.asarray(ev_w_in)[0])
    lv = f32(np.stack([np.asarray(ev_lq1)[0], np.asarray(ev_lk1)[0], np.asarray(ev_lq2)[0], np.asarray(ev_lk2)[0]]))
    subln = f32(np.asarray(ev_subln)[0][None, :])
    xT = [np.ascontiguousarray(x[b].T) for b in range(B)]
    maps = []
    for c in range(NCORES):
        b, h = divmod(c, H)
        dec = f32(np.array([[np.asarray(ev_ret_decay_f)[0][h], np.asarray(ev_ret_decay_b)[0][h]]]))
        maps.append({"xT": xT[b], "wA": _pack_mix0_weights(w_in0, h), "tabs": tabs0, "dec": dec, "lv": lv, "subln": subln})
    outs = _run(nc0, maps)
    cat = np.empty((B, T, D), np.float32)
    for c in range(NCORES):
        b, h = divmod(c, H)
        cat[b, :, h * 128:(h + 1) * 128] = outs[c]["o"][:, 0:128]
        cat[b, :, 512 + h * 128:512 + (h + 1) * 128] = outs[c]["o"][:, 128:256]
    x1 = post(0, x.reshape(B * T, D), cat.reshape(B * T, D), np.asarray(ev_w_out)[0]).reshape(B, T, D)

    nc1 = _prog("mix1", lambda: build_mix1(T))
    tabs1 = _mix1_tables(T)
    xT = [np.ascontiguousarray(x1[b].T) for b in range(B)]
    maps = []
    for c in range(NCORES):
        b, h = divmod(c, H)
        m = _pack_mix1(f32(np.asarray(od_w_in)[0]), f32(np.asarray(od_w_uq)[0]), f32(np.asarray(od_w_ukv)[0]), f32(np.asarray(od_q_norm)[0]),
                       f32(np.asarray(od_kv_norm)[0]), f32(np.asarray(od_gla_w2_f)[0]), f32(np.asarray(od_gla_b_f)[0]),
                       f32(np.asarray(od_gla_w2_b)[0]), f32(np.asarray(od_gla_b_b)[0]), f32(np.asarray(od_gla_norm)[0]), h)
        m["xT"] = xT[b]; m["tabs"] = tabs1
        maps.append(m)
    outs = _run(nc1, maps)
    for c in range(NCORES):
        b, h = divmod(c, H)
        cat[b, :, h * 128:(h + 1) * 128] = outs[c]["o"][:, 0:128]
        cat[b, :, 512 + h * 128:512 + (h + 1) * 128] = outs[c]["o"][:, 128:256]
    x2 = post(1, x1.reshape(B * T, D), cat.reshape(B * T, D), np.asarray(od_w_out)[0]).reshape(B, T, D)
    return x2.astype(np.float32)
```

```python
import contextlib
import math
import numpy as np
import concourse.bass as bass
import concourse.mybir as mybir
from concourse.bass_utils import run_bass_kernel_spmd

F32 = mybir.dt.float32
BF16 = mybir.dt.bfloat16
AF = mybir.ActivationFunctionType
ALU = mybir.AluOpType
AX = mybir.AxisListType

DEPTH = 2
ALPHA = (2.0 * DEPTH) ** 0.25
LN_EPS = 1e-5
NCORES = 8


class _Op:
    __slots__ = ("eng", "fn", "deps", "is_dma", "stream", "sidx", "signal", "cnt")


class Prog:
    ENGS = ("pe", "act", "dve", "pool", "sp")

    def __init__(self, nc, same_engine_sync=True):
        self.nc = nc
        self.ops = []
        self.lastw = {}
        self.readers = {}
        self.streams = {}
        self.same_engine_sync = same_engine_sync
        self.bank_last = {}
        self._cap = None

    def begin_capture(self):
        self._cap = []

    def end_capture(self):
        c, self._cap = self._cap, None
        return c

    def replay(self, cap, n):
        for _ in range(min(n, len(cap))):
            a = cap.pop(0)
            self._add(*a)

    def _add(self, eng, fn, reads, writes, is_dma=False, stream=None):
        if self._cap is not None:
            self._cap.append((eng, fn, reads, writes, is_dma, stream))
            return None
        o = _Op()
        o.eng = eng; o.fn = fn; o.is_dma = is_dma; o.stream = stream
        o.signal = False; o.cnt = 0; o.sidx = 0
        deps = set()
        for r in reads:
            if r in self.lastw:
                deps.add(self.lastw[r])
        for w in writes:
            if w in self.lastw:
                deps.add(self.lastw[w])
            for rr in self.readers.get(w, ()):
                deps.add(rr)
        oid = len(self.ops)
        banks = set()
        for k_ in tuple(reads) + tuple(writes):
            if k_.startswith("pb") and k_[2].isdigit():
                banks.add(k_[2])
        for b_ in banks:
            lb = self.bank_last.get(b_)
            if lb is not None and self.ops[lb].eng != eng:
                deps.add(lb)
            self.bank_last[b_] = oid
        o.deps = deps
        if is_dma:
            n = self.streams.get(stream, 0) + 1
            self.streams[stream] = n
            o.sidx = n
        self.ops.append(o)
        for w in writes:
            self.lastw[w] = oid
            self.readers[w] = []
        for r in reads:
            if r not in writes:
                self.readers.setdefault(r, []).append(oid)
        return oid

    def op(self, eng, fn, reads=(), writes=()):
        return self._add(eng, fn, tuple(reads), tuple(writes))

    def dma(self, q, stream, out, in_, reads=(), writes=()):
        return self._add(q, (out, in_), tuple(reads), tuple(writes), True, stream)

    def _skip(self, do, o):
        return (do.eng == o.eng and not o.is_dma and not do.is_dma
                and (do.eng == "pe" or not self.same_engine_sync))

    def emit(self):
        nc = self.nc
        ops = self.ops
        for o in ops:
            for d in o.deps:
                do = ops[d]
                if do.is_dma or self._skip(do, o):
                    continue
                do.signal = True
        cnt = {e: 0 for e in self.ENGS}
        for o in ops:
            if not o.is_dma and o.signal:
                cnt[o.eng] += 1
                o.cnt = cnt[o.eng]
        with contextlib.ExitStack() as st:
            esem = {e: st.enter_context(nc.semaphore("s_" + e)) for e in self.ENGS}
            ssem = {s: st.enter_context(nc.semaphore("d_" + s)) for s in self.streams}
            block = st.enter_context(nc.Block())
            per = {e: [i for i, o in enumerate(ops) if o.eng == e] for e in self.ENGS}

            def run(engname, eobj):
                known = {}
                for i in per[engname]:
                    o = ops[i]
                    need = {}
                    for d in o.deps:
                        do = ops[d]
                        if do.is_dma:
                            key = ("d", do.stream); val = 16 * do.sidx
                        else:
                            if self._skip(do, o):
                                continue
                            key = ("e", do.eng); val = do.cnt
                        if val > need.get(key, 0):
                            need[key] = val
                    for key, val in need.items():
                        if known.get(key, 0) >= val:
                            continue
                        known[key] = val
                        sem = ssem[key[1]] if key[0] == "d" else esem[key[1]]
                        eobj.wait_ge(sem, val)
                    if o.is_dma:
                        out, in_ = o.fn
                        eobj.dma_start(out=out, in_=in_).then_inc(ssem[o.stream], 16)
                    else:
                        ins = o.fn(eobj)
                        if o.signal:
                            ins.then_inc(esem[engname], 1)
                if engname == "sp":
                    for s, n in self.streams.items():
                        eobj.wait_ge(ssem[s], 16 * n)

            @block.tensor
            def _(e): run("pe", e)

            @block.scalar
            def _(e): run("act", e)

            @block.vector
            def _(e): run("dve", e)

            @block.gpsimd
            def _(e): run("pool", e)

            @block.sync
            def _(e): run("sp", e)


def _bcast_rows(handle, row, n, parts=128):
    return bass.AP(handle, row * n, [[0, parts], [1, n]])


def build_post(ntok=2048):
    nc = bass.Bass("TRN2", target_bir_lowering=False)
    NT = ntok // 128
    NB = ntok // 512
    D = 1024
    NE = 32
    xres_h = nc.dram_tensor("xres", [ntok, D], F32, kind="ExternalInput")
    catT_h = nc.dram_tensor("catT", [D, ntok], F32, kind="ExternalInput")
    wout_h = nc.dram_tensor("w_out", [D, D], F32, kind="ExternalInput")
    lnp_h = nc.dram_tensor("lnp", [4, D], F32, kind="ExternalInput")
    wr_h = nc.dram_tensor("w_r", [D, 36], F32, kind="ExternalInput")
    br_h = nc.dram_tensor("b_r", [1, 36], F32, kind="ExternalInput")
    wg_h = nc.dram_tensor("w_gate", [NE, D, 512], F32, kind="ExternalInput")
    wu_h = nc.dram_tensor("w_up", [NE, D, 512], F32, kind="ExternalInput")
    wd_h = nc.dram_tensor("w_down", [NE, 512, D], F32, kind="ExternalInput")
    xo_h = nc.dram_tensor("xo", [ntok, D], F32, kind="ExternalOutput")
    xres = xres_h.ap(); catT = catT_h.ap(); xo = xo_h.ap()
    BIG = 30000.0

    with contextlib.ExitStack() as st:
        def sb(name, shape, dt):
            return st.enter_context(nc.sbuf_tensor("s_" + name, shape, dt))
        wbuf = [sb("wbuf%d" % i, [128, 12288], BF16) for i in range(2)]
        x1T = sb("x1T", [128, 8, ntok], BF16)
        yacc = sb("yacc", [128, NT, D], F32)
        G = sb("G", [128, NT, NE], F32)
        gb = [sb("lng", [128, D], F32), sb("lnb", [128, D], F32)]
        wr = sb("wr", [128, 8, 36], F32)
        brb = sb("brb", [128, 36], F32)
        ident = sb("ident", [128, 128], F32)
        ones = sb("ones", [128, 128], F32)
        ct = [sb("ct%d" % i, [128, 8, 128], BF16) for i in range(2)]
        xt = [sb("xt%d" % i, [128, D], F32) for i in range(2)]
        x1f = [sb("x1f%d" % i, [128, 8, 128], F32) for i in range(2)]
        hT = [sb("hT%d" % i, [128, 4, 512], BF16) for i in range(2)]
        sg = [sb("sg%d" % i, [128, 512], F32) for i in range(2)]
        sm = [sb("sm%d" % i, [128, 256], F32) for i in range(2)]
        pb = [st.enter_context(nc.psum_tensor("pb%d" % i, [128, 512], F32)) for i in range(8)]

        P = Prog(nc)
        P.op("pool", lambda e: e.memset(ones[:], 1.0), writes=["ones"])
        P.op("pool", lambda e: e.affine_select(out=ident[:], in_=ones[:], pattern=[[-1, 128]],
                                               compare_op=ALU.is_equal, fill=0.0, base=0,
                                               channel_multiplier=1),
             reads=["ones"], writes=["ident"])
        wout_v = wbuf[0][:, 0:8192].rearrange("p (k n) -> p k n", k=8)
        P.dma("pool", "wg0", wout_v, wout_h.ap().rearrange("(k p) n -> p k n", p=128), writes=["w0g", "w0u"])
        P.dma("sp", "c_lng", gb[0][:], _bcast_rows(lnp_h, 0, D), writes=["lng"])
        P.dma("sp", "c_lnb", gb[1][:], _bcast_rows(lnp_h, 1, D), writes=["lnb"])
        P.dma("sp", "c_wr", wr[:], wr_h.ap().rearrange("(k p) n -> p k n", p=128), writes=["wr"])
        P.dma("sp", "c_brb", brb[:], _bcast_rows(br_h, 0, 36), writes=["brb"])

        def layer_norm(src_ap_fn, srckey, s, dst_ap, dstkey, alpha):
            smt = sm[s]; k = "sm%d" % s
            stats = smt[:, 0:12]; mv = smt[:, 12:14]; rs = smt[:, 14:15]
            P.op("dve", lambda e: e.bn_stats(out=smt[:, 0:6], in_=src_ap_fn(0, 512)), reads=[srckey], writes=[k + "a"])
            P.op("dve", lambda e: e.bn_stats(out=smt[:, 6:12], in_=src_ap_fn(512, 1024)), reads=[srckey], writes=[k + "b"])
            P.op("dve", lambda e: e.bn_aggr(out=mv, in_=stats), reads=[k + "a", k + "b"], writes=[k + "mv"])
            P.op("act", lambda e: e.activation(out=rs, in_=smt[:, 13:14], func=AF.Sqrt, bias=LN_EPS,
                                               scale=alpha * alpha), reads=[k + "mv"], writes=[k + "rs"])
            P.op("dve", lambda e: e.reciprocal(rs, rs), reads=[k + "rs"], writes=[k + "rs"])
            if alpha != 1.0:
                P.op("dve", lambda e: e.tensor_scalar(rs, rs, alpha, None, ALU.mult), reads=[k + "rs"], writes=[k + "rs"])
            P.op("dve", lambda e: e.tensor_scalar(dst_ap, src_ap_fn(0, 1024), smt[:, 12:13], rs, ALU.subtract, ALU.mult),
                 reads=[srckey, k + "mv", k + "rs"], writes=[dstkey])
            P.op("dve", lambda e: e.tensor_tensor(dst_ap, dst_ap, gb[0][:], ALU.mult), reads=[dstkey, "lng"], writes=[dstkey])
            P.op("dve", lambda e: e.tensor_tensor(dst_ap, dst_ap, gb[1][:], ALU.add), reads=[dstkey, "lnb"], writes=[dstkey])

        for i in range(NT):
            s = i % 2
            cts = ct[s]; xts = xt[s]; x1fs = x1f[s]; smt = sm[s]
            tsl = slice(i * 128, (i + 1) * 128)
            P.dma("pool", "ct%d" % s, cts[:], catT.rearrange("(k p) t -> p k t", p=128)[:, :, tsl], writes=["ct%d" % s])
            P.dma("sp", "xt%d" % s, xts[:], xres[tsl, :], writes=["xt%d" % s])
            for h in range(2):
                for kc in range(8):
                    P.op("pe", (lambda e, h=h, kc=kc, cts=cts: e.matmul(pb[h][:], cts[:, kc, :], wout_v[:, kc, h * 512:(h + 1) * 512],
                                                                        start=(kc == 0), stop=(kc == 7))),
                         reads=["ct%d" % s, "w0g", "w0u"], writes=["pb%d" % h])
            for h in range(2):
                P.op("dve", (lambda e, h=h, xts=xts: e.scalar_tensor_tensor(out=xts[:, h * 512:(h + 1) * 512], in0=xts[:, h * 512:(h + 1) * 512],
                                                                            scalar=ALPHA, in1=pb[h][:], op0=ALU.mult, op1=ALU.add)),
                     reads=["xt%d" % s, "pb%d" % h], writes=["xt%d" % s])
            layer_norm(lambda a, b, xts=xts: xts[:, a:b], "xt%d" % s, s, yacc[:, i, :], "yacc%d" % i, 1.0)
            for kc in range(8):
                P.op("pe", (lambda e, kc=kc, i=i: e.transpose(pb[2 + kc // 4][:, (kc % 4) * 128:(kc % 4 + 1) * 128],
                                                              yacc[:, i, kc * 128:(kc + 1) * 128], ident[:])),
                     reads=["yacc%d" % i, "ident"], writes=["pb%d" % (2 + kc // 4)])
            P.op("act", lambda e, x1fs=x1fs: e.copy(out=x1fs[:, 0:4, :], in_=pb[2][:].rearrange("p (k t) -> p k t", k=4)),
                 reads=["pb2"], writes=["x1f%da" % s])
            P.op("dve", lambda e, x1fs=x1fs: e.tensor_copy(out=x1fs[:, 4:8, :], in_=pb[3][:].rearrange("p (k t) -> p k t", k=4)),
                 reads=["pb3"], writes=["x1f%db" % s])
            P.op("act", lambda e, tsl=tsl: e.copy(out=x1T[:, 0:4, tsl], in_=pb[2][:].rearrange("p (k t) -> p k t", k=4)),
                 reads=["pb2"], writes=["x1T_%da" % i])
            P.op("dve", lambda e, tsl=tsl: e.tensor_copy(out=x1T[:, 4:8, tsl], in_=pb[3][:].rearrange("p (k t) -> p k t", k=4)),
                 reads=["pb3"], writes=["x1T_%db" % i])
            for kc in range(8):
                P.op("pe", (lambda e, kc=kc, x1fs=x1fs: e.matmul(pb[4][:, 0:36], x1fs[:, kc, :], wr[:, kc, :], start=(kc == 0), stop=(kc == 7))),
                     reads=["x1f%da" % s, "x1f%db" % s, "wr"], writes=["pb4"])
            k = "r%d" % s
            lg = smt[:, 16:52]; gl = smt[:, 16:20]; el = smt[:, 20:52]
            gmax = smt[:, 52:53]; ngmax = smt[:, 53:54]; gsum = smt[:, 54:55]
            oh = smt[:, 56:60]; pen = smt[:, 60:64]; eg = smt[:, 64:68]
            msk = smt[:, 68:100]; m1 = smt[:, 100:132]; m2 = smt[:, 132:164]; msk2 = smt[:, 164:196]
            top1 = smt[:, 196:197]; top2 = smt[:, 197:198]; dd = smt[:, 198:199]; ee = smt[:, 199:200]
            w1 = smt[:, 200:201]; w2 = smt[:, 201:202]; tmp = smt[:, 204:236]
            P.op("dve", lambda e, lg=lg: e.tensor_tensor(lg, pb[4][:, 0:36], brb[:], ALU.add), reads=["pb4", "brb"], writes=[k])
            P.op("dve", lambda e, gmax=gmax, gl=gl: e.reduce_max(out=gmax, in_=gl, axis=AX.X), reads=[k], writes=[k + "gm"])
            P.op("dve", lambda e, oh=oh, gl=gl, gmax=gmax: e.tensor_scalar(oh, gl, gmax, None, ALU.is_ge), reads=[k, k + "gm"], writes=[k + "oh"])
            P.op("dve", lambda e, ngmax=ngmax, gmax=gmax: e.tensor_scalar(ngmax, gmax, -1.0, None, ALU.mult), reads=[k + "gm"], writes=[k + "ngm"])
            P.op("act", lambda e, eg=eg, gl=gl, ngmax=ngmax, gsum=gsum: e.activation(out=eg, in_=gl, func=AF.Exp, bias=ngmax, scale=1.0, accum_out=gsum),
                 reads=[k, k + "ngm"], writes=[k + "eg", k + "gs"])
            P.op("dve", lambda e, gsum=gsum: e.reciprocal(gsum, gsum), reads=[k + "gs"], writes=[k + "gs"])
            P.op("dve", lambda e, pen=pen, oh=oh: e.tensor_scalar(pen, oh, 1.0, BIG, ALU.subtract, ALU.mult), reads=[k + "oh"], writes=[k + "pen"])
            P.op("dve", lambda e, msk=msk, el=el, pen=pen: e.tensor_tensor(msk.rearrange("p (g j) -> p g j", g=4), el.rearrange("p (g j) -> p g j", g=4),
                                                                          pen.unsqueeze(2).to_broadcast([128, 4, 8]), ALU.add),
                 reads=[k, k + "pen"], writes=[k + "msk"])
            P.op("dve", lambda e, top1=top1, msk=msk: e.reduce_max(out=top1, in_=msk, axis=AX.X), reads=[k + "msk"], writes=[k + "t1"])
            P.op("dve", lambda e, m1=m1, msk=msk, top1=top1: e.tensor_scalar(m1, msk, top1, None, ALU.is_ge), reads=[k + "msk", k + "t1"], writes=[k + "m1"])
            P.op("dve", lambda e, msk2=msk2, m1=m1, msk=msk: e.scalar_tensor_tensor(out=msk2, in0=m1, scalar=-BIG, in1=msk, op0=ALU.mult, op1=ALU.add),
                 reads=[k + "m1", k + "msk"], writes=[k + "msk2"])
            P.op("dve", lambda e, top2=top2, msk2=msk2: e.reduce_max(out=top2, in_=msk2, axis=AX.X), reads=[k + "msk2"], writes=[k + "t2"])
            P.op("dve", lambda e, m2=m2, msk2=msk2, top2=top2: e.tensor_scalar(m2, msk2, top2, None, ALU.is_ge), reads=[k + "msk2", k + "t2"], writes=[k + "m2"])
            P.op("dve", lambda e, dd=dd, top2=top2, top1=top1: e.tensor_tensor(dd, top2, top1, ALU.subtract), reads=[k + "t1", k + "t2"], writes=[k + "dd"])
            P.op("act", lambda e, ee=ee, dd=dd: e.activation(out=ee, in_=dd, func=AF.Exp), reads=[k + "dd"], writes=[k + "ee"])
            P.op("dve", lambda e, w1=w1, ee=ee: e.tensor_scalar(w1, ee, 1.0, None, ALU.add), reads=[k + "ee"], writes=[k + "w1"])
            P.op("dve", lambda e, w1=w1: e.reciprocal(w1, w1), reads=[k + "w1"], writes=[k + "w1"])
            P.op("dve", lambda e, w1=w1, gsum=gsum: e.tensor_scalar(w1, w1, gsum, 1.0 / ALPHA, ALU.mult, ALU.mult), reads=[k + "w1", k + "gs"], writes=[k + "w1"])
            P.op("dve", lambda e, w2=w2, ee=ee, w1=w1: e.tensor_tensor(w2, ee, w1, ALU.mult), reads=[k + "ee", k + "w1"], writes=[k + "w2"])
            P.op("dve", lambda e, tmp=tmp, m1=m1, w1=w1: e.tensor_scalar(tmp, m1, w1, None, ALU.mult), reads=[k + "m1", k + "w1"], writes=[k + "tmp"])
            P.op("dve", lambda e, i=i, m2=m2, w2=w2, tmp=tmp: e.scalar_tensor_tensor(out=G[:, i, :], in0=m2, scalar=w2, in1=tmp, op0=ALU.mult, op1=ALU.add),
                 reads=[k + "m2", k + "w2", k + "tmp"], writes=["G%d" % i])

        P.dma("sp", "c_lng", gb[0][:], _bcast_rows(lnp_h, 2, D), writes=["lng"])
        P.dma("sp", "c_lnb", gb[1][:], _bcast_rows(lnp_h, 3, D), writes=["lnb"])

        def load_expert(e_):
            s = e_ % 2
            wb = wbuf[s]
            P.dma("pool", "wg%d" % s, wb[:, 0:4096].rearrange("p (k n) -> p k n", k=8),
                  wg_h.ap()[e_].rearrange("(k p) n -> p k n", p=128), writes=["w%dg" % s])
            P.dma("pool", "wu%d" % s, wb[:, 4096:8192].rearrange("p (k n) -> p k n", k=8),
                  wu_h.ap()[e_].rearrange("(k p) n -> p k n", p=128), writes=["w%du" % s])
            P.dma("pool", "wd%d" % s, wb[:, 8192:12288].rearrange("p (k n) -> p k n", k=4),
                  wd_h.ap()[e_].rearrange("(k p) n -> p k n", p=128), writes=["w%dd" % s])

        load_expert(0)
        hcnt = 0
        for e_ in range(NE):
            s = e_ % 2
            wb = wbuf[s]
            wgv = wb[:, 0:4096].rearrange("p (k n) -> p k n", k=8)
            wuv = wb[:, 4096:8192].rearrange("p (k n) -> p k n", k=8)
            wdv = wb[:, 8192:12288].rearrange("p (k n) -> p k n", k=4)
            if e_ + 1 < NE:
                load_expert(e_ + 1)
            for tb in range(NB):
                hs = hcnt % 2; hcnt += 1
                hTs = hT[hs]
                tkeys = []
                for j in range(4):
                    tkeys += ["x1T_%da" % (tb * 4 + j), "x1T_%db" % (tb * 4 + j)]
                for c in range(4):
                    pg = pb[(c % 2) * 2]; pu = pb[(c % 2) * 2 + 1]
                    kg = "pb%d" % ((c % 2) * 2); ku = "pb%d" % ((c % 2) * 2 + 1)
                    for kc in range(8):
                        P.op("pe", (lambda e, pg=pg, kc=kc, c=c, wgv=wgv, tb=tb: e.matmul(pg[:], wgv[:, kc, c * 128:(c + 1) * 128],
                                                                                          x1T[:, kc, tb * 512:(tb + 1) * 512], start=(kc == 0), stop=(kc == 7))),
                             reads=["w%dg" % s] + tkeys, writes=[kg])
                    for kc in range(8):
                        P.op("pe", (lambda e, pu=pu, kc=kc, c=c, wuv=wuv, tb=tb: e.matmul(pu[:], wuv[:, kc, c * 128:(c + 1) * 128],
                                                                                          x1T[:, kc, tb * 512:(tb + 1) * 512], start=(kc == 0), stop=(kc == 7))),
                             reads=["w%du" % s] + tkeys, writes=[ku])
                    sgs = sg[c % 2]
                    P.op("act", lambda e, sgs=sgs, pg=pg: e.activation(out=sgs[:], in_=pg[:], func=AF.Silu), reads=[kg], writes=["sg%d" % (c % 2)])
                    P.op("dve", lambda e, hTs=hTs, c=c, sgs=sgs, pu=pu: e.tensor_tensor(hTs[:, c, :], sgs[:], pu[:], ALU.mult),
                         reads=["sg%d" % (c % 2), ku], writes=["hT%d_%d" % (hs, c)])
                for tt in range(4):
                    ti = tb * 4 + tt
                    for dh in range(2):
                        py = pb[4 + (tt * 2 + dh) % 4]; ky = "pb%d" % (4 + (tt * 2 + dh) % 4)
                        for c in range(4):
                            P.op("pe", (lambda e, py=py, c=c, tt=tt, dh=dh, hTs=hTs, wdv=wdv: e.matmul(py[:], hTs[:, c, tt * 128:(tt + 1) * 128],
                                                                                                 wdv[:, c, dh * 512:(dh + 1) * 512], start=(c == 0), stop=(c == 3))),
                                 reads=["hT%d_%d" % (hs, c) for c in range(4)] + ["w%dd" % s], writes=[ky])
                        P.op("dve", (lambda e, py=py, ti=ti, dh=dh, e_=e_: e.scalar_tensor_tensor(out=yacc[:, ti, dh * 512:(dh + 1) * 512], in0=py[:],
                                                                                                scalar=G[:, ti, e_:e_ + 1], in1=yacc[:, ti, dh * 512:(dh + 1) * 512],
                                                                                                op0=ALU.mult, op1=ALU.add)),
                             reads=[ky, "G%d" % ti, "yacc%d" % ti], writes=["yacc%d" % ti])

        for i in range(NT):
            s = i % 2
            xts = xt[s]
            layer_norm(lambda a, b, i=i: yacc[:, i, a:b], "yacc%d" % i, s, xts[:], "xt%d" % s, ALPHA)
            P.dma("sp", "out%d" % s, xo[i * 128:(i + 1) * 128, :], xts[:], reads=["xt%d" % s], writes=["xo%d" % i])
        P.emit()
    return nc


def _mk_consts(nc, P, sb):
    C = {}
    C["ones"] = sb("c_ones", [128, 128], F32)
    C["ident"] = sb("c_ident", [128, 128], F32)
    C["tri_le"] = sb("c_tri_le", [128, 128], F32)
    C["tri_ge"] = sb("c_tri_ge", [128, 128], F32)
    C["tri_gt"] = sb("c_tri_gt", [128, 128], F32)
    C["tri_lt"] = sb("c_tri_lt", [128, 128], F32)
    C["ones_bf"] = sb("c_ones_bf", [128, 128], BF16)
    ones = C["ones"]
    P.op("pool", lambda e: e.memset(ones[:], 1.0), writes=["c_ones"])
    P.op("pool", lambda e: e.memset(C["ones_bf"][:], 1.0), writes=["c_ones_bf"])

    def sel(name, step, cm, cmp):
        t = C[name]
        P.op("pool", lambda e: e.affine_select(out=t[:], in_=ones[:], pattern=[[step, 128]], compare_op=cmp,
                                               fill=0.0, base=0, channel_multiplier=cm),
             reads=["c_ones"], writes=["c_" + name])
    sel("ident", -1, 1, ALU.is_equal)
    sel("tri_le", 1, -1, ALU.is_ge)
    sel("tri_ge", -1, 1, ALU.is_ge)
    sel("tri_gt", -1, 1, ALU.is_gt)
    sel("tri_lt", 1, -1, ALU.is_gt)
    return C


def _scan_factors(P, C, ps, pskey, gf, gfkey, gb, gbkey, dk, F, fkey):
    P.op("pe", lambda e: e.matmul(ps[0:dk, 0:128], gf, C["tri_le"][:], start=True, stop=True),
         reads=[gfkey, "c_tri_le"], writes=[pskey + "a"])
    P.op("pe", lambda e: e.matmul(ps[0:dk, 128:256], gb, C["tri_ge"][:], start=True, stop=True),
         reads=[gbkey, "c_tri_ge"], writes=[pskey + "b"])
    P.op("pe", lambda e: e.matmul(ps[:, 256:256 + dk], C["tri_gt"][:], gf, start=True, stop=True),
         reads=[gfkey, "c_tri_gt"], writes=[pskey + "c"])
    P.op("pe", lambda e: e.matmul(ps[:, 384:384 + dk], C["tri_lt"][:], gb, start=True, stop=True),
         reads=[gbkey, "c_tri_lt"], writes=[pskey + "d"])
    P.op("act", lambda e: e.activation(out=F["E1f"], in_=ps[0:dk, 0:128], func=AF.Exp), reads=[pskey + "a"], writes=[fkey + "E1f"])
    P.op("act", lambda e: e.activation(out=F["E2f"], in_=ps[0:dk, 0:128], func=AF.Exp, scale=-1.0), reads=[pskey + "a"], writes=[fkey + "E2f"])
    P.op("act", lambda e: e.activation(out=F["E1b"], in_=ps[0:dk, 128:256], func=AF.Exp), reads=[pskey + "b"], writes=[fkey + "E1b"])
    P.op("act", lambda e: e.activation(out=F["E2b"], in_=ps[0:dk, 128:256], func=AF.Exp, scale=-1.0), reads=[pskey + "b"], writes=[fkey + "E2b"])
    P.op("act", lambda e: e.activation(out=F["E3f"], in_=ps[:, 256:256 + dk], func=AF.Exp), reads=[pskey + "c"], writes=[fkey + "E3f"])
    P.op("act", lambda e: e.activation(out=F["E3b"], in_=ps[:, 384:384 + dk], func=AF.Exp), reads=[pskey + "d"], writes=[fkey + "E3b"])


def build_mix0(T=8192, stage=99):
    nc = bass.Bass("TRN2", target_bir_lowering=False)
    NBK = T // 512
    NCH = T // 128
    D = 1024
    NW = 11 * 128
    xT_h = nc.dram_tensor("xT", [D, T], F32, kind="ExternalInput")
    wA_h = nc.dram_tensor("wA", [D, NW], F32, kind="ExternalInput")
    tabs_h = nc.dram_tensor("tabs", [4, 128, T], F32, kind="ExternalInput")
    dec_h = nc.dram_tensor("dec", [1, 2], F32, kind="ExternalInput")
    lv_h = nc.dram_tensor("lv", [4, 64], F32, kind="ExternalInput")
    subln_h = nc.dram_tensor("subln", [1, 128], F32, kind="ExternalInput")
    o_h = nc.dram_tensor("o", [T, 256], F32, kind="ExternalOutput")
    xT = xT_h.ap().rearrange("(k p) t -> p k t", p=128)
    tabs = tabs_h.ap().rearrange("f p t -> p f t")
    oo = o_h.ap()
    KSC = 128.0 ** -0.5
    LAM_INIT = 0.8 - 0.6 * math.exp(-0.3 * 0)
    SCL = 64.0 ** -0.5

    with contextlib.ExitStack() as st:
        def sb(name, shape, dt):
            return st.enter_context(nc.sbuf_tensor("s_" + name, shape, dt))
        P = Prog(nc)
        C = _mk_consts(nc, P, sb)
        wA = sb("wA", [128, 8, NW], BF16)
        xb = [sb("xb%d" % i, [128, 8, 512], BF16) for i in range(2)]
        tab = [sb("tab%d" % i, [128, 4, 512], F32) for i in range(2)]
        KT_r = sb("KT_r", [128, T], BF16)
        Ktok_r = sb("Ktok_r", [128, NCH, 128], BF16)
        V_r = sb("V_r", [128, NCH, 128], BF16)
        Rst = sb("Rst", [128, NCH, 128], BF16)
        KT_d = sb("KT_d", [128, T], BF16)
        V_d = sb("V_d", [128, NCH, 130], BF16)
        fac = sb("fac", [128, 6, 128], F32)
        Gc = sb("Gc", [128, 2, 128], F32)
        sc = sb("sc", [128, 64], F32)
        lvb = sb("lvb", [128, 4, 64], F32)
        sublnb = sb("sublnb", [128, 128], F32)
        Rf = sb("Rf", [128, 128], F32)
        Sf = sb("Sf", [128, 128], F32)
        S_bf = sb("S_bf", [128, 128], BF16)
        t1 = [sb("t1_%d" % i, [128, 512], F32) for i in range(2)]
        t2 = [sb("t2_%d" % i, [128, 512], F32) for i in range(2)]
        ktf = sb("ktf", [128, 512], F32)
        sq = sb("sq", [128, 512], BF16)
        QTd = [sb("QTd%d" % i, [128, 512], BF16) for i in range(2)]
        gate = [sb("gate%d" % i, [128, 4, 128], F32) for i in range(2)]
        outt = [sb("outt%d" % i, [128, 4, 256], F32) for i in range(2)]
        cw = [sb("cw%d" % i, [128, 6, 128], BF16) for i in range(2)]
        pm = [sb("pm%d" % i, [128, 2, 128], BF16) for i in range(2)]
        pT = [sb("pT%d" % i, [128, 512], BF16) for i in range(4)]
        od = [sb("od%d" % i, [128, 128], F32) for i in range(2)]
        smx = [sb("smx%d" % i, [128, 32], F32) for i in range(2)]
        pb = [st.enter_context(nc.psum_tensor("pb%d" % i, [128, 512], F32)) for i in range(8)]

        if stage == -3:
            P.emit(); return nc
        P.dma("pool", "wA", wA[:], wA_h.ap().rearrange("(k p) n -> p k n", p=128), writes=["wA"])
        P.dma("sp", "c_dec", sc[:, 0:2], _bcast_rows(dec_h, 0, 2), writes=["dec"])
        P.dma("sp", "c_lv", lvb[:], bass.AP(lv_h, 0, [[0, 128], [1, 256]]), writes=["lvb"])
        P.dma("sp", "c_subln", sublnb[:], _bcast_rows(subln_h, 0, 128), writes=["sublnb"])
        if stage == -2:
            P.emit(); return nc
        P.op("act", lambda e: e.activation(out=sc[:, 2:4], in_=sc[:, 0:2], func=AF.Exp), reads=["dec"], writes=["la"])
        P.op("dve", lambda e: e.tensor_scalar(sc[:, 2:4], sc[:, 2:4], -1.0, None, ALU.mult), reads=["la"], writes=["la"])
        for d_ in range(2):
            P.op("dve", lambda e, d_=d_: e.tensor_scalar(Gc[:, d_, :], C["ones"][:], sc[:, 2 + d_:3 + d_], None, ALU.mult),
                 reads=["la", "c_ones"], writes=["Gc%d" % d_])
        F = {"E1f": fac[:, 0, :], "E2f": fac[:, 1, :], "E1b": fac[:, 2, :], "E2b": fac[:, 3, :], "E3f": fac[:, 4, :], "E3b": fac[:, 5, :]}
        _scan_factors(P, C, pb[7], "pb7", Gc[:, 0, :], "Gc0", Gc[:, 1, :], "Gc1", 128, F, "fac")
        FK = ["fac" + k for k in ("E1f", "E2f", "E1b", "E2b", "E3f", "E3b")]
        dSf = fac[:, 0, 127:128]
        dSb = fac[:, 2, 0:1]
        if stage == -1:
            P.emit(); return nc
        P.op("dve", lambda e: e.tensor_tensor(lvb[:, 0, :], lvb[:, 0, :], lvb[:, 1, :], ALU.mult), reads=["lvb"], writes=["lvb"])
        P.op("dve", lambda e: e.tensor_tensor(lvb[:, 2, :], lvb[:, 2, :], lvb[:, 3, :], ALU.mult), reads=["lvb"], writes=["lvb"])
        P.op("dve", lambda e: e.reduce_sum(out=sc[:, 4:5], in_=lvb[:, 0, :], axis=AX.X), reads=["lvb"], writes=["lam_a"])
        P.op("dve", lambda e: e.reduce_sum(out=sc[:, 5:6], in_=lvb[:, 2, :], axis=AX.X), reads=["lvb"], writes=["lam_b"])
        P.op("act", lambda e: e.activation(out=sc[:, 6:8], in_=sc[:, 4:6], func=AF.Exp), reads=["lam_a", "lam_b"], writes=["lam_e"])
        P.op("dve", lambda e: e.tensor_tensor(sc[:, 8:9], sc[:, 6:7], sc[:, 7:8], ALU.subtract), reads=["lam_e"], writes=["lam"])
        P.op("dve", lambda e: e.tensor_scalar(sc[:, 9:10], sc[:, 8:9], LAM_INIT, -1.0, ALU.add, ALU.mult), reads=["lam"], writes=["nlam"])
        P.op("dve", lambda e: e.tensor_scalar(sublnb[:], sublnb[:], 1.0 - LAM_INIT, None, ALU.mult), reads=["sublnb"], writes=["sublnb"])
        P.op("pool", lambda e: e.memset(Rf[:], 0.0), writes=["Rf"])
        P.op("pool", lambda e: e.memset(Sf[:], 0.0), writes=["Sf"])
        P.op("pool", lambda e: e.memset(S_bf[:], 0.0), writes=["S_bf"])
        P.op("pool", lambda e: e.memset(sc[:, 10:11], 0.0), writes=["kmax2"])
        P.op("pool", lambda e: e.memset(V_d[:, :, 128:130], 1.0), writes=["V_d_ones"])

        def load_block(tb, s):
            P.dma("pool", "xb%d" % s, xb[s][:], xT[:, :, tb * 512:(tb + 1) * 512], writes=["xb%d" % s])
            P.dma("sp", "tab%d" % s, tab[s][:], tabs[:, :, tb * 512:(tb + 1) * 512], writes=["tab%d" % s])

        def proj_fm(blk, bank, s):
            for kc in range(8):
                P.op("pe", (lambda e, kc=kc: e.matmul(pb[bank][:], wA[:, kc, blk * 128:(blk + 1) * 128], xb[s][:, kc, :],
                                                      start=(kc == 0), stop=(kc == 7))),
                     reads=["wA", "xb%d" % s], writes=["pb%d" % bank])

        def rotary(bx, bp, s, fc, fs, dst, dstkey, slot):
            a = t1[slot]; b = t2[slot]
            P.op("dve", lambda e: e.tensor_tensor(a[:], pb[bp][:], tab[s][:, fs, :], ALU.mult), reads=["pb%d" % bp, "tab%d" % s], writes=["t1_%d" % slot])
            P.op("dve", lambda e: e.tensor_tensor(b[:], pb[bx][:], tab[s][:, fc, :], ALU.mult), reads=["pb%d" % bx, "tab%d" % s], writes=["t2_%d" % slot])
            P.op("pool", lambda e: e.tensor_tensor(dst, a[:], b[:], ALU.add), reads=["t1_%d" % slot, "t2_%d" % slot], writes=[dstkey])

        def sumsq_max(src, srckey, bank, dst, dstkey):
            P.op("pool", lambda e: e.tensor_tensor(sq[:], src, src, ALU.mult), reads=[srckey], writes=["sq"])
            P.op("pe", lambda e: e.matmul(pb[bank][:], C["ones_bf"][:], sq[:], start=True, stop=True), reads=["sq", "c_ones_bf"], writes=["pb%d" % bank])
            P.op("dve", lambda e: e.reduce_max(out=dst, in_=pb[bank][:], axis=AX.X), reads=["pb%d" % bank], writes=[dstkey])

        def mm(out, lhsT, rhs, start, stop, reads, writes, skip=False):
            P.op("pe", lambda e: e.matmul(out, lhsT, rhs, start=start, stop=stop, skip_group_check=skip), reads=reads, writes=writes)

        def p1_values(tb, s, tt):
            bank = 4 + tt // 2
            o0 = (tt % 2) * 256
            key = "pb%d_%d" % (bank, tt % 2)
            for kc in range(8):
                mm(pb[bank][:, o0:o0 + 256], xb[s][:, kc, tt * 128:(tt + 1) * 128], wA[:, kc, 8 * 128:10 * 128], kc == 0, kc == 7,
                   ["wA", "xb%d" % s], [key])
            ci = tb * 4 + tt
            P.op("act", lambda e: e.copy(out=V_r[:, ci, :], in_=pb[bank][:, o0:o0 + 128]), reads=[key], writes=["V_r%d" % ci])
            P.op("dve", lambda e: e.tensor_copy(out=V_d[:, ci, 0:128], in_=pb[bank][:, o0 + 128:o0 + 256]), reads=[key], writes=["V_d%d" % ci])

        def p1_chunk(ci):
            cs = ci % 2
            P.op("act", lambda e: e.copy(out=Rst[:, ci, :], in_=Rf[:]), reads=["Rf"], writes=["Rst%d" % ci])
            P.op("pool", lambda e: e.tensor_tensor(cw[cs][:, 4, :], Ktok_r[:, ci, :], F["E3b"], ALU.mult),
                 reads=["Ktok_r%d" % (ci // 4), "facE3b"], writes=["cw%d_4" % cs])
            mm(pb[7][:, 0:128], cw[cs][:, 4, :], V_r[:, ci, :], True, True, ["cw%d_4" % cs, "V_r%d" % ci], ["pb7u"])
            P.op("dve", lambda e: e.scalar_tensor_tensor(out=Rf[:], in0=Rf[:], scalar=dSb, in1=pb[7][:, 0:128], op0=ALU.mult, op1=ALU.add),
                 reads=["Rf", "pb7u", "facE1b"], writes=["Rf"])

        def p1_block(n_, tb):
            s = n_ % 2
            bsl = slice(tb * 512, (tb + 1) * 512)
            load_block(tb, s)
            proj_fm(2, 0, s); proj_fm(3, 1, s); proj_fm(6, 2, s); proj_fm(7, 3, s)
            rotary(0, 1, s, 0, 1, ktf[:], "ktf", 0)
            P.op("act", lambda e: e.copy(out=KT_r[:, bsl], in_=ktf[:]), reads=["ktf"], writes=["KT_r%d" % tb])
            for j in range(4):
                P.op("pe", lambda e, j=j: e.transpose(pb[6][:, j * 128:(j + 1) * 128], ktf[:, j * 128:(j + 1) * 128], C["ident"][:]),
                     reads=["ktf", "c_ident"], writes=["pb6"])
            P.op("act", lambda e: e.copy(out=Ktok_r[:, tb * 4:(tb + 1) * 4, :], in_=pb[6][:].rearrange("p (j d) -> p j d", j=4)),
                 reads=["pb6"], writes=["Ktok_r%d" % tb])
            rotary(2, 3, s, 2, 3, KT_d[:, bsl], "KT_d%d" % tb, 1)
            sumsq_max(KT_d[:, bsl], "KT_d%d" % tb, 2, sc[:, 11:12], "ktmp")
            P.op("dve", lambda e: e.tensor_tensor(sc[:, 10:11], sc[:, 10:11], sc[:, 11:12], ALU.max), reads=["kmax2", "ktmp"], writes=["kmax2"])
            for tt in range(4):
                p1_values(tb, s, tt)
            for ci in range(tb * 4 + 3, tb * 4 - 1, -1):
                p1_chunk(ci)

        for n_, tb in enumerate(range(NBK - 1, -1, -1) if stage >= 1 else []):
            p1_block(n_, tb)

        def p2_gate_tt(s, tt):
            for kc in range(8):
                mm(pb[3][:, tt * 128:(tt + 1) * 128], xb[s][:, kc, tt * 128:(tt + 1) * 128], wA[:, kc, 10 * 128:11 * 128], kc == 0, kc == 7,
                   ["wA", "xb%d" % s], ["pb3"])

        def p2_ret_chunk(tb, s, j):
            ci = tb * 4 + j
            cs = ci % 2
            csl = slice(j * 128, (j + 1) * 128)
            gsl = slice(ci * 128, (ci + 1) * 128)
            cwk = "cw%d_" % cs
            c = cw[cs]
            P.op("dve", lambda e: e.scalar_tensor_tensor(out=c[:, 0, :], in0=ktf[:, csl], scalar=KSC, in1=F["E1f"], op0=ALU.mult, op1=ALU.mult),
                 reads=["ktf", "facE1f"], writes=[cwk + "0"])
            P.op("dve", lambda e: e.scalar_tensor_tensor(out=c[:, 1, :], in0=ktf[:, csl], scalar=KSC, in1=F["E1b"], op0=ALU.mult, op1=ALU.mult),
                 reads=["ktf", "facE1b"], writes=[cwk + "1"])
            P.op("dve", lambda e: e.tensor_tensor(c[:, 2, :], KT_r[:, gsl], F["E2f"], ALU.mult), reads=["KT_r%d" % tb, "facE2f"], writes=[cwk + "2"])
            P.op("pool", lambda e: e.tensor_tensor(c[:, 3, :], KT_r[:, gsl], F["E2b"], ALU.mult), reads=["KT_r%d" % tb, "facE2b"], writes=[cwk + "3"])
            mm(pb[2][:, 0:128], c[:, 2, :], c[:, 0, :], True, True, [cwk + "2", cwk + "0"], ["pb2a"])
            mm(pb[2][:, 128:256], c[:, 3, :], c[:, 1, :], True, True, [cwk + "3", cwk + "1"], ["pb2b"])
            pmc = pm[cs]
            P.op("dve", lambda e: e.tensor_tensor(pmc[:, 0, :], pb[2][:, 0:128], C["tri_le"][:], ALU.mult), reads=["pb2a", "c_tri_le"], writes=["pm%d_0" % cs])
            P.op("dve", lambda e: e.tensor_tensor(pmc[:, 1, :], pb[2][:, 128:256], C["tri_gt"][:], ALU.mult), reads=["pb2b", "c_tri_gt"], writes=["pm%d_1" % cs])
            oreg = pb[2][:, 256:384]
            mm(oreg, pmc[:, 0, :], V_r[:, ci, :], True, False, ["pm%d_0" % cs, "V_r%d" % ci], ["pb2o"])
            mm(oreg, pmc[:, 1, :], V_r[:, ci, :], False, False, ["pm%d_1" % cs, "V_r%d" % ci], ["pb2o"])
            mm(oreg, c[:, 0, :], S_bf[:], False, False, [cwk + "0", "S_bf"], ["pb2o"])
            mm(oreg, c[:, 1, :], Rst[:, ci, :], False, True, [cwk + "1", "Rst%d" % ci], ["pb2o"])
            P.op("pool", lambda e: e.tensor_tensor(c[:, 4, :], Ktok_r[:, ci, :], F["E3f"], ALU.mult), reads=["Ktok_r%d" % tb, "facE3f"], writes=[cwk + "4"])
            mm(pb[2][:, 384:512], c[:, 4, :], V_r[:, ci, :], True, True, [cwk + "4", "V_r%d" % ci], ["pb2u"])
            P.op("dve", lambda e: e.scalar_tensor_tensor(out=Sf[:], in0=Sf[:], scalar=dSf, in1=pb[2][:, 384:512], op0=ALU.mult, op1=ALU.add),
                 reads=["Sf", "pb2u", "facE1f"], writes=["Sf"])
            P.op("act", lambda e: e.copy(out=S_bf[:], in_=Sf[:]), reads=["Sf"], writes=["S_bf"])
            sx = smx[s]; xk = "gn%d" % s
            odc = od[cs]
            P.op("dve", lambda e: e.bn_stats(out=sx[:, 8:14], in_=oreg), reads=["pb2o"], writes=[xk + "st"])
            P.op("dve", lambda e: e.bn_aggr(out=sx[:, 14:16], in_=sx[:, 8:14]), reads=[xk + "st"], writes=[xk + "mv"])
            P.op("act", lambda e: e.activation(out=sx[:, 16:17], in_=sx[:, 15:16], func=AF.Sqrt, bias=LN_EPS, scale=1.0), reads=[xk + "mv"], writes=[xk + "rs"])
            P.op("dve", lambda e: e.reciprocal(sx[:, 16:17], sx[:, 16:17]), reads=[xk + "rs"], writes=[xk + "rs"])
            P.op("dve", lambda e: e.tensor_scalar(odc[:], oreg, sx[:, 14:15], sx[:, 16:17], ALU.subtract, ALU.mult),
                 reads=["pb2o", xk + "mv", xk + "rs"], writes=["od%d" % cs])
            P.op("pool", lambda e: e.tensor_tensor(outt[s][:, j, 0:128], odc[:], gate[s][:, j, :], ALU.mult),
                 reads=["od%d" % cs, "gate%d" % s], writes=["outt%d_r%d" % (s, j)])

        def p2_attn_qk(s, comp, kt, i):
            rows = slice(comp * 64, comp * 64 + 64)
            sbank = i % 2
            psl = i % 4
            negc = smx[s][:, 3:4]
            mm(pb[sbank][:], KT_d[rows, kt * 128:(kt + 1) * 128], QTd[s][rows, :], True, True, ["KT_d%d" % (kt // 4), "QTd%d" % s], ["pb%d" % sbank])
            P.op("act", lambda e: e.activation(out=pT[psl][:], in_=pb[sbank][:], func=AF.Exp, bias=negc, scale=SCL),
                 reads=["pb%d" % sbank, "negc%d" % s], writes=["pT%d" % psl])

        def p2_attn_pv(s, comp, kt, i):
            psl = i % 4
            for qt in range(4):
                bank = 4 + comp * 2 + qt // 2
                areg = pb[bank][:, (qt % 2) * 256:(qt % 2) * 256 + 129]
                mm(areg, pT[psl][:, qt * 128:(qt + 1) * 128], V_d[:, kt, 0:129], kt == 0 and qt % 2 == 0, kt == NCH - 1,
                   ["pT%d" % psl, "V_d%d" % kt, "V_d_ones"], ["pb%d_acc%d" % (bank, qt)], skip=True)

        def p2_attention(s, cap):
            steps = [(comp, kt) for comp in range(2) for kt in range(NCH)]
            per = 0 if not cap else -(-len(cap) // max(1, len(steps) - 4))
            for i in range(min(2, len(steps))):
                p2_attn_qk(s, steps[i][0], steps[i][1], i)
            for i, (comp, kt) in enumerate(steps):
                p2_attn_pv(s, comp, kt, i)
                if i + 2 < len(steps):
                    p2_attn_qk(s, steps[i + 2][0], steps[i + 2][1], i + 2)
                if cap:
                    P.replay(cap, per)

        def p2_attn_epi(s, qt):
            a0 = pb[4 + qt // 2][:, (qt % 2) * 256:(qt % 2) * 256 + 129]
            a1 = pb[6 + qt // 2][:, (qt % 2) * 256:(qt % 2) * 256 + 129]
            k0 = "pb%d_acc%d" % (4 + qt // 2, qt); k1 = "pb%d_acc%d" % (6 + qt // 2, qt)
            sx = smx[s]; xk = "da%d" % s; cs = qt % 2
            odc = od[cs]
            P.op("dve", lambda e: e.reciprocal(sx[:, 20:21], a0[:, 128:129]), reads=[k0], writes=[xk + "z0"])
            P.op("dve", lambda e: e.reciprocal(sx[:, 21:22], a1[:, 128:129]), reads=[k1], writes=[xk + "z1"])
            P.op("dve", lambda e: e.tensor_tensor(sx[:, 21:22], sx[:, 21:22], sc[:, 9:10], ALU.mult), reads=[xk + "z1", "nlam"], writes=[xk + "z1"])
            P.op("dve", lambda e: e.tensor_scalar(odc[:], a0[:, 0:128], sx[:, 20:21], None, ALU.mult), reads=[k0, xk + "z0"], writes=["od%d" % cs])
            P.op("dve", lambda e: e.scalar_tensor_tensor(out=odc[:], in0=a1[:, 0:128], scalar=sx[:, 21:22], in1=odc[:], op0=ALU.mult, op1=ALU.add),
                 reads=[k1, xk + "z1", "od%d" % cs], writes=["od%d" % cs])
            P.op("act", lambda e: e.activation(out=t1[0][:, 0:128], in_=odc[:], func=AF.Square, accum_out=sx[:, 22:23]),
                 reads=["od%d" % cs], writes=["t1_0", xk + "ss"])
            P.op("act", lambda e: e.activation(out=sx[:, 23:24], in_=sx[:, 22:23], func=AF.Sqrt, bias=1e-6, scale=1.0 / 128.0), reads=[xk + "ss"], writes=[xk + "rs"])
            P.op("dve", lambda e: e.reciprocal(sx[:, 23:24], sx[:, 23:24]), reads=[xk + "rs"], writes=[xk + "rs"])
            P.op("dve", lambda e: e.scalar_tensor_tensor(out=outt[s][:, qt, 128:256], in0=odc[:], scalar=sx[:, 23:24], in1=sublnb[:], op0=ALU.mult, op1=ALU.mult),
                 reads=["od%d" % cs, xk + "rs", "sublnb"], writes=["outt%d_d%d" % (s, qt)])

        def p2_prologue(tb):
            s = tb % 2
            sx = smx[s]
            load_block(tb, s)
            proj_fm(0, 2, s); proj_fm(1, 3, s)
            rotary(2, 3, s, 0, 1, ktf[:], "ktf", 0)
            proj_fm(4, 2, s); proj_fm(5, 3, s)
            rotary(2, 3, s, 2, 3, QTd[s][:], "QTd%d" % s, 1)
            sumsq_max(QTd[s][:], "QTd%d" % s, 2, sx[:, 0:1], "qmax%d" % s)
            P.op("dve", lambda e: e.tensor_tensor(sx[:, 1:2], sx[:, 0:1], sc[:, 10:11], ALU.mult), reads=["qmax%d" % s, "kmax2"], writes=["c2_%d" % s])
            P.op("act", lambda e: e.activation(out=sx[:, 2:3], in_=sx[:, 1:2], func=AF.Sqrt, scale=(1.01 * SCL) ** 2), reads=["c2_%d" % s], writes=["c_%d" % s])
            P.op("dve", lambda e: e.tensor_scalar(sx[:, 3:4], sx[:, 2:3], -1.0, None, ALU.mult), reads=["c_%d" % s], writes=["negc%d" % s])
            for tt in range(4):
                p2_gate_tt(s, tt)
            P.op("act", lambda e: e.activation(out=gate[s][:], in_=pb[3][:].rearrange("p (j d) -> p j d", j=4), func=AF.Silu), reads=["pb3"], writes=["gate%d" % s])
            for j in range(4):
                p2_ret_chunk(tb, s, j)

        def p2_block(tb, cap):
            s = tb % 2
            if stage >= 3:
                p2_attention(s, cap)
                for qt in range(4):
                    p2_attn_epi(s, qt)
            P.dma("sp", "out%d" % s, oo[tb * 512:(tb + 1) * 512, :].rearrange("(j p) c -> p j c", p=128), outt[s][:],
                  reads=["outt%d_r%d" % (s, j) for j in range(4)] + (["outt%d_d%d" % (s, j) for j in range(4)] if stage >= 3 else []), writes=["o%d" % tb])

        cap = None
        for tb in (range(NBK) if stage >= 2 else []):
            if tb == 0:
                p2_prologue(0)
            else:
                P.replay(cap, len(cap))
            cap = None
            if tb + 1 < NBK:
                P.begin_capture()
                p2_prologue(tb + 1)
                cap = P.end_capture()
            p2_block(tb, cap)
        P.emit()
    return nc


def build_mix1(T=8192, stage=99):
    nc = bass.Bass("TRN2", target_bir_lowering=False)
    NBK = T // 512
    NCH = T // 128
    D = 1024
    NW = 8 * 128 + 320
    TOK0 = 8 * 128
    xT_h = nc.dram_tensor("xT", [D, T], F32, kind="ExternalInput")
    wC_h = nc.dram_tensor("wC", [D, NW], F32, kind="ExternalInput")
    wuq_h = nc.dram_tensor("wuq", [256, 192], F32, kind="ExternalInput")
    wukv_h = nc.dram_tensor("wukv", [128, 192], F32, kind="ExternalInput")
    nrm_h = nc.dram_tensor("nrm", [128, 3], F32, kind="ExternalInput")
    w2_h = nc.dram_tensor("w2", [48, 64], F32, kind="ExternalInput")
    gbias_h = nc.dram_tensor("gbias", [1, 128], F32, kind="ExternalInput")
    gnorm_h = nc.dram_tensor("gnorm", [1, 128], F32, kind="ExternalInput")
    tabs_h = nc.dram_tensor("tabs", [2, 128, T], F32, kind="ExternalInput")
    o_h = nc.dram_tensor("o", [T, 256], F32, kind="ExternalOutput")
    xT = xT_h.ap().rearrange("(k p) t -> p k t", p=128)
    tabs = tabs_h.ap().rearrange("f p t -> p f t")
    oo = o_h.ap()
    SCL = 96.0 ** -0.5
    QSC = 64.0 ** -0.5

    with contextlib.ExitStack() as st:
        def sb(name, shape, dt):
            return st.enter_context(nc.sbuf_tensor("s_" + name, shape, dt))
        P = Prog(nc)
        C = _mk_consts(nc, P, sb)
        wC = sb("wC", [128, 8, NW], BF16)
        wuq = sb("wuq", [128, 2, 192], BF16)
        wukv = sb("wukv", [128, 192], BF16)
        nrm = sb("nrm", [128, 3], F32)
        w2 = sb("w2", [48, 64], BF16)
        gbb = sb("gbb", [128, 2, 64], F32)
        gnb = sb("gnb", [128, 128], F32)
        xb = [sb("xb%d" % i, [128, 8, 512], BF16) for i in range(2)]
        tab = [sb("tab%d" % i, [128, 2, 512], F32) for i in range(2)]
        KT_m = sb("KT_m", [128, T], BF16)
        V_m = sb("V_m", [128, NCH, 130], BF16)
        KT_g = sb("KT_g", [128, T], BF16)
        Ktok_g = sb("Ktok_g", [128, NCH, 64], BF16)
        V_g = sb("V_g", [128, NCH, 128], BF16)
        Rst = sb("Rst", [128, NCH, 128], BF16)
        fac = sb("fac", [128, 6, 128], F32)
        sc = sb("sc", [128, 64], F32)
        Rf = sb("Rf", [128, 128], F32)
        Sf = sb("Sf", [128, 128], F32)
        S_bf = sb("S_bf", [128, 128], BF16)
        t1 = sb("t1", [128, 512], F32)
        t2 = sb("t2", [128, 512], F32)
        sqf = sb("sqf", [128, 2, 512], F32)
        rsb = sb("rsb", [128, 512], F32)
        cg = sb("cg", [128, 2, 512], BF16)
        sq = sb("sq", [128, 512], BF16)
        qtf = sb("qtf", [128, 512], F32)
        lrT = sb("lrT", [128, 512], BF16)
        Gblk = sb("Gblk", [128, 2, 4, 64], F32)
        zt = sb("zt", [128, 512], F32)
        QTm = [sb("QTm%d" % i, [128, 512], BF16) for i in range(2)]
        gate = [sb("gate%d" % i, [128, 4, 128], F32) for i in range(2)]
        outt = [sb("outt%d" % i, [128, 4, 256], F32) for i in range(2)]
        cw = [sb("cw%d" % i, [128, 6, 128], BF16) for i in range(2)]
        pm = [sb("pm%d" % i, [128, 2, 128], BF16) for i in range(2)]
        pT = [sb("pT%d" % i, [128, 512], BF16) for i in range(4)]
        od = [sb("od%d" % i, [128, 128], F32) for i in range(2)]
        smx = [sb("smx%d" % i, [128, 32], F32) for i in range(2)]
        pb = [st.enter_context(nc.psum_tensor("pb%d" % i, [128, 512], F32)) for i in range(8)]

        P.dma("pool", "wC", wC[:], wC_h.ap().rearrange("(k p) n -> p k n", p=128), writes=["wC"])
        P.dma("pool", "wuq", wuq[:], wuq_h.ap().rearrange("(k p) n -> p k n", p=128), writes=["wuq"])
        P.dma("pool", "wukv", wukv[:], wukv_h.ap(), writes=["wukv"])
        P.dma("pool", "w2", w2[:], w2_h.ap(), writes=["w2"])
        P.dma("sp", "c_nrm", nrm[:], nrm_h.ap(), writes=["nrm"])
        P.dma("sp", "c_gbb", gbb[:], bass.AP(gbias_h, 0, [[0, 128], [1, 128]]), writes=["gbb"])
        P.dma("sp", "c_gnb", gnb[:], _bcast_rows(gnorm_h, 0, 128), writes=["gnb"])
        P.op("pool", lambda e: e.memset(Rf[:], 0.0), writes=["Rf"])
        P.op("pool", lambda e: e.memset(Sf[:], 0.0), writes=["Sf"])
        P.op("pool", lambda e: e.memset(S_bf[:], 0.0), writes=["S_bf"])
        P.op("pool", lambda e: e.memset(sc[:, 10:11], 0.0), writes=["kmax2"])
        P.op("pool", lambda e: e.memset(V_m[:, :, 128:130], 1.0), writes=["V_m_ones"])
        F = {"E1f": fac[0:64, 0, :], "E2f": fac[0:64, 1, :], "E1b": fac[0:64, 2, :], "E2b": fac[0:64, 3, :], "E3f": fac[:, 4, 0:64], "E3b": fac[:, 5, 0:64]}
        dSf = fac[0:64, 0, 127:128]
        dSb = fac[0:64, 2, 0:1]

        def mm(out, lhsT, rhs, start, stop, reads, writes, skip=False):
            P.op("pe", lambda e: e.matmul(out, lhsT, rhs, start=start, stop=stop, skip_group_check=skip), reads=reads, writes=writes)

        def load_block(tb, s):
            P.dma("pool", "xb%d" % s, xb[s][:], xT[:, :, tb * 512:(tb + 1) * 512], writes=["xb%d" % s])
            P.dma("sp", "tab%d" % s, tab[s][:], tabs[:, :, tb * 512:(tb + 1) * 512], writes=["tab%d" % s])

        def proj_fm(blk, bank, s):
            for kc in range(8):
                mm(pb[bank][:], wC[:, kc, blk * 128:(blk + 1) * 128], xb[s][:, kc, :], kc == 0, kc == 7, ["wC", "xb%d" % s], ["pb%d" % bank])

        def rms_bcast(src_banks, nchunk, bank, ndim, dst, dstkey):
            for c in range(nchunk):
                P.op("act", lambda e, c=c: e.activation(out=sqf[:, c, :], in_=pb[src_banks[c]][:], func=AF.Square),
                     reads=["pb%d" % src_banks[c]], writes=["sqf%d" % c])
            for c in range(nchunk):
                mm(pb[bank][:], C["ones"][:], sqf[:, c, :], c == 0, c == nchunk - 1, ["c_ones", "sqf%d" % c], ["pb%d" % bank])
            P.op("act", lambda e: e.activation(out=dst, in_=pb[bank][:], func=AF.Sqrt, bias=1e-6, scale=1.0 / ndim), reads=["pb%d" % bank], writes=[dstkey])
            P.op("dve", lambda e: e.reciprocal(dst, dst), reads=[dstkey], writes=[dstkey])

        def sumsq_max(src, srckey, nrows, bank, dst, dstkey):
            P.op("pool", lambda e: e.tensor_tensor(sq[0:nrows, :], src, src, ALU.mult), reads=list(srckey), writes=["sq"])
            mm(pb[bank][:], C["ones_bf"][0:nrows, :], sq[0:nrows, :], True, True, ["sq", "c_ones_bf"], ["pb%d" % bank])
            P.op("dve", lambda e: e.reduce_max(out=dst, in_=pb[bank][:], axis=AX.X), reads=["pb%d" % bank], writes=[dstkey])

        def rope_rows(bx, bp, s, dst, dstkey, extra=None, extrakey=None):
            r = slice(64, 96)
            P.op("dve", lambda e: e.tensor_tensor(t1[r, :], pb[bp][r, :], tab[s][r, 1, :], ALU.mult), reads=["pb%d" % bp, "tab%d" % s], writes=["t1r"])
            P.op("dve", lambda e: e.tensor_tensor(t2[r, :], pb[bx][r, :], tab[s][r, 0, :], ALU.mult), reads=["pb%d" % bx, "tab%d" % s], writes=["t2r"])
            if extra is None:
                P.op("pool", lambda e: e.tensor_tensor(dst, t1[r, :], t2[r, :], ALU.add), reads=["t1r", "t2r"], writes=[dstkey])
            else:
                P.op("pool", lambda e: e.tensor_tensor(t1[r, :], t1[r, :], t2[r, :], ALU.add), reads=["t1r", "t2r"], writes=["t1r"])
                P.op("pool", lambda e: e.tensor_tensor(dst, t1[r, :], extra, ALU.mult), reads=["t1r", extrakey], writes=[dstkey])

        def gates(s, d_, tt):
            r = slice(0, 16) if d_ == 0 else slice(32, 48)
            reg = pb[6][:, (d_ * 4 + tt) * 64:(d_ * 4 + tt + 1) * 64]
            key = "pb6g%d_%d" % (d_, tt)
            mm(reg, lrT[r, tt * 128:(tt + 1) * 128], w2[r, :], True, True, ["lrT", "w2"], [key])
            z = zt[:, (d_ * 4 + tt) * 64:(d_ * 4 + tt + 1) * 64]
            zk = "zt%d_%d" % (d_, tt)
            P.op("dve", lambda e: e.tensor_tensor(z, reg, gbb[:, d_, :], ALU.add), reads=[key, "gbb"], writes=[zk])
            P.op("act", lambda e: e.activation(out=z, in_=z, func=AF.Exp, scale=-1.0), reads=[zk], writes=[zk])
            P.op("act", lambda e: e.activation(out=z, in_=z, func=AF.Ln, bias=1.0, scale=1.0), reads=[zk], writes=[zk])
            P.op("dve", lambda e: e.tensor_scalar(Gblk[:, d_, tt, :], z, -1.0 / 16.0, None, ALU.mult), reads=[zk], writes=["G%d_%d" % (d_, tt)])

        def p1_tt(tb, s, tt):
            ci = tb * 4 + tt
            tsl = slice(tt * 128, (tt + 1) * 128)
            mm(pb[5][:, tsl], cg[:, 0, tsl], wukv[:, 64:192], True, True, ["cg0", "wukv"], ["pb5v%d" % tt])
            mm(pb[2][:, 256 + tt:257 + tt], sqf[:, 0, tsl], C["ones"][:, 0:1], True, True, ["sqf0", "c_ones"], ["pb2c%d" % tt])
            sx = smx[s]
            P.op("act", lambda e: e.activation(out=sx[:, 24 + tt:25 + tt], in_=pb[2][:, 256 + tt:257 + tt], func=AF.Sqrt, bias=1e-6, scale=1.0 / 128.0),
                 reads=["pb2c%d" % tt], writes=["rc%d_%d" % (s, tt)])
            P.op("dve", lambda e: e.reciprocal(sx[:, 24 + tt:25 + tt], sx[:, 24 + tt:25 + tt]), reads=["rc%d_%d" % (s, tt)], writes=["rc%d_%d" % (s, tt)])
            P.op("dve", lambda e: e.tensor_scalar(V_m[:, ci, 0:128], pb[5][:, tsl], sx[:, 24 + tt:25 + tt], None, ALU.mult),
                 reads=["pb5v%d" % tt, "rc%d_%d" % (s, tt)], writes=["V_m%d" % ci])
            bank = tt % 2
            key = "pb%dtok" % bank
            for kc in range(8):
                mm(pb[bank][:, 0:320], xb[s][:, kc, tsl], wC[:, kc, TOK0:TOK0 + 320], kc == 0, kc == 7, ["wC", "xb%d" % s], [key])
            P.op("act", lambda e: e.copy(out=V_g[:, ci, :], in_=pb[bank][:, 0:128]), reads=[key], writes=["V_g%d" % ci])
            P.op("dve", lambda e: e.tensor_copy(out=Ktok_g[:, ci, :], in_=pb[bank][:, 256:320]), reads=[key], writes=["Ktok_g%d" % ci])
            gates(s, 1, tt)

        def p1_chunk(tb, tt):
            ci = tb * 4 + tt
            cs = ci % 2
            g = Gblk[:, 1, tt, :]
            _scan_factors(P, C, pb[7], "pb7", g, "G1_%d" % tt, g, "G1_%d" % tt, 64, F, "fac")
            P.op("act", lambda e: e.copy(out=Rst[0:64, ci, :], in_=Rf[0:64, :]), reads=["Rf"], writes=["Rst%d" % ci])
            P.op("pool", lambda e: e.tensor_tensor(cw[cs][:, 4, 0:64], Ktok_g[:, ci, :], F["E3b"], ALU.mult),
                 reads=["Ktok_g%d" % ci, "facE3b"], writes=["cw%d_4" % cs])
            mm(pb[4][0:64, 128:256], cw[cs][:, 4, 0:64], V_g[:, ci, :], True, True, ["cw%d_4" % cs, "V_g%d" % ci], ["pb4u"])
            P.op("dve", lambda e: e.scalar_tensor_tensor(out=Rf[0:64, :], in0=Rf[0:64, :], scalar=dSb, in1=pb[4][0:64, 128:256], op0=ALU.mult, op1=ALU.add),
                 reads=["Rf", "pb4u", "facE1b"], writes=["Rf"])

        def p1_block(n_, tb):
            s = n_ % 2
            bsl = slice(tb * 512, (tb + 1) * 512)
            load_block(tb, s)
            proj_fm(2, 0, s)
            rms_bcast([0], 1, 1, 128.0, rsb[:], "rsb")
            P.op("dve", lambda e: e.tensor_scalar(cg[:, 0, :], pb[0][:], nrm[:, 2:3], None, ALU.mult), reads=["pb0", "nrm"], writes=["cg0"])
            mm(pb[2][0:64, :], wukv[:, 0:64], cg[:, 0, :], True, True, ["wukv", "cg0"], ["pb2"])
            P.op("dve", lambda e: e.tensor_tensor(KT_m[0:64, bsl], pb[2][0:64, :], rsb[0:64, :], ALU.mult), reads=["pb2", "rsb"], writes=["KT_m%da" % tb])
            proj_fm(3, 3, s); proj_fm(4, 4, s)
            rope_rows(3, 4, s, KT_m[64:96, bsl], "KT_m%db" % tb)
            sumsq_max(KT_m[0:96, bsl], ["KT_m%da" % tb, "KT_m%db" % tb], 96, 3, sc[:, 11:12], "ktmp")
            P.op("dve", lambda e: e.tensor_tensor(sc[:, 10:11], sc[:, 10:11], sc[:, 11:12], ALU.max), reads=["kmax2", "ktmp", "KT_m%db" % tb], writes=["kmax2"])
            proj_fm(6, 3, s)
            P.op("act", lambda e: e.copy(out=KT_g[0:64, bsl], in_=pb[3][0:64, :]), reads=["pb3"], writes=["KT_g%d" % tb])
            proj_fm(7, 4, s)
            P.op("act", lambda e: e.copy(out=lrT[0:48, :], in_=pb[4][0:48, :]), reads=["pb4"], writes=["lrT"])
            for tt in range(4):
                p1_tt(tb, s, tt)
            for tt in range(3, -1, -1):
                p1_chunk(tb, tt)

        for n_, tb in enumerate(range(NBK - 1, -1, -1) if stage >= 1 else []):
            p1_block(n_, tb)

        def p2_gate_tt(s, tt):
            tsl = slice(tt * 128, (tt + 1) * 128)
            for kc in range(8):
                mm(pb[2][:, tsl], xb[s][:, kc, tsl], wC[:, kc, TOK0 + 128:TOK0 + 256], kc == 0, kc == 7, ["wC", "xb%d" % s], ["pb2"])

        def p2_gla_chunk(tb, s, j):
            ci = tb * 4 + j
            cs = ci % 2
            csl = slice(j * 128, (j + 1) * 128)
            gsl = slice(ci * 128, (ci + 1) * 128)
            cwk = "cw%d_" % cs
            c = cw[cs]
            _scan_factors(P, C, pb[7], "pb7", Gblk[:, 0, j, :], "G0_%d" % j, Gblk[:, 1, j, :], "G1_%d" % j, 64, F, "fac")
            P.op("dve", lambda e: e.scalar_tensor_tensor(out=c[0:64, 0, :], in0=qtf[0:64, csl], scalar=QSC, in1=F["E1f"], op0=ALU.mult, op1=ALU.mult),
                 reads=["qtf", "facE1f"], writes=[cwk + "0"])
            P.op("dve", lambda e: e.scalar_tensor_tensor(out=c[0:64, 1, :], in0=qtf[0:64, csl], scalar=QSC, in1=F["E1b"], op0=ALU.mult, op1=ALU.mult),
                 reads=["qtf", "facE1b"], writes=[cwk + "1"])
            P.op("dve", lambda e: e.tensor_tensor(c[0:64, 2, :], KT_g[0:64, gsl], F["E2f"], ALU.mult), reads=["KT_g%d" % tb, "facE2f"], writes=[cwk + "2"])
            P.op("pool", lambda e: e.tensor_tensor(c[0:64, 3, :], KT_g[0:64, gsl], F["E2b"], ALU.mult), reads=["KT_g%d" % tb, "facE2b"], writes=[cwk + "3"])
            mm(pb[3][:, 0:128], c[0:64, 2, :], c[0:64, 0, :], True, True, [cwk + "2", cwk + "0"], ["pb3a"])
            mm(pb[3][:, 128:256], c[0:64, 3, :], c[0:64, 1, :], True, True, [cwk + "3", cwk + "1"], ["pb3b"])
            pmc = pm[cs]
            P.op("dve", lambda e: e.tensor_tensor(pmc[:, 0, :], pb[3][:, 0:128], C["tri_le"][:], ALU.mult), reads=["pb3a", "c_tri_le"], writes=["pm%d_0" % cs])
            P.op("dve", lambda e: e.tensor_tensor(pmc[:, 1, :], pb[3][:, 128:256], C["tri_gt"][:], ALU.mult), reads=["pb3b", "c_tri_gt"], writes=["pm%d_1" % cs])
            oreg = pb[3][:, 256:384]
            mm(oreg, pmc[:, 0, :], V_g[:, ci, :], True, False, ["pm%d_0" % cs, "V_g%d" % ci], ["pb3o"])
            mm(oreg, pmc[:, 1, :], V_g[:, ci, :], False, False, ["pm%d_1" % cs, "V_g%d" % ci], ["pb3o"])
            mm(oreg, c[0:64, 0, :], S_bf[0:64, :], False, False, [cwk + "0", "S_bf"], ["pb3o"])
            mm(oreg, c[0:64, 1, :], Rst[0:64, ci, :], False, True, [cwk + "1", "Rst%d" % ci], ["pb3o"])
            P.op("pool", lambda e: e.tensor_tensor(c[:, 4, 0:64], Ktok_g[:, ci, :], F["E3f"], ALU.mult), reads=["Ktok_g%d" % ci, "facE3f"], writes=[cwk + "4"])
            mm(pb[3][0:64, 384:512], c[:, 4, 0:64], V_g[:, ci, :], True, True, [cwk + "4", "V_g%d" % ci], ["pb3u"])
            P.op("dve", lambda e: e.scalar_tensor_tensor(out=Sf[0:64, :], in0=Sf[0:64, :], scalar=dSf, in1=pb[3][0:64, 384:512], op0=ALU.mult, op1=ALU.add),
                 reads=["Sf", "pb3u", "facE1f"], writes=["Sf"])
            P.op("act", lambda e: e.copy(out=S_bf[0:64, :], in_=Sf[0:64, :]), reads=["Sf"], writes=["S_bf"])
            sx = smx[s]; xk = "gn%d" % s
            odc = od[cs]
            P.op("act", lambda e: e.activation(out=odc[:], in_=oreg, func=AF.Square, accum_out=sx[:, 8:9]), reads=["pb3o"], writes=["od%d" % cs, xk + "ss"])
            P.op("act", lambda e: e.activation(out=sx[:, 9:10], in_=sx[:, 8:9], func=AF.Sqrt, bias=1e-6, scale=1.0 / 128.0), reads=[xk + "ss"], writes=[xk + "rs"])
            P.op("dve", lambda e: e.reciprocal(sx[:, 9:10], sx[:, 9:10]), reads=[xk + "rs"], writes=[xk + "rs"])
            P.op("dve", lambda e: e.scalar_tensor_tensor(out=odc[:], in0=oreg, scalar=sx[:, 9:10], in1=gnb[:], op0=ALU.mult, op1=ALU.mult),
                 reads=["pb3o", xk + "rs", "gnb", "od%d" % cs], writes=["od%d" % cs])
            P.op("pool", lambda e: e.tensor_tensor(outt[s][:, j, 128:256], odc[:], gate[s][:, j, :], ALU.mult),
                 reads=["od%d" % cs, "gate%d" % s], writes=["outt%d_g%d" % (s, j)])

        def p2_attn_qk(s, kt, i):
            sbank = i % 2
            psl = i % 4
            negc = smx[s][:, 3:4]
            mm(pb[sbank][:], KT_m[0:96, kt * 128:(kt + 1) * 128], QTm[s][0:96, :], True, True,
               ["KT_m%da" % (kt // 4), "KT_m%db" % (kt // 4), "QTm%da" % s, "QTm%db" % s], ["pb%d" % sbank])
            P.op("act", lambda e: e.activation(out=pT[psl][:], in_=pb[sbank][:], func=AF.Exp, bias=negc, scale=SCL),
                 reads=["pb%d" % sbank, "negc%d" % s], writes=["pT%d" % psl])

        def p2_attn_pv(s, kt, i):
            psl = i % 4
            for qt in range(4):
                bank = 4 + qt // 2
                areg = pb[bank][:, (qt % 2) * 256:(qt % 2) * 256 + 129]
                mm(areg, pT[psl][:, qt * 128:(qt + 1) * 128], V_m[:, kt, 0:129], kt == 0 and qt % 2 == 0, kt == NCH - 1,
                   ["pT%d" % psl, "V_m%d" % kt, "V_m_ones"], ["pb%d_acc%d" % (bank, qt)], skip=True)

        def p2_attention(s, cap):
            per = 0 if not cap else -(-len(cap) // max(1, NCH - 4))
            for i in range(min(2, NCH)):
                p2_attn_qk(s, i, i)
            for kt in range(NCH):
                p2_attn_pv(s, kt, kt)
                if kt + 2 < NCH:
                    p2_attn_qk(s, kt + 2, kt + 2)
                if cap:
                    P.replay(cap, per)

        def p2_attn_epi(s, qt):
            a0 = pb[4 + qt // 2][:, (qt % 2) * 256:(qt % 2) * 256 + 129]
            k0 = "pb%d_acc%d" % (4 + qt // 2, qt)
            sx = smx[s]; xk = "da%d" % s
            P.op("dve", lambda e: e.reciprocal(sx[:, 20:21], a0[:, 128:129]), reads=[k0], writes=[xk + "z0"])
            P.op("dve", lambda e: e.tensor_scalar(outt[s][:, qt, 0:128], a0[:, 0:128], sx[:, 20:21], None, ALU.mult), reads=[k0, xk + "z0"], writes=["outt%d_m%d" % (s, qt)])

        def p2_prologue(tb):
            s = tb % 2
            sx = smx[s]
            load_block(tb, s)
            proj_fm(0, 2, s); proj_fm(1, 3, s)
            rms_bcast([2, 3], 2, 6, 256.0, rsb[:], "rsb")
            for c_ in range(2):
                P.op("dve", lambda e, c_=c_: e.tensor_scalar(cg[:, c_, :], pb[2 + c_][:], nrm[:, c_:c_ + 1], None, ALU.mult), reads=["pb%d" % (2 + c_), "nrm"], writes=["cg%d" % c_])
            for c_ in range(2):
                mm(pb[2][0:96, :], wuq[:, c_, 0:96], cg[:, c_, :], c_ == 0, c_ == 1, ["wuq", "cg0", "cg1"], ["pb2"])
            for c_ in range(2):
                mm(pb[3][0:96, :], wuq[:, c_, 96:192], cg[:, c_, :], c_ == 0, c_ == 1, ["wuq", "cg0", "cg1"], ["pb3"])
            P.op("dve", lambda e: e.tensor_tensor(QTm[s][0:64, :], pb[2][0:64, :], rsb[0:64, :], ALU.mult), reads=["pb2", "rsb"], writes=["QTm%da" % s])
            rope_rows(2, 3, s, QTm[s][64:96, :], "QTm%db" % s, extra=rsb[64:96, :], extrakey="rsb")
            sumsq_max(QTm[s][0:96, :], ["QTm%da" % s, "QTm%db" % s], 96, 6, sx[:, 0:1], "qmax%d" % s)
            P.op("dve", lambda e: e.tensor_tensor(sx[:, 1:2], sx[:, 0:1], sc[:, 10:11], ALU.mult), reads=["qmax%d" % s, "kmax2", "QTm%db" % s], writes=["c2_%d" % s])
            P.op("act", lambda e: e.activation(out=sx[:, 2:3], in_=sx[:, 1:2], func=AF.Sqrt, scale=(1.01 * SCL) ** 2), reads=["c2_%d" % s], writes=["c_%d" % s])
            P.op("dve", lambda e: e.tensor_scalar(sx[:, 3:4], sx[:, 2:3], -1.0, None, ALU.mult), reads=["c_%d" % s], writes=["negc%d" % s])
            proj_fm(5, 6, s)
            P.op("act", lambda e: e.copy(out=qtf[0:64, :], in_=pb[6][0:64, :]), reads=["pb6"], writes=["qtf"])
            proj_fm(7, 6, s)
            P.op("act", lambda e: e.copy(out=lrT[0:48, :], in_=pb[6][0:48, :]), reads=["pb6"], writes=["lrT"])
            for tt in range(4):
                gates(s, 0, tt)
                gates(s, 1, tt)
            for tt in range(4):
                p2_gate_tt(s, tt)
            P.op("act", lambda e: e.activation(out=gate[s][:], in_=pb[2][:].rearrange("p (j d) -> p j d", j=4), func=AF.Silu), reads=["pb2"], writes=["gate%d" % s])
            for j in range(4):
                p2_gla_chunk(tb, s, j)

        def p2_block(tb, cap):
            s = tb % 2
            if stage >= 3:
                p2_attention(s, cap)
                for qt in range(4):
                    p2_attn_epi(s, qt)
            P.dma("sp", "out%d" % s, oo[tb * 512:(tb + 1) * 512, :].rearrange("(j p) c -> p j c", p=128), outt[s][:],
                  reads=["outt%d_g%d" % (s, j) for j in range(4)] + (["outt%d_m%d" % (s, j) for j in range(4)] if stage >= 3 else []), writes=["o%d" % tb])

        cap = None
        for tb in (range(NBK) if stage >= 2 else []):
            if tb == 0:
                p2_prologue(0)
            else:
                P.replay(cap, len(cap))
            cap = None
            if tb + 1 < NBK:
                P.begin_capture()
                p2_prologue(tb + 1)
                cap = P.end_capture()
            p2_block(tb, cap)
        P.emit()
    return nc


def _rot_tables(T, rot_dim, theta, ndim, period):
    half = rot_dim // 2
    pos = np.arange(T, dtype=np.float32)
    inv = np.power(np.float32(theta), -np.arange(0, rot_dim, 2, dtype=np.float32) / np.float32(rot_dim)).astype(np.float32)
    ang = (pos[None, :] * inv[:, None]).astype(np.float32)
    cos = np.cos(ang.astype(np.float64)).astype(np.float32)
    sin = np.sin(ang.astype(np.float64)).astype(np.float32)
    ct = np.ones((ndim, T), np.float32)
    stb = np.zeros((ndim, T), np.float32)
    for d in range(ndim):
        l = d % period
        if l < half:
            ct[d] = cos[l]; stb[d] = -sin[l]
        elif l < rot_dim:
            ct[d] = cos[l - half]; stb[d] = sin[l - half]
    return ct, stb


def _rot_perm(ndim, rot_dim, period, offset=0):
    half = rot_dim // 2
    p = np.arange(ndim)
    for d in range(ndim):
        l = (d - offset) % period
        if d < offset:
            continue
        if l < half:
            p[d] = d + half
        elif l < rot_dim:
            p[d] = d - half
    return p


def _pack_mix0_weights(w_in, h):
    rq = w_in[:, 0 * 512 + h * 128: 0 * 512 + (h + 1) * 128]
    rk = w_in[:, 1 * 512 + h * 128: 1 * 512 + (h + 1) * 128]
    rv = w_in[:, 2 * 512 + h * 128: 2 * 512 + (h + 1) * 128]
    rg = w_in[:, 3 * 512 + h * 128: 3 * 512 + (h + 1) * 128]
    dq = w_in[:, 4 * 512 + h * 128: 4 * 512 + (h + 1) * 128]
    dk = w_in[:, 5 * 512 + h * 128: 5 * 512 + (h + 1) * 128]
    dv = w_in[:, 6 * 512 + h * 128: 6 * 512 + (h + 1) * 128]
    pr = _rot_perm(128, 128, 128)
    pd = _rot_perm(128, 16, 64)
    return np.ascontiguousarray(np.concatenate([rq, rq[:, pr], rk, rk[:, pr], dq, dq[:, pd], dk, dk[:, pd], rv, dv, rg], axis=1))


def _mix0_tables(T):
    cR, sR = _rot_tables(T, 128, 10000.0, 128, 128)
    cD, sD = _rot_tables(T, 16, 500000.0, 128, 64)
    return np.ascontiguousarray(np.stack([cR, sR, cD, sD]))


def _pack_mix1(inp_w_in, w_uq, w_ukv, q_norm, kv_norm, w2f, bf, w2b, bb, gla_norm, h):
    o = np.cumsum([0, 256, 128, 32, 256, 256, 512, 512, 16, 16])
    w = inp_w_in
    cq = w[:, o[0]:o[1]]; ckv = w[:, o[1]:o[2]]; kr = w[:, o[2]:o[3]]
    gq = w[:, o[3] + h * 64:o[3] + (h + 1) * 64]; gk = w[:, o[4] + h * 64:o[4] + (h + 1) * 64]
    gv = w[:, o[5] + h * 128:o[5] + (h + 1) * 128]; gg = w[:, o[6] + h * 128:o[6] + (h + 1) * 128]
    lrf = w[:, o[7]:o[8]]; lrb = w[:, o[8]:o[9]]
    z = lambda n: np.zeros((1024, n), np.float32)
    p32 = np.concatenate([np.arange(16, 32), np.arange(0, 16)])
    blk3 = np.concatenate([z(64), kr, z(32)], 1)
    blk4 = np.concatenate([z(64), kr[:, p32], z(32)], 1)
    blk5 = np.concatenate([gq, z(64)], 1)
    blk6 = np.concatenate([gk, z(64)], 1)
    blk7 = np.concatenate([lrf, z(16), lrb, z(80)], 1)
    wC = np.ascontiguousarray(np.concatenate([cq, ckv, blk3, blk4, blk5, blk6, blk7, gv, gg, gk], 1))
    uq = w_uq[:, h * 96:(h + 1) * 96]
    pq = np.concatenate([np.arange(64), 64 + p32])
    wuq = np.ascontiguousarray(np.concatenate([uq, uq[:, pq]], 1))
    wukv = np.ascontiguousarray(w_ukv[:, h * 192:(h + 1) * 192])
    nrm = np.ascontiguousarray(np.stack([q_norm[0:128], q_norm[128:256], kv_norm], 1))
    w2 = np.zeros((48, 64), np.float32)
    w2[0:16] = w2f[:, h * 64:(h + 1) * 64]; w2[32:48] = w2b[:, h * 64:(h + 1) * 64]
    gbias = np.concatenate([bf[h * 64:(h + 1) * 64], bb[h * 64:(h + 1) * 64]])[None, :]
    return {"wC": wC, "wuq": wuq, "wukv": wukv, "nrm": nrm, "w2": w2, "gbias": np.ascontiguousarray(gbias), "gnorm": np.ascontiguousarray(gla_norm[None, :])}


def _mix1_tables(T):
    half = 16
    pos = np.arange(T, dtype=np.float32)
    inv = np.power(np.float32(500000.0), -np.arange(0, 32, 2, dtype=np.float32) / np.float32(32)).astype(np.float32)
    ang = (pos[None, :] * inv[:, None]).astype(np.float32)
    cos = np.cos(ang.astype(np.float64)).astype(np.float32); sin = np.sin(ang.astype(np.float64)).astype(np.float32)
    ct = np.ones((128, T), np.float32); stb = np.zeros((128, T), np.float32)
    for l in range(32):
        if l < half:
            ct[64 + l] = cos[l]; stb[64 + l] = -sin[l]
        else:
            ct[64 + l] = cos[l - half]; stb[64 + l] = sin[l - half]
    return np.ascontiguousarray(np.stack([ct, stb]))


_PROGS = {}


def _prog(name, builder):
    if name not in _PROGS:
        _PROGS[name] = builder()
    return _PROGS[name]


def _run(nc, maps):
    res = run_bass_kernel_spmd(nc, maps, core_ids=list(range(NCORES)))
    return res.results


def _post_launch(x_flat, cat_flat, w_out, lnp, w_r, b_r, wg, wu, wd):
    nc = _prog("post", lambda: build_post(2048))
    maps = []
    for c in range(NCORES):
        sl = slice(c * 2048, (c + 1) * 2048)
        maps.append({"xres": np.ascontiguousarray(x_flat[sl]), "catT": np.ascontiguousarray(cat_flat[sl].T), "w_out": w_out,
                     "lnp": lnp, "w_r": w_r, "b_r": b_r, "w_gate": wg, "w_up": wu, "w_down": wd})
    outs = _run(nc, maps)
    return np.concatenate([outs[c]["xo"] for c in range(NCORES)], axis=0)


def kernel(x, ev_w_in, ev_ret_decay_f, ev_ret_decay_b, ev_lq1, ev_lk1, ev_lq2, ev_lk2, ev_subln, ev_w_out,
           od_w_in, od_q_norm, od_w_uq, od_kv_norm, od_w_ukv, od_gla_w2_f, od_gla_b_f, od_gla_w2_b, od_gla_b_b, od_gla_norm, od_w_out,
           ln1_g, ln1_b, ln2_g, ln2_b, moe_w_grp, moe_b_grp, moe_w_exp, moe_b_exp, moe_w_gate, moe_w_up, moe_w_down):
    f32 = lambda a: np.ascontiguousarray(np.asarray(a, dtype=np.float32))
    x = f32(x)
    B, T, D = x.shape
    H = 4

    def post(layer, x_flat, cat_flat, w_out):
        lnp = f32(np.stack([np.asarray(ln1_g)[layer], np.asarray(ln1_b)[layer], np.asarray(ln2_g)[layer], np.asarray(ln2_b)[layer]]))
        w_r = f32(np.concatenate([np.asarray(moe_w_grp)[layer], np.asarray(moe_w_exp)[layer]], axis=1))
        b_r = f32(np.concatenate([np.asarray(moe_b_grp)[layer], np.asarray(moe_b_exp)[layer]])[None, :])
        return _post_launch(x_flat, cat_flat, f32(w_out), lnp, w_r, b_r, f32(np.asarray(moe_w_gate)[layer]),
                            f32(np.asarray(moe_w_up)[layer]), f32(np.asarray(moe_w_down)[layer]))

    nc0 = _prog("mix0", lambda: build_mix0(T))
    tabs0 = _mix0_tables(T)
    w_in0 = f32(np.asarray(ev_w_in)[0])
    lv = f32(np.stack([np.asarray(ev_lq1)[0], np.asarray(ev_lk1)[0], np.asarray(ev_lq2)[0], np.asarray(ev_lk2)[0]]))
    subln = f32(np.asarray(ev_subln)[0][None, :])
    xT = [np.ascontiguousarray(x[b].T) for b in range(B)]
    maps = []
    for c in range(NCORES):
        b, h = divmod(c, H)
        dec = f32(np.array([[np.asarray(ev_ret_decay_f)[0][h], np.asarray(ev_ret_decay_b)[0][h]]]))
        maps.append({"xT": xT[b], "wA": _pack_mix0_weights(w_in0, h), "tabs": tabs0, "dec": dec, "lv": lv, "subln": subln})
    outs = _run(nc0, maps)
    cat = np.empty((B, T, D), np.float32)
    for c in range(NCORES):
        b, h = divmod(c, H)
        cat[b, :, h * 128:(h + 1) * 128] = outs[c]["o"][:, 0:128]
        cat[b, :, 512 + h * 128:512 + (h + 1) * 128] = outs[c]["o"][:, 128:256]
    x1 = post(0, x.reshape(B * T, D), cat.reshape(B * T, D), np.asarray(ev_w_out)[0]).reshape(B, T, D)

    nc1 = _prog("mix1", lambda: build_mix1(T))
    tabs1 = _mix1_tables(T)
    xT = [np.ascontiguousarray(x1[b].T) for b in range(B)]
    maps = []
    for c in range(NCORES):
        b, h = divmod(c, H)
        m = _pack_mix1(f32(np.asarray(od_w_in)[0]), f32(np.asarray(od_w_uq)[0]), f32(np.asarray(od_w_ukv)[0]), f32(np.asarray(od_q_norm)[0]),
                       f32(np.asarray(od_kv_norm)[0]), f32(np.asarray(od_gla_w2_f)[0]), f32(np.asarray(od_gla_b_f)[0]),
                       f32(np.asarray(od_gla_w2_b)[0]), f32(np.asarray(od_gla_b_b)[0]), f32(np.asarray(od_gla_norm)[0]), h)
        m["xT"] = xT[b]; m["tabs"] = tabs1
        maps.append(m)
    outs = _run(nc1, maps)
    for c in range(NCORES):
        b, h = divmod(c, H)
        cat[b, :, h * 128:(h + 1) * 128] = outs[c]["o"][:, 0:128]
        cat[b, :, 512 + h * 128:512 + (h + 1) * 128] = outs[c]["o"][:, 128:256]
    x2 = post(1, x1.reshape(B * T, D), cat.reshape(B * T, D), np.asarray(od_w_out)[0]).reshape(B, T, D)
    return x2.astype(np.float32)
```
